# Optimizing a Trainium2 kernel written in Bass

```python
import math
import jax, jax.numpy as jnp
from jax import lax
import numpy as np

D_MODEL = 1024
BATCH = 8
SEQ = 8192
DEPTH = 4

HEAD_DIM = 64
GROUP_W = D_MODEL // 4
HEADS_PER_MIXER = GROUP_W // HEAD_DIM
MIX_W = 4 * GROUP_W
NORM_EPS = 1e-6
GDN_CONV = 4
GDN_CHUNK = 64
SGU_CHUNK = 128
SGU_LN_EPS = 1e-5
SC_CONV = 3
RW_DECAY_LORA = 64
RW_AAA_LORA = 64
RW_GATE_LORA = 128
RW_GN_EPS = 64e-5
RW_IN_W = 3 * GROUP_W + RW_DECAY_LORA + RW_AAA_LORA + RW_GATE_LORA
IN_SIZES = (3 * GROUP_W, GROUP_W, HEADS_PER_MIXER, HEADS_PER_MIXER, GROUP_W, GROUP_W,
            GROUP_W, GROUP_W, GROUP_W, RW_IN_W)
IN_W = sum(IN_SIZES)
N_GROUPS = 8
EXPERTS_PER_GROUP = 8
N_EXPERTS = N_GROUPS * EXPERTS_PER_GROUP
TOP_K = 2
D_EXPERT = 256
MOE_BLOCK = 128

kernel_name = "hybrid_headgroup_gdn_sgu_conv_rwkv7_hmoe"

F32 = jnp.float32


def _split(p, sizes):
    idx, s = [], 0
    for n in sizes[:-1]:
        s += n
        idx.append(s)
    return jnp.split(p, idx, axis=-1)


def rmsnorm(x, g):
    xf = x.astype(F32)
    y = xf * lax.rsqrt(jnp.mean(xf * xf, -1, keepdims=True) + NORM_EPS)
    return (y * g.astype(F32)).astype(x.dtype)


def modulate(h, shift, scale):
    return h * (1.0 + scale[:, None, :]) + shift[:, None, :]


def causal_conv(x, w):
    k = w.shape[0]
    return lax.conv_general_dilated(x, w[:, None, :].astype(x.dtype), window_strides=(1,),
                                    padding=[(k - 1, 0)], dimension_numbers=('NWC', 'WIO', 'NWC'),
                                    feature_group_count=x.shape[-1])


def token_shift(x):
    return jnp.pad(x[:, :-1], ((0, 0), (1, 0), (0, 0)))


def l2norm(x):
    return x * lax.rsqrt(jnp.sum(x * x, -1, keepdims=True) + 1e-6)


def gated_delta_chunked(q, k, v, g, beta):
    bn, s, h, dh = q.shape
    c = GDN_CHUNK
    n = s // c

    def chunks(t):
        return jnp.moveaxis(t.reshape(bn, n, c, h, -1), 3, 1)

    q = chunks(q) * (dh ** -0.5)
    k = chunks(k)
    v = chunks(v)
    gc = jnp.cumsum(jnp.moveaxis(g.reshape(bn, n, c, h), 3, 1), axis=-1)
    beta = jnp.moveaxis(beta.reshape(bn, n, c, h), 3, 1)
    pos = jnp.arange(c)
    causal = pos[:, None] >= pos[None, :]
    strict = pos[:, None] > pos[None, :]
    decay = jnp.exp(jnp.where(causal, gc[..., :, None] - gc[..., None, :], -jnp.inf))
    k_beta = k * beta[..., None]
    a_mat = jnp.where(strict, jnp.einsum('bhnid,bhnjd->bhnij', k_beta, k) * decay, 0.0)
    rhs = jnp.concatenate([v * beta[..., None], k_beta * jnp.exp(gc)[..., None]], -1)
    sol = lax.linalg.triangular_solve(a_mat, rhs, left_side=True, lower=True, unit_diagonal=True)
    u, w = sol[..., :dh], sol[..., dh:]
    attn = jnp.einsum('bhnid,bhnjd->bhnij', q, k) * decay
    q_dec = q * jnp.exp(gc)[..., None]
    k_tail = k * jnp.exp(gc[..., -1:] - gc)[..., None]
    chunk_dec = jnp.exp(gc[..., -1])

    def step(state, inp):
        u_i, w_i, a_i, qd_i, kt_i, cd_i = inp
        v_new = u_i - jnp.einsum('bhck,bhkv->bhcv', w_i, state)
        o = jnp.einsum('bhck,bhkv->bhcv', qd_i, state) + jnp.einsum('bhij,bhjv->bhiv', a_i, v_new)
        state = state * cd_i[..., None, None] + jnp.einsum('bhck,bhcv->bhkv', kt_i, v_new)
        return state, o

    xs = tuple(jnp.moveaxis(t, 2, 0) for t in (u, w, attn, q_dec, k_tail, chunk_dec))
    _, o = lax.scan(step, jnp.zeros((bn, h, dh, dh), F32), xs)
    return jnp.transpose(o, (1, 0, 3, 2, 4)).reshape(bn, s, h, dh)


def gdn_mixer(qkv, z, a, b, conv_w, a_log, dt_bias, norm_g):
    bn, s, _ = qkv.shape
    h, n = HEADS_PER_MIXER, HEAD_DIM
    qkv = jax.nn.silu(causal_conv(qkv, conv_w)).astype(F32)
    q, k, v = (t.reshape(bn, s, h, n) for t in jnp.split(qkv, 3, axis=-1))
    q = l2norm(q)
    k = l2norm(k)
    g = -jnp.exp(a_log.astype(F32)) * jax.nn.softplus(a.astype(F32) + dt_bias.astype(F32))
    beta = jax.nn.sigmoid(b.astype(F32))
    o = gated_delta_chunked(q, k, v, g, beta)
    o = o * lax.rsqrt(jnp.mean(o * o, -1, keepdims=True) + NORM_EPS) * norm_g.astype(F32)
    o = o.reshape(bn, s, GROUP_W) * jax.nn.silu(z.astype(F32))
    return o.astype(z.dtype)


def sgu_mixer(u, v, ln_g, ln_b, w_s, b_s):
    bn, s, _ = u.shape
    t, h = SGU_CHUNK, HEADS_PER_MIXER
    u = jax.nn.gelu(u)
    vf = jax.nn.gelu(v).astype(F32)
    mean = jnp.mean(vf, -1, keepdims=True)
    var = jnp.mean(jnp.square(vf - mean), -1, keepdims=True)
    v = ((vf - mean) * lax.rsqrt(var + SGU_LN_EPS) * ln_g.astype(F32) + ln_b.astype(F32)).astype(u.dtype)
    v = v.reshape(bn, s // t, t, h, HEAD_DIM)
    ws = jnp.where(jnp.tril(jnp.ones((t, t), bool)), w_s, 0.0).astype(u.dtype)
    mixed = jnp.einsum('hts,bnshc->bnthc', ws, v) + b_s.T[None, None, :, :, None]
    return (u.reshape(bn, s // t, t, h, HEAD_DIM) * mixed).reshape(bn, s, GROUP_W)


def short_conv_mixer(gate_b, gate_c, hx, conv_w):
    return gate_b * causal_conv(gate_c * hx, conv_w)


def rwkv7_scan(r, w, k, v, za, zb):
    bn, s, h, n = r.shape
    xs = tuple(jnp.moveaxis(t, 1, 0) for t in (r, w, k, v, za, zb))

    def step(state, inp):
        r_t, w_t, k_t, v_t, a_t, b_t = inp
        sa = jnp.einsum('bhvk,bhk->bhv', state, a_t)
        state = (state * w_t[:, :, None, :] + sa[..., None] * b_t[:, :, None, :]
                 + v_t[..., None] * k_t[:, :, None, :])
        return state, jnp.einsum('bhvk,bhk->bhv', state, r_t)

    _, y = lax.scan(step, jnp.zeros((bn, h, n, n), F32), xs)
    return jnp.moveaxis(y, 0, 1)


def rwkv7_mixer(p, mu, w0, w_up, a0, a_up, g_up, k_k, k_a, r_k, gn_g, gn_b):
    bn, s, _ = p.shape
    g_w, h, n = GROUP_W, HEADS_PER_MIXER, HEAD_DIM
    p = p + (token_shift(p) - p) * mu
    r, k, v, xw, xa, xg = _split(p, (g_w, g_w, g_w, RW_DECAY_LORA, RW_AAA_LORA, RW_GATE_LORA))
    w_log = -jax.nn.softplus(-(w0 + jnp.tanh(xw) @ w_up).astype(F32)) - 0.5
    decay = jnp.exp(-jnp.exp(w_log))
    a = jax.nn.sigmoid((a0 + xa @ a_up).astype(F32))
    gate = jax.nn.sigmoid(xg) @ g_up

    def heads(t):
        return t.astype(F32).reshape(bn, s, h, n)

    kf = k.astype(F32)
    kk = heads(kf * k_k.astype(F32))
    kk = kk * lax.rsqrt(jnp.sum(kk * kk, -1, keepdims=True) + 1e-12)
    k_mod = heads(kf * (1.0 + (a - 1.0) * k_a.astype(F32)))
    a_h = heads(a)
    r_h = heads(r)
    v_h = heads(v)
    y = rwkv7_scan(r_h, heads(decay), k_mod, v_h, -kk, kk * a_h)
    mean = jnp.mean(y, -1, keepdims=True)
    var = jnp.mean(jnp.square(y - mean), -1, keepdims=True)
    y = (y - mean) * lax.rsqrt(var + RW_GN_EPS)
    y = y * gn_g.astype(F32).reshape(h, n) + gn_b.astype(F32).reshape(h, n)
    y = y + jnp.sum(r_h * k_mod * r_k.astype(F32), -1, keepdims=True) * v_h
    return (y.reshape(bn, s, g_w) * gate.astype(F32)).astype(p.dtype)


def expert_dispatch(h, eidx, gates, w_gate, w_up, w_down):
    t, d = h.shape
    a_n = t * TOP_K
    flat_e = eidx.reshape(a_n)
    flat_tok = jnp.repeat(jnp.arange(t, dtype=jnp.int32), TOP_K)
    flat_g = gates.reshape(a_n)
    order = jnp.argsort(flat_e)
    se = flat_e[order]
    counts = jnp.bincount(flat_e, length=N_EXPERTS)
    start = jnp.cumsum(counts) - counts
    padded = (counts + MOE_BLOCK - 1) // MOE_BLOCK * MOE_BLOCK
    pad_end = jnp.cumsum(padded)
    pad_start = pad_end - padded
    dest = pad_start[se] + jnp.arange(a_n, dtype=jnp.int32) - start[se]
    p_rows = (a_n + MOE_BLOCK - 1) // MOE_BLOCK * MOE_BLOCK + N_EXPERTS * MOE_BLOCK
    n_blk = p_rows // MOE_BLOCK
    row_tok = jnp.full((p_rows,), t, jnp.int32).at[dest].set(flat_tok[order])
    row_g = jnp.zeros((p_rows,), h.dtype).at[dest].set(flat_g[order])
    blk_e = jnp.minimum(jnp.searchsorted(pad_end, jnp.arange(n_blk) * MOE_BLOCK, side='right'),
                        N_EXPERTS - 1)
    h_pad = jnp.concatenate([h, jnp.zeros((1, d), h.dtype)], 0)

    def run(args):
        tok, e, g = args
        xb = h_pad[tok]
        hid = jax.nn.silu(xb @ w_gate[e]) * (xb @ w_up[e])
        return (hid @ w_down[e]) * g[:, None]

    y = lax.map(run, (row_tok.reshape(n_blk, MOE_BLOCK), blk_e, row_g.reshape(n_blk, MOE_BLOCK)))
    out = jnp.zeros((t + 1, d), h.dtype).at[row_tok].add(y.reshape(p_rows, d))
    return out[:t]


def hier_moe(h, w_group, b_group, w_router, b_router, w_gate, w_up, w_down):
    bn, s, d = h.shape
    t = bn * s
    hf = h.reshape(t, d)
    glog = (hf @ w_group + b_group).astype(F32)
    gprob = jax.nn.softmax(glog, axis=-1)
    gsel = jnp.argmax(glog, axis=-1).astype(jnp.int32)
    p_group = jnp.take_along_axis(gprob, gsel[:, None], axis=-1)
    elog = (hf @ w_router + b_router).astype(F32).reshape(t, N_GROUPS, EXPERTS_PER_GROUP)
    elog = jnp.take_along_axis(elog, gsel[:, None, None], axis=1)[:, 0]
    top_v, top_i = lax.top_k(elog, TOP_K)
    gates = jax.nn.softmax(top_v, axis=-1) * p_group
    eidx = gsel[:, None] * EXPERTS_PER_GROUP + top_i.astype(jnp.int32)
    y = expert_dispatch(hf, eidx, gates.astype(h.dtype), w_gate, w_up, w_down)
    return y.reshape(bn, s, d)


def setup_inputs(seed: int = 0) -> dict:
    key = jax.random.key(seed)
    keys = jax.random.split(key, 48)
    cnt = [0]
    L, D, G, H, N = DEPTH, D_MODEL, GROUP_W, HEADS_PER_MIXER, HEAD_DIM

    def nk():
        cnt[0] += 1
        return keys[cnt[0] - 1]

    def nrm(shape, scale):
        return jax.random.normal(nk(), shape, F32) * scale

    def gain(shape):
        return 1.0 + nrm(shape, 0.02)

    def unif(shape, lo, hi):
        return jax.random.uniform(nk(), shape, F32, lo, hi)

    x = nrm((BATCH, SEQ, D), 1.0)
    c = nrm((BATCH, D), 1.0)
    ada_w = nrm((L, D, 6 * D), 0.5 * D ** -0.5)
    ada_b = nrm((L, 6 * D), 0.02)
    mix_norm_g = gain((L, D))
    ffn_norm_g = gain((L, D))
    w_in = nrm((L, D, IN_W), D ** -0.5)
    w_out = nrm((L, MIX_W, D), MIX_W ** -0.5)
    gdn_conv_w = nrm((L, GDN_CONV, 3 * G), GDN_CONV ** -0.5)
    gdn_a_log = jnp.log(unif((L, H), 1.0, 16.0))
    dt = jnp.exp(unif((L, H), math.log(1e-3), math.log(1e-1)))
    gdn_dt_bias = dt + jnp.log(-jnp.expm1(-dt))
    gdn_norm_g = gain((L, N))
    sgu_ln_g = gain((L, G))
    sgu_ln_b = nrm((L, G), 0.02)
    sgu_w = nrm((L, H, SGU_CHUNK, SGU_CHUNK), SGU_CHUNK ** -0.5)
    sgu_b = gain((L, H, SGU_CHUNK))
    sc_conv_w = nrm((L, SC_CONV, G), SC_CONV ** -0.5)
    rw_mu = unif((L, RW_IN_W), 0.0, 1.0)
    rw_w0 = unif((L, G), -4.0, 1.0)
    rw_w_up = nrm((L, RW_DECAY_LORA, G), 0.5 * RW_DECAY_LORA ** -0.5)
    rw_a0 = nrm((L, G), 0.5)
    rw_a_up = nrm((L, RW_AAA_LORA, G), RW_AAA_LORA ** -0.5)
    rw_g_up = nrm((L, RW_GATE_LORA, G), RW_GATE_LORA ** -0.5)
    rw_k_k = 0.85 + nrm((L, G), 0.02)
    rw_k_a = gain((L, G))
    rw_r_k = nrm((L, H, N), 0.1)
    rw_gn_g = gain((L, G))
    rw_gn_b = nrm((L, G), 0.02)
    moe_w_group = nrm((L, D, N_GROUPS), D ** -0.5)
    moe_b_group = nrm((L, N_GROUPS), 0.01)
    moe_w_router = nrm((L, D, N_EXPERTS), D ** -0.5)
    moe_b_router = nrm((L, N_EXPERTS), 0.01)
    moe_w_gate = nrm((L, N_EXPERTS, D, D_EXPERT), D ** -0.5)
    moe_w_up = nrm((L, N_EXPERTS, D, D_EXPERT), D ** -0.5)
    moe_w_down = nrm((L, N_EXPERTS, D_EXPERT, D), D_EXPERT ** -0.5)
    final_norm_g = gain((D,))
    return {'x': x, 'c': c, 'ada_w': ada_w, 'ada_b': ada_b, 'mix_norm_g': mix_norm_g,
            'ffn_norm_g': ffn_norm_g, 'w_in': w_in, 'w_out': w_out, 'gdn_conv_w': gdn_conv_w,
            'gdn_a_log': gdn_a_log, 'gdn_dt_bias': gdn_dt_bias, 'gdn_norm_g': gdn_norm_g,
            'sgu_ln_g': sgu_ln_g, 'sgu_ln_b': sgu_ln_b, 'sgu_w': sgu_w, 'sgu_b': sgu_b,
            'sc_conv_w': sc_conv_w, 'rw_mu': rw_mu, 'rw_w0': rw_w0, 'rw_w_up': rw_w_up,
            'rw_a0': rw_a0, 'rw_a_up': rw_a_up, 'rw_g_up': rw_g_up, 'rw_k_k': rw_k_k,
            'rw_k_a': rw_k_a, 'rw_r_k': rw_r_k, 'rw_gn_g': rw_gn_g, 'rw_gn_b': rw_gn_b,
            'moe_w_group': moe_w_group, 'moe_b_group': moe_b_group, 'moe_w_router': moe_w_router,
            'moe_b_router': moe_b_router, 'moe_w_gate': moe_w_gate, 'moe_w_up': moe_w_up,
            'moe_w_down': moe_w_down, 'final_norm_g': final_norm_g}


def reference(x, c, ada_w, ada_b, mix_norm_g, ffn_norm_g, w_in, w_out, gdn_conv_w, gdn_a_log,
              gdn_dt_bias, gdn_norm_g, sgu_ln_g, sgu_ln_b, sgu_w, sgu_b, sc_conv_w, rw_mu, rw_w0,
              rw_w_up, rw_a0, rw_a_up, rw_g_up, rw_k_k, rw_k_a, rw_r_k, rw_gn_g, rw_gn_b,
              moe_w_group, moe_b_group, moe_w_router, moe_b_router, moe_w_gate, moe_w_up,
              moe_w_down, final_norm_g):
    c_act = jax.nn.silu(c)
    for l in range(DEPTH):
        mod = c_act @ ada_w[l] + ada_b[l]
        sh_m, sc_m, gt_m, sh_f, sc_f, gt_f = jnp.split(mod, 6, axis=-1)
        h = modulate(rmsnorm(x, mix_norm_g[l]), sh_m, sc_m)
        p = h @ w_in[l]
        qkv, z, ga, gb, su, sv, cb, cc, ch, rp = _split(p, IN_SIZES)
        o_a = gdn_mixer(qkv, z, ga, gb, gdn_conv_w[l], gdn_a_log[l], gdn_dt_bias[l], gdn_norm_g[l])
        o_b = sgu_mixer(su, sv, sgu_ln_g[l], sgu_ln_b[l], sgu_w[l], sgu_b[l])
        o_c = short_conv_mixer(cb, cc, ch, sc_conv_w[l])
        o_d = rwkv7_mixer(rp, rw_mu[l], rw_w0[l], rw_w_up[l], rw_a0[l], rw_a_up[l], rw_g_up[l],
                          rw_k_k[l], rw_k_a[l], rw_r_k[l], rw_gn_g[l], rw_gn_b[l])
        mixed = jnp.concatenate([o_a, o_b, o_c, o_d], axis=-1) @ w_out[l]
        x = x + gt_m[:, None, :] * mixed
        h = modulate(rmsnorm(x, ffn_norm_g[l]), sh_f, sc_f)
        y = hier_moe(h, moe_w_group[l], moe_b_group[l], moe_w_router[l], moe_b_router[l],
                     moe_w_gate[l], moe_w_up[l], moe_w_down[l])
        x = x + gt_f[:, None, :] * y
    return rmsnorm(x, final_norm_g)
```

```python
import contextlib
import numpy as np
import concourse.bass as bass
import concourse.mybir as mybir

F32 = mybir.dt.float32
BF16 = mybir.dt.bfloat16
I32 = mybir.dt.int32
AF = mybir.ActivationFunctionType
ALU = mybir.AluOpType
AX = mybir.AxisListType


class V:
    __slots__ = ("t", "ap")

    def __init__(self, t, ap):
        self.t = t
        self.ap = ap

    def __getitem__(self, k):
        return V(self.t, self.ap[k])

    def bc(self, shape):
        return V(self.t, self.ap.to_broadcast(list(shape)))

    def re(self, pat, **kw):
        return V(self.t, self.ap.rearrange(pat, **kw))

    def bitcast(self, dt):
        return V(self.t, self.ap.bitcast(dt))

    def pbc(self, n):
        return V(self.t, self.ap.partition_broadcast(n))

    def unsq(self, ax):
        return V(self.t, self.ap.unsqueeze(ax))


class T:
    def __init__(self, h, name):
        self.h = h
        self.name = name
        self.w = None
        self.r = {}
        self.excl = False

    def __getitem__(self, k):
        return V(self, self.h[k])

    @property
    def v(self):
        return V(self, self.h[:])


def _ap(x):
    return x.ap if isinstance(x, V) else x


def _ts(*xs):
    out = []
    for x in xs:
        if isinstance(x, V) and x.t not in out:
            out.append(x.t)
    return out


class Prog:
    ENG = ("pe", "act", "dve", "pool", "sp")

    def __init__(self, nc):
        self.nc = nc
        self.ops = {e: [] for e in self.ENG}
        self.cnt = {}
        self.seen = {e: {} for e in self.ENG}
        self.nops = 0
        self._stacks = []
        self._dslot = {}

    @contextlib.contextmanager
    def scope(self):
        st = contextlib.ExitStack()
        self._stacks.append(st)
        try:
            with st:
                yield
                self.barrier()
        finally:
            self._stacks.pop()

    def _uniq(self, name):
        self._uid = getattr(self, "_uid", 0) + 1
        return "%s_u%d" % (name, self._uid)

    def sb(self, name, shape, dt=F32):
        name = self._uniq(name)
        h = self._stacks[-1].enter_context(self.nc.sbuf_tensor(name, list(shape), dt))
        return T(h, name)

    def ps(self, name, shape, dt=F32):
        name = self._uniq(name)
        h = self._stacks[-1].enter_context(self.nc.psum_tensor(name, list(shape), dt))
        t = T(h, name)
        t.excl = True
        return t

    def dram(self, name, shape, dt=F32, kind="Internal"):
        name = self._uniq(name)
        return T(self.nc.dram_tensor(name, list(shape), dt, kind=kind), name)

    def _emit(self, eng, key, inc, fn, reads, writes):
        waits = {}

        def need(dep):
            if dep is None:
                return
            k, c = dep
            if k == eng and eng == "pe":
                return
            if self.seen[eng].get(k, 0) >= c:
                return
            if waits.get(k, 0) < c:
                waits[k] = c

        if key != eng and self.cnt.get(key, 0) > 0:
            need((key, self.cnt[key]))
        for b in reads:
            need(b.w)
            if b.excl:
                for k, c in b.r.items():
                    if k != key:
                        need((k, c))
        for b in writes:
            need(b.w)
            for k, c in b.r.items():
                need((k, c))
        for k, c in waits.items():
            self.seen[eng][k] = c
        self.cnt[key] = self.cnt.get(key, 0) + 1
        my = self.cnt[key]
        self.ops[eng].append((fn, sorted(waits.items()), key, inc))
        for b in writes:
            b.w = (key, my)
            b.r = {}
        for b in reads:
            if b not in writes:
                b.r[key] = my
        self.nops += 1

    def barrier(self):
        snap = dict(self.cnt)
        for eng in self.ENG:
            waits = {}
            for k, c in snap.items():
                if self.seen[eng].get(k, 0) < c:
                    waits[k] = c
                    self.seen[eng][k] = c
            if waits:
                self.ops[eng].append((None, sorted(waits.items()), None, 0))

    def op(self, eng, fn, reads=(), writes=()):
        self._emit(eng, eng, 1, fn, list(reads), list(writes))

    NSLOT = 20

    def _dkey(self, q):
        n = self._dslot.get(q, 0)
        self._dslot[q] = n + 1
        return "dma_%s_%d" % (q, n % self.NSLOT)

    def dma(self, q, out, in_, extra_reads=(), **kw):
        key = self._dkey(q)
        o, i = _ap(out), _ap(in_)
        self._emit(q, key, 16, lambda e: e.dma_start(out=o, in_=i, **kw),
                   _ts(in_) + list(extra_reads), _ts(out))

    def gather(self, out, in_, idx):
        o, i, ix = _ap(out), _ap(in_), _ap(idx)
        self._emit("pool", self._dkey("pool"), 16,
                   lambda e: e.indirect_dma_start(out=o, out_offset=None, in_=i,
                                                  in_offset=bass.IndirectOffsetOnAxis(ap=ix, axis=0)),
                   _ts(in_, idx), _ts(out))

    def scatter(self, out, in_, idx):
        o, i, ix = _ap(out), _ap(in_), _ap(idx)
        self._emit("pool", self._dkey("pool"), 16,
                   lambda e: e.indirect_dma_start(out=o, out_offset=bass.IndirectOffsetOnAxis(ap=ix, axis=0),
                                                  in_=i, in_offset=None),
                   _ts(in_, idx), _ts(out))

    def mm(self, out, lhsT, rhs, start=True, stop=True):
        o, l, r = _ap(out), _ap(lhsT), _ap(rhs)
        self.op("pe", lambda e: e.matmul(o, l, r, start=start, stop=stop),
                reads=_ts(lhsT, rhs), writes=_ts(out))

    def tr(self, out, in_, ident):
        o, i, d = _ap(out), _ap(in_), _ap(ident)
        self.op("pe", lambda e: e.transpose(o, i, d), reads=_ts(in_, ident), writes=_ts(out))

    def tt(self, out, a, b, op, eng="dve"):
        o, x, y = _ap(out), _ap(a), _ap(b)
        self.op(eng, lambda e: e.tensor_tensor(out=o, in0=x, in1=y, op=op),
                reads=_ts(a, b), writes=_ts(out))

    def ts(self, out, a, s1, op0, s2=None, op1=None, eng="dve", accum=None):
        o, x, p1, p2, ac = _ap(out), _ap(a), _ap(s1), _ap(s2), _ap(accum)
        kw = {}
        if op1 is not None:
            kw["op1"] = op1
        if accum is not None:
            kw["accum_out"] = ac
        self.op(eng, lambda e: e.tensor_scalar(out=o, in0=x, scalar1=p1, scalar2=p2, op0=op0, **kw),
                reads=_ts(a, s1, s2), writes=_ts(out, accum))

    def stt(self, out, a, s, b, op0, op1, eng="dve"):
        o, x, p, y = _ap(out), _ap(a), _ap(s), _ap(b)
        self.op(eng, lambda e: e.scalar_tensor_tensor(out=o, in0=x, scalar=p, in1=y, op0=op0, op1=op1),
                reads=_ts(a, s, b), writes=_ts(out))

    def act(self, out, a, func, bias=None, scale=None, accum=None):
        o, x, b, s, ac = _ap(out), _ap(a), _ap(bias), _ap(scale), _ap(accum)
        kw = {}
        if bias is not None:
            kw["bias"] = b
        if scale is not None:
            kw["scale"] = s
        if accum is not None:
            kw["accum_out"] = ac
        self.op("act", lambda e: e.activation(out=o, in_=x, func=func, **kw),
                reads=_ts(a, bias, scale), writes=_ts(out, accum))

    def copy(self, out, a, eng="dve"):
        o, x = _ap(out), _ap(a)
        if eng == "act":
            self.op("act", lambda e: e.activation(out=o, in_=x, func=AF.Copy), reads=_ts(a), writes=_ts(out))
        else:
            self.op(eng, lambda e: e.tensor_copy(out=o, in_=x), reads=_ts(a), writes=_ts(out))

    def memset(self, out, val, eng="dve"):
        o = _ap(out)
        self.op(eng, lambda e: e.memset(o, val), writes=_ts(out))

    def reduce(self, out, a, op, axis=None, eng="dve"):
        o, x = _ap(out), _ap(a)
        ax = axis if axis is not None else AX.X
        self.op(eng, lambda e: e.tensor_reduce(out=o, in_=x, axis=ax, op=op), reads=_ts(a), writes=_ts(out))

    def scan(self, out, d0, d1, init, op0, op1):
        o, x, y, i = _ap(out), _ap(d0), _ap(d1), _ap(init)
        self.op("dve", lambda e: e.tensor_tensor_scan(out=o, data0=x, data1=y, initial=i, op0=op0, op1=op1),
                reads=_ts(d0, d1, init), writes=_ts(out))

    def run(self, body):
        nc = self.nc
        with contextlib.ExitStack() as st:
            self._stacks.append(st)
            body(self)
            self.barrier()
            keys = sorted(self.cnt.keys())
            sems = {k: st.enter_context(nc.semaphore("s_" + k)) for k in keys}
            blk = st.enter_context(nc.Block())
            mult = {k: (16 if k.startswith("dma_") else 1) for k in keys}

            def replay(engname):
                def f(e):
                    for fn, waits, key, inc in self.ops[engname]:
                        for k, c in waits:
                            e.wait_ge(sems[k], c * mult[k])
                        if fn is not None:
                            fn(e).then_inc(sems[key], inc)
                return f

            blk.tensor(replay("pe"))
            blk.scalar(replay("act"))
            blk.vector(replay("dve"))
            blk.gpsimd(replay("pool"))
            blk.sync(replay("sp"))
        return nc
from concourse.bass_utils import run_bass_kernel_spmd

D = 1024
INW = 3336
NEXP = 64
NEG = -1.0e30
C_Z, C_A, C_B, C_SU, C_SV = 0, 256, 260, 264, 520
NPT = 776
FM_MU, FM_W0, FM_A0, FM_KK, FM_KA, FM_RK, FM_GNG, FM_GNB, FM_CW, FM_SC = 0, 8, 10, 12, 14, 16, 18, 20, 22, 46
NFM = 52
BS_LNG, BS_LNB, BS_GN, BS_ALOG, BS_DT, BS_RB = 0, 256, 512, 768, 772, 776
NBS = 848
GELU_C = 1.5957691216057308


def make_consts(nblk):
    p = np.arange(128)
    blk = p // 64
    same = blk[:, None] == blk[None, :]
    i = p[:, None]
    j = p[None, :]
    parts = {}
    parts["ident"] = np.eye(128)
    parts["BD"] = same
    parts["UBLK"] = same & (i <= j)
    parts["ones"] = np.ones((128, 128))
    parts["NSL"] = -(same & (i > j)).astype(np.float64)
    parts["SL"] = same & (i > j)
    parts["SU"] = same & (i < j)
    parts["IU"] = same & (i <= j)
    parts["NEGL"] = np.where(same & (i >= j), 0.0, NEG)
    parts["NEGU"] = np.where(same & (i <= j), 0.0, NEG)
    parts["CHI"] = (blk[:, None] == np.arange(2)[None, :])
    parts["SU128"] = (i < j)
    parts["IU128"] = (i <= j)
    parts["iota64"] = np.tile(np.arange(64)[None, :], (128, 1))
    parts["pidx"] = p[:, None]
    parts["iotaB"] = np.tile((np.arange(nblk) * 128)[None, :], (128, 1))
    offs = {}
    cols = []
    o = 0
    for k, v in parts.items():
        v = np.asarray(v, dtype=np.float32)
        offs[k] = (o, o + v.shape[1])
        cols.append(v)
        o += v.shape[1]
    return np.ascontiguousarray(np.concatenate(cols, axis=1)), offs


TAPSHAPES = lambda SEQ, NT: {"p_tm": [SEQ, NPT], "oT": [NT, 128, 8 * 128], "x1": [SEQ, D], "h2": [SEQ, D],
                             "route": [SEQ, 6], "x2": [SEQ, D]}


def build(SEQ, DEPTH, taps=()):
    import os
    STOP = os.environ.get('KSTOP', '')
    NT = SEQ // 128
    PROWS = 2 * SEQ + NEXP * 128
    NBLK = PROWS // 128
    cvals, coffs = make_consts(NBLK)
    NCONST = cvals.shape[1]
    L = DEPTH
    nc = bass.Bass("TRN2", target_bir_lowering=False)

    def din(name, shape, dt=F32):
        return T(nc.dram_tensor(name, list(shape), dt, kind="ExternalInput"), name)

    x_d = din("x", [SEQ, D])
    cfm_d = din("cfm", [128, 8])
    adaw_d = din("ada_w", [L, D, 6 * D])
    adab_d = din("ada_b", [L, 6 * D])
    g12_d = din("g12", [L, 2 * D])
    win_d = din("w_in", [L, D, INW])
    wout_d = din("w_out", [L, D, D])
    fmp_d = din("fmp", [L, 128, NFM])
    bcs_d = din("bcs", [L, NBS])
    swT_d = din("sgu_wT", [L, 4, 128, 128])
    sbT_d = din("sgu_bT", [L, 128, 4])
    lwa_d = din("lora_wa", [L, 128, 256])
    gup_d = din("g_up", [L, 128, 256])
    wr_d = din("wr", [L, D, 72])
    ewg_d = din("ewg", [L * NEXP * 128, 2048])
    ewu_d = din("ewu", [L * NEXP * 128, 2048])
    ewd_d = din("ewd", [L * NEXP * 128, 2048])
    fing_d = din("fin_g", [D])
    con_d = din("consts", [128, NCONST])
    out_d = T(nc.dram_tensor("out", [SEQ, D], F32, kind="ExternalOutput"), "out")
    tap_d = {}
    for tname in taps:
        tap_d[tname] = T(nc.dram_tensor("tap_" + tname, TAPSHAPES(SEQ, NT)[tname], F32, kind="ExternalOutput"), tname)

    P = Prog(nc)

    def body(P):
        xres = P.dram("xres", [SEQ, D])
        hf = P.dram("hf", [SEQ, D])
        hs = P.dram("hs", [PROWS, D], BF16)
        ys = P.dram("ys", [PROWS, D])

        con = P.sb("con", [128, NCONST])
        P.dma("sp", con.v, con_d.v)

        def K(name):
            a, b = coffs[name]
            return con[:, a:b]

        ident, BDm, UBLK, ONES = K("ident"), K("BD"), K("UBLK"), K("ones")
        NSL, SLm, SUm, IUm, NEGL, NEGU = K("NSL"), K("SL"), K("SU"), K("IU"), K("NEGL"), K("NEGU")
        CHI, SU128, IU128, IOTA64, PIDX, IOTAB = K("CHI"), K("SU128"), K("IU128"), K("iota64"), K("pidx"), K("iotaB")
        identb = P.sb("identb", [128, 128], BF16)
        P.copy(identb.v, ident)

        banks = [P.ps("bank%d" % i, [128, 512]) for i in range(8)]
        bstate = [0]

        def nb():
            b = banks[bstate[0] % 8]
            bstate[0] += 1
            return b

        with P.scope():
            zt = P.sb("zt", [128, D], BF16)
            P.memset(zt.v, 0.0)
            for b in range(NBLK):
                P.dma("sp", hs[b * 128:(b + 1) * 128, :], zt.v)

        cact = P.sb("cact", [128, 8])
        P.dma("sp", cact.v, cfm_d.v)
        P.act(cact.v, cact.v, AF.Silu)

        eidx = P.sb("eidx", [128, NT, 2])
        gate = P.sb("gate", [128, NT, 2])
        rank = P.sb("rank", [128, NT, 2])
        dest_f = P.sb("dest_f", [128, NT, 2])
        dest_i = P.sb("dest_i", [128, NT, 2], I32)
        base = P.sb("base", [128, 64])
        idxb = P.sb("idxb", [128, NBLK], I32)
        modb = P.sb("modb", [128, 4, D])
        MB_GTM, MB_SHF, MB_G2, MB_GTF = 0, 1, 2, 3
        mfm = P.sb("mfm", [128, 16])
        gtf_prev = P.sb("gtf_prev", [128, D])

        def recip(out, in_):
            o, i_ = out.ap, in_.ap
            P.op("dve", lambda e: e.reciprocal(out=o, in_=i_), reads=_ts(in_), writes=_ts(out))

        def rsqrt(out, in_, eps, scale=1.0):
            P.act(out, in_, AF.Sqrt, bias=eps, scale=scale)
            recip(out, out)

        def tap(name, dst_fn, src):
            if name in tap_d:
                P.dma("sp", V(tap_d[name], dst_fn(tap_d[name].h)), src)

        def v3(t):
            return t.v.re("p (c t) -> p c t", c=2)

        def h4(v):
            return v.re("p (h c) -> p h c", h=4)

        def bc4(v):
            return v.unsq(2).bc([128, 4, 64])

        for l in range(L):
            with P.scope():
                pan = [P.sb("pan%d" % i, [128, 8, 512]) for i in range(2)]
                adab = P.sb("adab", [128, 6 * D])
                g12 = P.sb("g12", [128, 2 * D])
                crep = P.sb("crep", [128, 8, 128])
                mtmp = P.sb("mtmp", [128, 2, D])
                for k in range(8):
                    P.ts(crep[:, k, :], ONES, cact[:, k:k + 1], ALU.mult)
                P.dma("sp", adab.v, adab_d[l, :].pbc(128))
                P.dma("sp", g12.v, g12_d[l, :].pbc(128))
                awv = adaw_d[l].re("(k p) n -> p k n", p=128)
                dst = {0: mtmp[:, 0, :], 1: mtmp[:, 1, :], 2: modb[:, MB_GTM, :], 3: modb[:, MB_SHF, :],
                       4: modb[:, MB_G2, :], 5: modb[:, MB_GTF, :]}
                for n in range(12):
                    pn = pan[n % 2]
                    P.dma("sp", pn.v, awv[:, :, n * 512:(n + 1) * 512])
                    bk = nb()
                    for k in range(8):
                        P.mm(bk[:, :], crep[:, k, :], pn[:, k, :], start=(k == 0), stop=(k == 7))
                    seg, half = n // 2, n % 2
                    P.tt(dst[seg][:, half * 512:(half + 1) * 512], bk[:, :], adab[:, n * 512:(n + 1) * 512], ALU.add)
                P.stt(mtmp[:, 1, :], mtmp[:, 1, :], 1.0, g12[:, 0:D], ALU.add, ALU.mult)
                P.stt(modb[:, MB_G2, :], modb[:, MB_G2, :], 1.0, g12[:, D:2 * D], ALU.add, ALU.mult)
                for (which, col0) in ((1, 0), (0, 8)):
                    for half in range(2):
                        bk = nb()
                        for c4 in range(4):
                            c = half * 4 + c4
                            P.tr(bk[:, c4 * 128:(c4 + 1) * 128], mtmp[:, which, c * 128:(c + 1) * 128], ident)
                        P.copy(mfm[:, col0 + half * 4:col0 + half * 4 + 4], bk[:, :].re("p (c t) -> p c t", c=4)[:, :, 0])

            if STOP == 'mod':
                return
            with P.scope():
                w_in = P.sb("w_in", [128, 8, INW], BF16)
                w_out = P.sb("w_out", [128, 8, D], BF16)
                wiv = win_d[l].re("(k p) n -> p k n", p=128)
                wov = wout_d[l].re("(k p) n -> p k n", p=128)
                for k in range(8):
                    P.dma("pool", w_in[:, k, 0:1668], wiv[:, k, 0:1668])
                    P.dma("pool", w_in[:, k, 1668:INW], wiv[:, k, 1668:INW])
                    P.dma("pool", w_out[:, k, :], wov[:, k, :])
                fmp = P.sb("fmp", [128, NFM])
                bcs = P.sb("bcs", [128, NBS])
                P.dma("sp", fmp.v, fmp_d[l])
                P.dma("sp", bcs.v, bcs_d[l, :].pbc(128))
                nexpA = P.sb("nexpA", [128, 4])
                P.act(nexpA.v, bcs[:, BS_ALOG:BS_ALOG + 4], AF.Exp)
                P.ts(nexpA.v, nexpA.v, -1.0, ALU.mult)
                wsT = P.sb("wsT", [128, 4, 128])
                P.dma("sp", wsT.v, swT_d[l].re("h s t -> s h t"))
                for h in range(4):
                    P.tt(wsT[:, h, :], wsT[:, h, :], IU128, ALU.mult)
                bsT = P.sb("bsT", [128, 4])
                P.dma("sp", bsT.v, sbT_d[l])
                lwW = P.sb("lwW", [128, 256])
                lwA = P.sb("lwA", [128, 256])
                gup = P.sb("gup", [128, 256])
                P.memset(lwW.v, 0.0)
                P.memset(lwA.v, 0.0)
                P.dma("sp", lwW[0:64, :], lwa_d[l, 0:64, :])
                P.dma("sp", lwA[64:128, :], lwa_d[l, 64:128, :])
                P.dma("sp", gup.v, gup_d[l])
                wr = P.sb("wr", [128, 8, 72])
                P.dma("sp", wr.v, wr_d[l].re("(k p) n -> p k n", p=128))

                Sg = P.sb("Sg", [128, 2, 128])
                Sr = P.sb("Sr", [128, 2, 128])
                qkv_raw = P.sb("qkv_raw", [128, 6, 131])
                prodC = P.sb("prodC", [128, 2, 130])
                rw_raw = P.sb("rw_raw", [128, 8, 129])
                for t_ in (Sg, Sr, qkv_raw, prodC, rw_raw, base):
                    P.memset(t_.v, 0.0)

                xt = P.sb("xt", [128, D])
                hh = P.sb("hh", [128, D])
                hT = P.sb("hT", [128, 8, 128], BF16)
                h2T = P.sb("h2T", [128, 8, 128])
                h2f = h2T.v.re("p k t -> p (k t)")
                p_tm = P.sb("p_tm", [128, NPT])
                cacc = P.sb("cacc", [128, 6, 128])
                cbch = P.sb("cbch", [128, 6, 128])
                rw = P.sb("rw", [128, 8, 128])
                oT = P.sb("oT", [128, 8, 128], BF16)
                oTf = P.sb("oTf", [128, 8, 128]) if "oT" in tap_d else None
                s1 = P.sb("s1", [128, 8])
                qk = P.sb("qk", [128, 4, 128])
                sq4 = P.sb("sq4", [128, 4, 128])
                kv_tm = P.sb("kv_tm", [128, 512])
                g4 = P.sb("g4", [128, 4])
                t4 = P.sb("t4", [128, 4])
                beta4 = P.sb("beta4", [128, 4])
                gm = P.sb("gm", [128, 2, 4])
                gc = P.sb("gc", [128, 4])
                ngc = P.sb("ngc", [128, 4])
                glr = P.sb("glr", [128, 8])
                glt = P.sb("glt", [128, 4])
                egc = P.sb("egc", [128, 4])
                ktf = P.sb("ktf", [128, 4])
                cdg = P.sb("cdg", [128, 8])
                bg = P.sb("bg", [128, 4])
                gcrow = P.sb("gcrow", [128, 4, 128])
                egcrow = P.sb("egcrow", [128, 4, 128])
                qdT = P.sb("qdT", [128, 256])
                vb = P.sb("vb", [128, 256])
                kbg = P.sb("kbg", [128, 256])
                ktail = P.sb("ktail", [128, 256])
                stmp = P.sb("stmp", [128, 128])
                cdp = P.sb("cdp", [128, 4])
                u_sb = P.sb("u_sb", [128, 256])
                wT_sb = P.sb("wT_sb", [128, 256])
                vnew = P.sb("vnew", [128, 256])
                o_sb = P.sb("o_sb", [128, 256])
                o_sq = P.sb("o_sq", [128, 256])
                sz = P.sb("sz", [128, 256])
                DD = [dict(tmp=P.sb("dd_tmp%d" % i, [128, 128]), D=P.sb("dd_D%d" % i, [128, 128]),
                           DT=P.sb("dd_DT%d" % i, [128, 128])) for i in range(2)]
                HM = []
                for h in range(4):
                    HM.append(dict(M1=P.sb("hm_M1%d" % h, [128, 128]), M2=P.sb("hm_M2%d" % h, [128, 128]),
                                   M3=P.sb("hm_M3%d" % h, [128, 128]), PQ=P.sb("hm_PQ%d" % h, [128, 2, 128]),
                                   R=P.sb("hm_R%d" % h, [128, 128])))
                lg = P.sb("lg", [128, 72])
                r8 = P.sb("r8", [128, 8, 8])
                sel = P.sb("sel", [128, 8])
                ohg = P.sb("ohg", [128, 8])
                oh1 = P.sb("oh1", [128, 8])
                oh2 = P.sb("oh2", [128, 8])
                Mx = [P.sb("Mx%d" % i, [128, 64]) for i in range(3)]
                rke = P.sb("rke", [128, 64])
                rt = P.sb("rt", [128, 64])
                sc = P.sb("sc", [128, 8])
                for t_ in (vnew, o_sq, o_sb, sz):
                    P.memset(t_.v, 0.0)

                kkT, kmT = qk[:, 0:2, :], qk[:, 2:4, :]
                cum, eW = gcrow[:, 0:2, :], gcrow[:, 2:4, :]
                eWi, eWp = egcrow[:, 0:2, :], egcrow[:, 2:4, :]
                AtT, BtT = kv_tm[:, 0:256].re("p (c t) -> p c t", c=2), kv_tm[:, 256:512].re("p (c t) -> p c t", c=2)
                KtT, RtT = cbch[:, 0:2, :], cbch[:, 2:4, :]
                lz, asg, gateT, lact = v3(qdT), v3(vb), v3(kbg), v3(ktail)
                rkb, ynT = v3(u_sb), v3(wT_sb)
                Z_sb, U_sb, y_sb, y_sq = vnew, o_sq, o_sb, sz
                bkv_tm = cacc.v.re("p (q x) t -> p q (x t)", q=3)
                ugl, vgl, ob = o_sb, o_sq, sz
                wT3 = v3(wT_sb)
                qd3 = v3(qdT)

                def neumann():
                    for lvl in range(1, 6):
                        bks = []
                        for h in range(4):
                            hm = HM[h]
                            bk = nb()
                            Pk, Qk = hm["PQ"][:, 0, :], hm["PQ"][:, 1, :]
                            P.mm(bk[:, 0:128], Qk, Pk)
                            if lvl < 5:
                                P.mm(bk[:, 128:256], Pk, Qk)
                            bks.append(bk)
                        for h in range(4):
                            n = 256 if lvl < 5 else 128
                            P.copy(HM[h]["PQ"].v.re("p a t -> p (a t)")[:, 0:n], bks[h][:, 0:n], eng="act")
                        for h in range(4):
                            P.mm(bks[h][:, 256:384], HM[h]["PQ"][:, 0, :], HM[h]["R"].v)
                        for h in range(4):
                            P.tt(HM[h]["R"].v, bks[h][:, 256:384], HM[h]["R"].v, ALU.add)

                def rstd_of(dst, src, eps):
                    P.act(h2f, src, AF.Square, accum=s1[:, 0:1])
                    rsqrt(dst, s1[:, 0:1], eps, 1.0 / D)

                for i in range(NT):
                    tsl = slice(i * 128, (i + 1) * 128)
                    if l == 0:
                        P.dma("sp", xt.v, x_d[tsl, :])
                    else:
                        P.dma("sp", xt.v, xres[tsl, :])
                        P.gather(hh.v, ys.v, dest_i[:, i, 0:1])
                        P.gather(h2f, ys.v, dest_i[:, i, 1:2])
                        P.ts(hh.v, hh.v, gate[:, i, 0:1], ALU.mult)
                        P.stt(hh.v, h2f, gate[:, i, 1:2], hh.v, ALU.mult, ALU.add)
                        P.tt(hh.v, hh.v, gtf_prev.v, ALU.mult)
                        P.tt(xt.v, xt.v, hh.v, ALU.add)
                        if l == 1:
                            tap("x2", lambda hd: hd[tsl, :], xt.v)
                    rstd_of(s1[:, 1:2], xt.v, 1e-6)
                    P.ts(hh.v, xt.v, s1[:, 1:2], ALU.mult)
                    for half in range(2):
                        bk = nb()
                        for c4 in range(4):
                            c = half * 4 + c4
                            P.tr(bk[:, c4 * 128:(c4 + 1) * 128], hh[:, c * 128:(c + 1) * 128], ident)
                        for c4 in range(4):
                            c = half * 4 + c4
                            P.act(hT[:, c, :], bk[:, c4 * 128:(c4 + 1) * 128], AF.Identity,
                                  bias=mfm[:, 8 + c:9 + c], scale=mfm[:, c:c + 1])
                    for (c0, c1) in ((768, 1280), (1280, 1544)):
                        bk = nb()
                        for k in range(8):
                            P.mm(bk[:, 0:c1 - c0], hT[:, k, :], w_in[:, k, c0:c1], start=(k == 0), stop=(k == 7))
                        P.copy(p_tm[:, c0 - 768:c1 - 768], bk[:, 0:c1 - c0], eng="act")
                    tap("p_tm", lambda hd: hd[tsl, :], p_tm.v)

                    def fm_group(col0, nch, dst_fn):
                        for g0 in range(0, nch, 4):
                            n = min(4, nch - g0)
                            bk = nb()
                            for cc in range(n):
                                cs_ = col0 + (g0 + cc) * 128
                                for k in range(8):
                                    P.mm(bk[:, cc * 128:(cc + 1) * 128], w_in[:, k, cs_:cs_ + 128], hT[:, k, :],
                                         start=(k == 0), stop=(k == 7))
                            P.copy(dst_fn(g0, n), bk[:, 0:n * 128].re("p (c t) -> p c t", c=n), eng="act")
                    fm_group(0, 6, lambda g0, n: qkv_raw[:, g0:g0 + n, 3:131])
                    fm_group(1544, 6, lambda g0, n: cbch[:, g0:g0 + n, :])
                    fm_group(2312, 8, lambda g0, n: rw_raw[:, g0:g0 + n, 1:129])

                    if STOP == 'proj':
                        continue
                    P.tt(prodC[:, :, 2:130], cbch[:, 2:4, :], cbch[:, 4:6, :], ALU.mult)
                    for c in range(2):
                        w = lambda k: fmp[:, FM_SC + c * 3 + k:FM_SC + c * 3 + k + 1]
                        P.ts(cacc[:, c, :], prodC[:, c, 2:130], w(2), ALU.mult)
                        P.stt(cacc[:, c, :], prodC[:, c, 1:129], w(1), cacc[:, c, :], ALU.mult, ALU.add)
                        P.stt(cacc[:, c, :], prodC[:, c, 0:128], w(0), cacc[:, c, :], ALU.mult, ALU.add)
                    P.tt(oT[:, 4:6, :], cacc[:, 0:2, :], cbch[:, 0:2, :], ALU.mult)
                    if oTf is not None:
                        P.tt(oTf[:, 4:6, :], cacc[:, 0:2, :], cbch[:, 0:2, :], ALU.mult)
                    P.copy(prodC[:, :, 0:2], prodC[:, :, 128:130])

                    suv = p_tm[:, C_SU:C_SU + 512]
                    gtmp = kv_tm.v
                    P.tt(gtmp, suv, suv, ALU.mult)
                    P.ts(gtmp, gtmp, 0.044715, ALU.mult, 1.0, ALU.add)
                    P.tt(gtmp, gtmp, suv, ALU.mult)
                    P.act(gtmp, gtmp, AF.Sigmoid, scale=GELU_C)
                    P.tt(ugl.v, gtmp[:, 0:256], suv[:, 0:256], ALU.mult)
                    P.tt(vgl.v, gtmp[:, 256:512], suv[:, 256:512], ALU.mult)
                    P.reduce(s1[:, 2:3], vgl.v, ALU.add)
                    P.ts(s1[:, 3:4], s1[:, 2:3], -1.0 / 256, ALU.mult)
                    P.ts(vgl.v, vgl.v, s1[:, 3:4], ALU.add)
                    P.act(gtmp[:, 0:256], vgl.v, AF.Square, accum=s1[:, 4:5])
                    rsqrt(s1[:, 5:6], s1[:, 4:5], 1e-5, 1.0 / 256)
                    P.stt(vgl.v, vgl.v, s1[:, 5:6], bcs[:, BS_LNG:BS_LNG + 256], ALU.mult, ALU.mult)
                    P.tt(vgl.v, vgl.v, bcs[:, BS_LNB:BS_LNB + 256], ALU.add)
                    bk = nb()
                    for h in range(4):
                        P.mm(bk[:, h * 64:(h + 1) * 64], wsT[:, h, :], vgl[:, h * 64:(h + 1) * 64])
                    P.tt(h4(ob.v), h4(bk[:, 0:256]), bc4(bsT.v), ALU.add)
                    P.tt(ob.v, ob.v, ugl.v, ALU.mult)
                    bk = nb()
                    for c in range(2):
                        P.tr(bk[:, c * 128:(c + 1) * 128], ob[:, c * 128:(c + 1) * 128], ident)
                    P.copy(oT[:, 2:4, :], bk[:, 0:256].re("p (c t) -> p c t", c=2))
                    if oTf is not None:
                        P.copy(oTf[:, 2:4, :], bk[:, 0:256].re("p (c t) -> p c t", c=2))

                    if STOP == 'sgu':
                        continue
                    for c in range(6):
                        w = lambda k: fmp[:, FM_CW + c * 4 + k:FM_CW + c * 4 + k + 1]
                        P.ts(cacc[:, c, :], qkv_raw[:, c, 3:131], w(3), ALU.mult)
                        for k in (2, 1, 0):
                            P.stt(cacc[:, c, :], qkv_raw[:, c, k:k + 128], w(k), cacc[:, c, :], ALU.mult, ALU.add)
                    P.act(cacc.v, cacc.v, AF.Silu)
                    qkv = cacc
                    P.copy(qkv_raw[:, :, 0:3], qkv_raw[:, :, 128:131])
                    P.tt(sq4.v, qkv[:, 0:4, :], qkv[:, 0:4, :], ALU.mult)
                    bk = nb()
                    for c in range(4):
                        P.mm(bk[:, c * 128:(c + 1) * 128], BDm, sq4[:, c, :])
                    rsqrt(sq4.v.re("p c t -> p (c t)"), bk[:, :], 1e-6)
                    P.stt(qk[:, 0:2, :], qkv[:, 0:2, :], 0.125, sq4[:, 0:2, :], ALU.mult, ALU.mult)
                    P.tt(qk[:, 2:4, :], qkv[:, 2:4, :], sq4[:, 2:4, :], ALU.mult)
                    bk = nb()
                    P.tr(bk[:, 0:128], qk[:, 2, :], ident)
                    P.tr(bk[:, 128:256], qk[:, 3, :], ident)
                    P.tr(bk[:, 256:384], qkv[:, 4, :], ident)
                    P.tr(bk[:, 384:512], qkv[:, 5, :], ident)
                    P.copy(kv_tm.v, bk[:, :], eng="act")
                    if STOP == 'g1':
                        continue
                    P.tt(t4.v, p_tm[:, C_A:C_A + 4], bcs[:, BS_DT:BS_DT + 4], ALU.add)
                    P.act(t4.v, t4.v, AF.Exp)
                    P.act(t4.v, t4.v, AF.Ln, bias=1.0)
                    P.tt(g4.v, t4.v, nexpA.v, ALU.mult)
                    P.act(beta4.v, p_tm[:, C_B:C_B + 4], AF.Sigmoid)
                    for c in range(2):
                        P.ts(gm[:, c, :], g4.v, CHI[:, c:c + 1], ALU.mult)
                    bk = nb()
                    P.mm(bk[:, 0:4], UBLK, g4.v)
                    P.mm(bk[:, 4:12], ONES, gm.v.re("p c h -> p (c h)"))
                    P.copy(gc.v, bk[:, 0:4])
                    P.copy(glr.v, bk[:, 4:12])
                    P.ts(ngc.v, gc.v, -1.0, ALU.mult)
                    P.ts(glt.v, glr[:, 0:4], CHI[:, 0:1], ALU.mult)
                    P.stt(glt.v, glr[:, 4:8], CHI[:, 1:2], glt.v, ALU.mult, ALU.add)
                    P.act(egc.v, gc.v, AF.Exp)
                    P.tt(t4.v, glt.v, gc.v, ALU.subtract)
                    P.act(ktf.v, t4.v, AF.Exp)
                    bkc_ = nb()
                    P.mm(bkc_[0:64, 0:4], ONES[:, 0:64], gm.v[:, :, 0::2])
                    P.mm(bkc_[64:128, 0:4], ONES[:, 0:64], gm.v[:, :, 1::2])
                    P.act(cdp.v, bkc_[:, 0:4], AF.Exp)
                    P.tt(bg.v, beta4.v, egc.v, ALU.mult)
                    if STOP == 'g2':
                        continue
                    G4 = sq4
                    for h in range(4):
                        P.ts(G4[:, h, :], UBLK, g4[:, h:h + 1], ALU.mult)
                    if STOP == 'x1':
                        continue
                    bk = nb()
                    P.mm(bk[:, :], ONES, G4.v.re("p h t -> p (h t)"))
                    if STOP == 'x2':
                        continue
                    P.copy(gcrow.v.re("p h t -> p (h t)"), bk[:, :])
                    if STOP == 'x3':
                        continue
                    P.act(egcrow.v.re("p h t -> p (h t)"), bk[:, :], AF.Exp)
                    if STOP == 'g2a':
                        continue
                    for h in range(4):
                        j, hp = h // 2, 64 * (h % 2)
                        P.tt(qd3[hp:hp + 64, j, :], qk[hp:hp + 64, j, :], egcrow[hp:hp + 64, h, :], ALU.mult)
                    if STOP == 'g2b':
                        continue
                    P.tt(h4(vb.v), h4(kv_tm[:, 256:512]), bc4(beta4.v), ALU.mult)
                    P.tt(h4(kbg.v), h4(kv_tm[:, 0:256]), bc4(bg.v), ALU.mult)
                    P.tt(h4(ktail.v), h4(kv_tm[:, 0:256]), bc4(ktf.v), ALU.mult)
                    if STOP == 'g3':
                        continue
                    for h in range(4):
                        j, hp = h // 2, 64 * (h % 2)
                        hm = HM[h]
                        dd = DD[h % 2]
                        kTh = qk[hp:hp + 64, 2 + j, :]
                        qTh = qk[hp:hp + 64, j, :]
                        bk = nb()
                        P.mm(bk[:, 0:128], kTh, kTh)
                        P.mm(bk[:, 128:256], kTh, qTh)
                        P.stt(dd["tmp"].v, gcrow[:, h, :], -1.0, NEGL, ALU.mult, ALU.add)
                        P.act(dd["D"].v, dd["tmp"].v, AF.Exp, bias=gc[:, h:h + 1])
                        P.tt(dd["tmp"].v, gcrow[:, h, :], NEGU, ALU.add)
                        P.act(dd["DT"].v, dd["tmp"].v, AF.Exp, bias=ngc[:, h:h + 1])
                        P.stt(dd["D"].v, bk[:, 0:128], beta4[:, h:h + 1], dd["D"].v, ALU.mult, ALU.mult)
                        P.tt(hm["PQ"][:, 0, :], dd["D"].v, NSL, ALU.mult)
                        P.tt(hm["M1"].v, bk[:, 128:256], dd["DT"].v, ALU.mult)
                        bk2 = nb()
                        P.tr(bk2[:, 0:128], hm["PQ"][:, 0, :], ident)
                        P.copy(hm["PQ"][:, 1, :], bk2[:, 0:128], eng="act")
                        P.tt(hm["R"].v, bk2[:, 0:128], ident, ALU.add)
                    if STOP == 'g4':
                        continue
                    neumann()
                    if STOP == 'g5':
                        continue
                    bku = nb()
                    bkw = nb()
                    for h in range(4):
                        j, hp = h // 2, 64 * (h % 2)
                        TinvT = HM[h]["R"].v
                        P.mm(bku[:, h * 64:(h + 1) * 64], TinvT, vb[:, h * 64:(h + 1) * 64])
                        P.mm(bkw[hp:hp + 64, j * 128:(j + 1) * 128], kbg[:, h * 64:(h + 1) * 64], TinvT)
                    P.copy(u_sb.v, bku[:, 0:256], eng="act")
                    P.copy(wT_sb.v, bkw[:, 0:256])
                    if STOP == 'g6':
                        continue
                    bko = nb()
                    for c in range(2):
                        cs = slice(64 * c, 64 * c + 64)
                        bkv = nb()
                        for j in range(2):
                            P.mm(bkv[cs, j * 128:(j + 1) * 128], wT3[:, j, cs], Sg[:, j, :])
                        P.tt(vnew[cs, :], u_sb[cs, :], bkv[cs, 0:256], ALU.subtract)
                        bks = nb()
                        for j in range(2):
                            pc = slice(j * 128, (j + 1) * 128)
                            P.mm(bko[cs, pc], qd3[:, j, cs], Sg[:, j, :], start=True, stop=False)
                            for h_ in range(2):
                                h = 2 * j + h_
                                hc = slice(h * 64, (h + 1) * 64)
                                P.mm(bko[cs, hc], HM[h]["M1"][:, cs], vnew[:, hc], start=False, stop=(h_ == 1))
                        for j in range(2):
                            pc = slice(j * 128, (j + 1) * 128)
                            P.mm(bks[:, pc], ktail[cs, pc], vnew[cs, pc])
                        for j in range(2):
                            pc = slice(j * 128, (j + 1) * 128)
                            P.tt(stmp.v, bks[:, pc], BDm, ALU.mult)
                            P.stt(Sg[:, j, :], Sg[:, j, :], cdp[:, c * 2 + j:c * 2 + j + 1], stmp.v, ALU.mult, ALU.add)
                    P.copy(o_sb.v, bko[:, 0:256], eng="act")
                    P.tt(o_sq.v, o_sb.v, o_sb.v, ALU.mult)
                    P.reduce(s1[:, 4:8], h4(o_sq.v), ALU.add)
                    rsqrt(t4.v, s1[:, 4:8], 1e-6, 1.0 / 64)
                    P.tt(h4(o_sb.v), h4(o_sb.v), bc4(t4.v), ALU.mult)
                    P.tt(o_sb.v, o_sb.v, bcs[:, BS_GN:BS_GN + 256], ALU.mult)
                    P.act(sz.v, p_tm[:, C_Z:C_Z + 256], AF.Silu)
                    P.tt(o_sb.v, o_sb.v, sz.v, ALU.mult)
                    bk = nb()
                    for c in range(2):
                        P.tr(bk[:, c * 128:(c + 1) * 128], o_sb[:, c * 128:(c + 1) * 128], ident)
                    P.copy(oT[:, 0:2, :], bk[:, 0:256].re("p (c t) -> p c t", c=2))
                    if oTf is not None:
                        P.copy(oTf[:, 0:2, :], bk[:, 0:256].re("p (c t) -> p c t", c=2))

                    if STOP == 'gdn':
                        continue
                    P.tt(rw.v, rw_raw[:, :, 0:128], rw_raw[:, :, 1:129], ALU.subtract)
                    for c in range(8):
                        P.stt(rw[:, c, :], rw[:, c, :], fmp[:, FM_MU + c:FM_MU + c + 1], rw_raw[:, c, 1:129],
                              ALU.mult, ALU.add)
                    P.copy(rw_raw[:, :, 0:1], rw_raw[:, :, 128:129])
                    P.act(lact[0:64, 0, :], rw[0:64, 6, :], AF.Tanh)
                    P.copy(lact[64:128, 0, :], rw[64:128, 6, :])
                    P.act(lact[:, 1, :], rw[:, 7, :], AF.Sigmoid)
                    bk = nb()
                    bkg = nb()
                    for c in range(2):
                        ch = slice(c * 128, (c + 1) * 128)
                        P.mm(bk[:, c * 128:(c + 1) * 128], lwW[:, ch], lact[:, 0, :])
                        P.mm(bk[:, 256 + c * 128:256 + (c + 1) * 128], lwA[:, ch], lact[:, 0, :])
                        P.mm(bkg[:, c * 128:(c + 1) * 128], gup[:, ch], lact[:, 1, :])
                    for c in range(2):
                        P.act(lz[:, c, :], bk[:, c * 128:(c + 1) * 128], AF.Sigmoid,
                              bias=fmp[:, FM_W0 + c:FM_W0 + c + 1])
                        P.act(asg[:, c, :], bk[:, 256 + c * 128:256 + (c + 1) * 128], AF.Sigmoid,
                              bias=fmp[:, FM_A0 + c:FM_A0 + c + 1])
                    P.ts(lz, lz, -0.6065306597126334, ALU.mult)
                    P.copy(gateT, bkg[:, 0:256].re("p (c t) -> p c t", c=2), eng="act")
                    for c in range(2):
                        P.ts(kkT[:, c, :], rw[:, 2 + c, :], fmp[:, FM_KK + c:FM_KK + c + 1], ALU.mult)
                    P.tt(sq4[:, 0:2, :], kkT, kkT, ALU.mult)
                    bk = nb()
                    for c in range(2):
                        P.mm(bk[:, c * 128:(c + 1) * 128], BDm, sq4[:, c, :])
                    rsqrt(sq4[:, 0:2, :], bk[:, 0:256].re("p (c t) -> p c t", c=2), 1e-12)
                    P.tt(kkT, kkT, sq4[:, 0:2, :], ALU.mult)
                    for c in range(2):
                        P.ts(kmT[:, c, :], asg[:, c, :], -1.0, ALU.add, fmp[:, FM_KA + c:FM_KA + c + 1], ALU.mult)
                    P.stt(kmT, kmT, 1.0, rw[:, 2:4, :], ALU.add, ALU.mult)
                    for c in range(2):
                        for cc in range(2):
                            P.scan(cum[:, c, cc * 64:(cc + 1) * 64], ONES[:, 0:64], lz[:, c, cc * 64:(cc + 1) * 64],
                                   0.0, ALU.mult, ALU.add)
                    P.tt(eWp, cum, lz, ALU.subtract)
                    P.act(eWp, eWp, AF.Exp)
                    P.act(eWi, cum, AF.Exp, scale=-1.0)
                    P.act(eW, cum, AF.Exp)
                    P.stt(AtT, kkT, -1.0, eWp, ALU.mult, ALU.mult)
                    P.tt(BtT, kkT, asg, ALU.mult)
                    P.tt(BtT, BtT, eWi, ALU.mult)
                    P.tt(KtT, kmT, eWi, ALU.mult)
                    P.tt(RtT, rw[:, 0:2, :], eW, ALU.mult)
                    for (q_, src) in ((0, BtT), (1, KtT), (2, rw[:, 4:6, :])):
                        bk = nb()
                        for c in range(2):
                            P.tr(bk[:, c * 128:(c + 1) * 128], src[:, c, :], ident)
                        P.copy(bkv_tm[:, q_, :], bk[:, 0:256], eng="act")
                    for h in range(4):
                        j, hp = h // 2, 64 * (h % 2)
                        hm = HM[h]
                        hs_ = slice(hp, hp + 64)
                        bk = nb()
                        P.mm(bk[:, 0:128], AtT[hs_, j, :], BtT[hs_, j, :])
                        P.mm(bk[:, 128:256], BtT[hs_, j, :], AtT[hs_, j, :])
                        P.mm(bk[:, 256:384], KtT[hs_, j, :], AtT[hs_, j, :])
                        bk2 = nb()
                        P.mm(bk2[:, 0:128], BtT[hs_, j, :], RtT[hs_, j, :])
                        P.mm(bk2[:, 128:256], KtT[hs_, j, :], RtT[hs_, j, :])
                        P.tt(hm["PQ"][:, 0, :], bk[:, 0:128], SLm, ALU.mult)
                        P.tt(hm["PQ"][:, 1, :], bk[:, 128:256], SUm, ALU.mult)
                        P.tt(hm["R"].v, hm["PQ"][:, 1, :], ident, ALU.add)
                        P.tt(hm["M1"].v, bk[:, 256:384], SUm, ALU.mult)
                        P.tt(hm["M2"].v, bk2[:, 0:128], IUm, ALU.mult)
                        P.tt(hm["M3"].v, bk2[:, 128:256], IUm, ALU.mult)
                    neumann()
                    bky = nb()
                    for c in range(2):
                        cs = slice(64 * c, 64 * c + 64)
                        last = 64 * c + 63
                        bkz = nb()
                        for j in range(2):
                            pc = slice(j * 128, (j + 1) * 128)
                            P.mm(bkz[cs, pc], AtT[:, j, cs], Sr[:, j, :], start=True, stop=False)
                            for h_ in range(2):
                                h = 2 * j + h_
                                hc = slice(h * 64, (h + 1) * 64)
                                P.mm(bkz[cs, hc], HM[h]["M1"][:, cs], bkv_tm[:, 2, hc], start=False, stop=(h_ == 1))
                        P.copy(Z_sb[cs, :], bkz[cs, 0:256])
                        bku_ = nb()
                        for h in range(4):
                            hc = slice(h * 64, (h + 1) * 64)
                            P.mm(bku_[cs, hc], HM[h]["R"][:, cs], Z_sb[:, hc])
                        P.copy(U_sb[cs, :], bku_[cs, 0:256], eng="act")
                        bks = nb()
                        for j in range(2):
                            pc = slice(j * 128, (j + 1) * 128)
                            P.mm(bky[cs, pc], RtT[:, j, cs], Sr[:, j, :], start=True, stop=False)
                            for h_ in range(2):
                                h = 2 * j + h_
                                hc = slice(h * 64, (h + 1) * 64)
                                P.mm(bky[cs, hc], HM[h]["M2"][:, cs], U_sb[:, hc], start=False, stop=False)
                                P.mm(bky[cs, hc], HM[h]["M3"][:, cs], bkv_tm[:, 2, hc], start=False, stop=(h_ == 1))
                        for j in range(2):
                            pc = slice(j * 128, (j + 1) * 128)
                            P.mm(bks[:, pc], bkv_tm[cs, 0, pc], U_sb[cs, pc], start=True, stop=False)
                            P.mm(bks[:, pc], bkv_tm[cs, 1, pc], bkv_tm[cs, 2, pc], start=False, stop=True)
                        for j in range(2):
                            pc = slice(j * 128, (j + 1) * 128)
                            P.tt(stmp.v, bks[:, pc], Sr[:, j, :], ALU.add)
                            P.stt(Sr[:, j, :], stmp.v, eW[:, j, last:last + 1], BDm, ALU.mult, ALU.mult)
                    P.copy(y_sb.v, bky[:, 0:256], eng="act")
                    P.reduce(s1[:, 4:8], h4(y_sb.v), ALU.add)
                    P.ts(t4.v, s1[:, 4:8], -1.0 / 64, ALU.mult)
                    P.tt(h4(y_sb.v), h4(y_sb.v), bc4(t4.v), ALU.add)
                    P.tt(y_sq.v, y_sb.v, y_sb.v, ALU.mult)
                    P.reduce(s1[:, 4:8], h4(y_sq.v), ALU.add)
                    rsqrt(t4.v, s1[:, 4:8], 64e-5, 1.0 / 64)
                    P.tt(h4(y_sb.v), h4(y_sb.v), bc4(t4.v), ALU.mult)
                    bk = nb()
                    for c in range(2):
                        P.tr(bk[:, c * 128:(c + 1) * 128], y_sb[:, c * 128:(c + 1) * 128], ident)
                    for c in range(2):
                        P.ts(ynT[:, c, :], bk[:, c * 128:(c + 1) * 128], fmp[:, FM_GNG + c:FM_GNG + c + 1], ALU.mult,
                             fmp[:, FM_GNB + c:FM_GNB + c + 1], ALU.add)
                        P.stt(rkb[:, c, :], rw[:, c, :], fmp[:, FM_RK + c:FM_RK + c + 1], kmT[:, c, :], ALU.mult, ALU.mult)
                    bk = nb()
                    for c in range(2):
                        P.mm(bk[:, c * 128:(c + 1) * 128], BDm, rkb[:, c, :])
                    P.tt(rkb, bk[:, 0:256].re("p (c t) -> p c t", c=2), rw[:, 4:6, :], ALU.mult)
                    P.tt(ynT, ynT, rkb, ALU.add)
                    P.tt(oT[:, 6:8, :], ynT, gateT, ALU.mult)
                    if oTf is not None:
                        P.tt(oTf[:, 6:8, :], ynT, gateT, ALU.mult)
                        tap("oT", lambda hd: hd[i], oTf.v.re("p c t -> p (c t)"))

                    if STOP == 'rwkv':
                        continue
                    for half in range(2):
                        bk = nb()
                        for c in range(8):
                            P.mm(bk[:, :], oT[:, c, :], w_out[:, c, half * 512:(half + 1) * 512],
                                 start=(c == 0), stop=(c == 7))
                        hsl = slice(half * 512, (half + 1) * 512)
                        P.tt(hh[:, hsl], bk[:, :], modb[:, MB_GTM, hsl], ALU.mult)
                        P.tt(xt[:, hsl], xt[:, hsl], hh[:, hsl], ALU.add)
                    P.dma("sp", xres[tsl, :], xt.v)
                    if l == 0:
                        tap("x1", lambda hd: hd[tsl, :], xt.v)

                    if STOP == 'wout':
                        continue
                    rstd_of(s1[:, 1:2], xt.v, 1e-6)
                    P.stt(hh.v, xt.v, s1[:, 1:2], modb[:, MB_G2, :], ALU.mult, ALU.mult)
                    P.tt(hh.v, hh.v, modb[:, MB_SHF, :], ALU.add)
                    P.dma("sp", hf[tsl, :], hh.v)
                    if l == 0:
                        tap("h2", lambda hd: hd[tsl, :], hh.v)
                    for half in range(2):
                        bk = nb()
                        for c4 in range(4):
                            c = half * 4 + c4
                            P.tr(bk[:, c4 * 128:(c4 + 1) * 128], hh[:, c * 128:(c + 1) * 128], ident)
                        P.copy(h2T[:, half * 4:(half + 1) * 4, :], bk[:, :].re("p (c t) -> p c t", c=4), eng="act")
                    bk = nb()
                    for k in range(8):
                        P.mm(bk[:, 0:72], h2T[:, k, :], wr[:, k, :], start=(k == 0), stop=(k == 7))
                    P.tt(lg.v, bk[:, 0:72], bcs[:, BS_RB:BS_RB + 72], ALU.add)
                    P.reduce(sc[:, 0:1], lg[:, 0:8], ALU.max)
                    P.ts(ohg.v, lg[:, 0:8], sc[:, 0:1], ALU.is_equal)
                    P.ts(sc[:, 1:2], sc[:, 0:1], -1.0, ALU.mult)
                    P.act(sel.v, lg[:, 0:8], AF.Exp, bias=sc[:, 1:2], accum=sc[:, 2:3])
                    recip(sc[:, 2:3], sc[:, 2:3])
                    P.tt(r8.v, lg[:, 8:72].re("p (g j) -> p g j", g=8), ohg.v.unsq(2).bc([128, 8, 8]), ALU.mult)
                    P.reduce(sel.v, r8.v.re("p g j -> p j g"), ALU.add)
                    P.reduce(sc[:, 3:4], sel.v, ALU.max)
                    P.ts(oh1.v, sel.v, sc[:, 3:4], ALU.is_equal)
                    P.stt(sel.v, oh1.v, -1.0e30, sel.v, ALU.mult, ALU.add)
                    P.reduce(sc[:, 4:5], sel.v, ALU.max)
                    P.ts(oh2.v, sel.v, sc[:, 4:5], ALU.is_equal)
                    P.tt(sc[:, 5:6], sc[:, 3:4], sc[:, 4:5], ALU.subtract)
                    P.act(sc[:, 5:6], sc[:, 5:6], AF.Sigmoid)
                    P.tt(gate[:, i, 0:1], sc[:, 5:6], sc[:, 2:3], ALU.mult)
                    P.tt(gate[:, i, 1:2], sc[:, 2:3], gate[:, i, 0:1], ALU.subtract)
                    for (mm_, oh) in ((Mx[0], oh1), (Mx[1], oh2)):
                        P.tt(mm_.v.re("p (g j) -> p g j", g=8), ohg.v.unsq(2).bc([128, 8, 8]),
                             oh.v.unsq(1).bc([128, 8, 8]), ALU.mult)
                    P.tt(Mx[2].v, Mx[0].v, Mx[1].v, ALU.add)
                    bk = nb()
                    P.mm(bk[:, 0:64], SU128, Mx[2].v)
                    P.mm(bk[:, 64:128], ONES, Mx[2].v)
                    P.tt(rke.v, bk[:, 0:64], base.v, ALU.add)
                    P.tt(base.v, base.v, bk[:, 64:128], ALU.add)
                    for cix in range(2):
                        P.tt(rt.v, Mx[cix].v, rke.v, ALU.mult)
                        P.reduce(rank[:, i, cix:cix + 1], rt.v, ALU.add)
                        P.tt(rt.v, Mx[cix].v, IOTA64, ALU.mult)
                        P.reduce(eidx[:, i, cix:cix + 1], rt.v, ALU.add)
                    if "route" in tap_d and l == 0:
                        P.copy(lg[:, 0:2], eidx[:, i, :])
                        P.copy(lg[:, 2:4], gate[:, i, :])
                        P.copy(lg[:, 4:6], rank[:, i, :])
                        tap("route", lambda hd: hd[tsl, :], lg[:, 0:6])

            if STOP in ('proj', 'sgu', 'gdn', 'rwkv', 'wout', 'A', 'x1', 'x2', 'x3', 'g1', 'g2', 'g2a', 'g2b', 'g3', 'g4', 'g5', 'g6', 'g7'):
                return
            with P.scope():
                padded = P.sb("padded", [128, 64])
                pend = P.sb("pend", [128, 64])
                pstart = P.sb("pstart", [128, 64])
                tm_ = P.sb("tm_", [128, 64])
                blke = P.sb("blke", [128, NBLK])
                dtmp = P.sb("dtmp", [128, NT * 2])
                padi = P.sb("padi", [128, 64], I32)
                P.ts(padded.v, base.v, 127.0, ALU.add)
                P.copy(padi.v, padded.v)
                P.ts(padi.v, padi.v, 7, ALU.arith_shift_right, 7, ALU.logical_shift_left)
                P.copy(padded.v, padi.v)
                P.scan(pend.v, ONES[:, 0:64], padded.v, 0.0, ALU.mult, ALU.add)
                P.tt(pstart.v, pend.v, padded.v, ALU.subtract)
                P.memset(blke.v, 0.0)
                P.copy(dest_f.v, rank.v)
                ef = eidx.v.re("p t c -> p (t c)")
                df = dest_f.v.re("p t c -> p (t c)")
                for e_ in range(NEXP):
                    P.stt(blke.v, IOTAB, pend[:, e_:e_ + 1], blke.v, ALU.is_ge, ALU.add)
                    P.stt(dtmp.v, ef, float(e_), pstart[:, e_:e_ + 1].bc([128, NT * 2]), ALU.is_equal, ALU.mult)
                    P.tt(df, df, dtmp.v, ALU.add)
                P.ts(blke.v, blke.v, 63.0, ALU.min)
                P.ts(blke.v, blke.v, float(l * NEXP), ALU.add, 128.0, ALU.mult)
                P.ts(blke.v, blke.v, PIDX[:, 0:1], ALU.add)
                P.copy(idxb.v, blke.v)
                P.copy(dest_i.v, dest_f.v)
                P.copy(gtf_prev.v, modb[:, MB_GTF, :])

            if STOP == 'fin':
                return
            with P.scope():
                hbuf = [P.sb("hbuf%d" % i, [128, D]) for i in range(2)]
                for i in range(NT):
                    hb = hbuf[i % 2]
                    P.dma("sp", hb.v, hf[i * 128:(i + 1) * 128, :])
                    P.scatter(hs.v, hb.v, dest_i[:, i, 0:1])
                    P.scatter(hs.v, hb.v, dest_i[:, i, 1:2])

            if STOP == 'scatter':
                return
            with P.scope():
                wgs = [P.sb("wgs%d" % i, [128, 8, 256], BF16) for i in range(2)]
                wus = [P.sb("wus%d" % i, [128, 8, 256], BF16) for i in range(2)]
                wds = [P.sb("wds%d" % i, [128, 2, D], BF16) for i in range(2)]
                xbs = [P.sb("xbs%d" % i, [128, D], BF16) for i in range(2)]
                xbT = P.sb("xbT", [128, 8, 128], BF16)
                sg = P.sb("sg", [128, 256])
                hidT = P.sb("hidT", [128, 2, 128], BF16)
                yo = [P.sb("yo%d" % i, [128, D]) for i in range(2)]
                for b in range(NBLK):
                    wg_, wu_, wd_, xb, yob = wgs[b % 2], wus[b % 2], wds[b % 2], xbs[b % 2], yo[b % 2]
                    P.dma("sp", xb.v, hs[b * 128:(b + 1) * 128, :])
                    ix = idxb[:, b:b + 1]
                    P.gather(wg_.v.re("p k f -> p (k f)"), ewg_d.v, ix)
                    P.gather(wu_.v.re("p k f -> p (k f)"), ewu_d.v, ix)
                    P.gather(wd_.v.re("p j d -> p (j d)"), ewd_d.v, ix)
                    bk = nb()
                    bkb = bk.v.bitcast(BF16)
                    xbv = xb.v.re("p (q k) -> p k q", k=8)
                    for k in range(8):
                        P.tr(bkb[:, k * 128:(k + 1) * 128], xbv[:, k, :], identb.v)
                    P.copy(xbT.v.re("p k t -> p (k t)"), bkb[:, 0:1024], eng="act")
                    bkg_ = nb()
                    bku_ = nb()
                    for (bk_, w_) in ((bkg_, wg_), (bku_, wu_)):
                        wv = w_.v.re("p k (q j) -> p k j q", j=2)
                        for j in range(2):
                            for k in range(8):
                                P.mm(bk_[:, j * 128:(j + 1) * 128], wv[:, k, j, :], xbT[:, k, :],
                                     start=(k == 0), stop=(k == 7))
                    P.act(sg.v, bkg_[:, 0:256], AF.Silu)
                    P.tt(hidT.v.re("p j t -> p (j t)"), sg.v, bku_[:, 0:256], ALU.mult)
                    for half in range(2):
                        bk = nb()
                        for j in range(2):
                            P.mm(bk[:, :], hidT[:, j, :], wd_[:, j, half * 512:(half + 1) * 512],
                                 start=(j == 0), stop=(j == 1))
                        P.copy(yob[:, half * 512:(half + 1) * 512], bk[:, :], eng=("act" if half else "dve"))
                    P.dma("sp", ys[b * 128:(b + 1) * 128, :], yob.v)

        if STOP == 'B':
            return
        with P.scope():
            fing = P.sb("fing", [128, D])
            P.dma("sp", fing.v, fing_d.v.pbc(128))
            xf = [P.sb("xf%d" % i, [128, D]) for i in range(2)]
            yf = [P.sb("yf%d" % i, [128, D]) for i in range(2)]
            zf = [P.sb("zf%d" % i, [128, D]) for i in range(2)]
            sf = P.sb("sf", [128, 4])
            for i in range(NT):
                tsl = slice(i * 128, (i + 1) * 128)
                xt, ya, yb = xf[i % 2], yf[i % 2], zf[i % 2]
                P.dma("sp", xt.v, xres[tsl, :])
                P.gather(ya.v, ys.v, dest_i[:, i, 0:1])
                P.gather(yb.v, ys.v, dest_i[:, i, 1:2])
                P.ts(ya.v, ya.v, gate[:, i, 0:1], ALU.mult)
                P.stt(ya.v, yb.v, gate[:, i, 1:2], ya.v, ALU.mult, ALU.add)
                P.tt(ya.v, ya.v, gtf_prev.v, ALU.mult)
                P.tt(xt.v, xt.v, ya.v, ALU.add)
                if L == 1:
                    tap("x2", lambda hd: hd[tsl, :], xt.v)
                P.act(yb.v, xt.v, AF.Square, accum=sf[:, 0:1])
                rsqrt(sf[:, 1:2], sf[:, 0:1], 1e-6, 1.0 / D)
                P.stt(xt.v, xt.v, sf[:, 1:2], fing.v, ALU.mult, ALU.mult)
                P.dma("sp", out_d[tsl, :], xt.v)

    P.run(body)
    return nc, cvals, P


def prep_inputs(inp, SEQ, DEPTH, cvals):
    f = lambda a: np.ascontiguousarray(np.asarray(a, dtype=np.float32))
    L = DEPTH
    H, N, G = 4, 64, 256

    def fm(v, nch):
        return np.asarray(v).reshape(L, nch, 128).transpose(0, 2, 1)

    fmp = np.concatenate([
        fm(inp["rw_mu"][:L], 8), fm(inp["rw_w0"][:L], 2), fm(inp["rw_a0"][:L], 2), fm(inp["rw_k_k"][:L], 2),
        fm(inp["rw_k_a"][:L], 2), fm(np.asarray(inp["rw_r_k"][:L]).reshape(L, G), 2), fm(inp["rw_gn_g"][:L], 2),
        fm(inp["rw_gn_b"][:L], 2),
        np.asarray(inp["gdn_conv_w"][:L]).reshape(L, 4, 6, 128).transpose(0, 3, 2, 1).reshape(L, 128, 24),
        np.asarray(inp["sc_conv_w"][:L]).reshape(L, 3, 2, 128).transpose(0, 3, 2, 1).reshape(L, 128, 6),
    ], axis=2)
    assert fmp.shape[2] == NFM
    bcs = np.concatenate([
        inp["sgu_ln_g"][:L], inp["sgu_ln_b"][:L], np.tile(np.asarray(inp["gdn_norm_g"][:L]), (1, 4)),
        inp["gdn_a_log"][:L], inp["gdn_dt_bias"][:L], inp["moe_b_group"][:L], inp["moe_b_router"][:L]], axis=1)
    assert bcs.shape[1] == NBS
    shared = {
        "ada_w": f(inp["ada_w"][:L]), "ada_b": f(inp["ada_b"][:L]),
        "g12": f(np.concatenate([inp["mix_norm_g"][:L], inp["ffn_norm_g"][:L]], axis=1)),
        "w_in": f(inp["w_in"][:L]), "w_out": f(inp["w_out"][:L]),
        "fmp": f(fmp), "bcs": f(bcs),
        "sgu_wT": f(np.asarray(inp["sgu_w"][:L]).transpose(0, 1, 3, 2)),
        "sgu_bT": f(np.asarray(inp["sgu_b"][:L]).transpose(0, 2, 1)),
        "lora_wa": f(np.concatenate([inp["rw_w_up"][:L], inp["rw_a_up"][:L]], axis=1)),
        "g_up": f(inp["rw_g_up"][:L]),
        "wr": f(np.concatenate([inp["moe_w_group"][:L], inp["moe_w_router"][:L]], axis=2)),
        "ewg": f(np.asarray(inp["moe_w_gate"][:L]).reshape(L * NEXP * 128, 2048)),
        "ewu": f(np.asarray(inp["moe_w_up"][:L]).reshape(L * NEXP * 128, 2048)),
        "ewd": f(np.asarray(inp["moe_w_down"][:L]).reshape(L * NEXP * 128, 2048)),
        "fin_g": f(inp["final_norm_g"]),
        "consts": cvals,
    }
    x = np.asarray(inp["x"])
    c = np.asarray(inp["c"])
    per_core = []
    for b in range(x.shape[0]):
        d = dict(shared)
        d["x"] = f(x[b, :SEQ])
        d["cfm"] = f(c[b].reshape(8, 128).T)
        per_core.append(d)
    return per_core


_CACHE = {}


def kernel(**inputs):
    SEQ, DEPTH, B = 8192, 4, 8
    if "nc" not in _CACHE:
        _CACHE["nc"] = build(SEQ, DEPTH)
    nc, cvals, _ = _CACHE["nc"]
    in_maps = prep_inputs(inputs, SEQ, DEPTH, cvals)
    res = run_bass_kernel_spmd(nc, in_maps, core_ids=list(range(B)))
    return np.stack([np.asarray(r["out"], dtype=np.float32) for r in res.results], axis=0)
```

```python
import contextlib
import numpy as np
import concourse.bass as bass
import concourse.mybir as mybir

F32 = mybir.dt.float32
BF16 = mybir.dt.bfloat16
I32 = mybir.dt.int32
F32R = mybir.dt.float32r
AF = mybir.ActivationFunctionType
ALU = mybir.AluOpType
AX = mybir.AxisListType


class V:
    __slots__ = ("t", "ap")

    def __init__(self, t, ap):
        self.t = t
        self.ap = ap

    def __getitem__(self, k):
        return V(self.t, self.ap[k])

    def bc(self, shape):
        return V(self.t, self.ap.to_broadcast(list(shape)))

    def re(self, pat, **kw):
        return V(self.t, self.ap.rearrange(pat, **kw))

    def bitcast(self, dt):
        return V(self.t, self.ap.bitcast(dt))

    def pbc(self, n):
        return V(self.t, self.ap.partition_broadcast(n))

    def unsq(self, ax):
        return V(self.t, self.ap.unsqueeze(ax))


class T:
    def __init__(self, h, name):
        self.h = h
        self.name = name
        self.w = None
        self.r = {}
        self.excl = False

    def __getitem__(self, k):
        return V(self, self.h[k])

    @property
    def v(self):
        return V(self, self.h[:])


def _ap(x):
    return x.ap if isinstance(x, V) else x


def _ts(*xs):
    out = []
    for x in xs:
        if isinstance(x, V) and x.t not in out:
            out.append(x.t)
    return out


class Prog:
    ENG = ("pe", "act", "dve", "pool", "sp")

    def __init__(self, nc):
        self.nc = nc
        self.ops = {e: [] for e in self.ENG}
        self.cnt = {}
        self.seen = {e: {} for e in self.ENG}
        self.nops = 0
        self._stacks = []
        self._dslot = {}

    @contextlib.contextmanager
    def scope(self):
        st = contextlib.ExitStack()
        self._stacks.append(st)
        try:
            with st:
                yield
                self.barrier()
        finally:
            self._stacks.pop()

    def _uniq(self, name):
        self._uid = getattr(self, "_uid", 0) + 1
        return "%s_u%d" % (name, self._uid)

    def sb(self, name, shape, dt=F32):
        name = self._uniq(name)
        h = self._stacks[-1].enter_context(self.nc.sbuf_tensor(name, list(shape), dt))
        return T(h, name)

    def ps(self, name, shape, dt=F32):
        name = self._uniq(name)
        h = self._stacks[-1].enter_context(self.nc.psum_tensor(name, list(shape), dt))
        t = T(h, name)
        t.excl = True
        return t

    def dram(self, name, shape, dt=F32, kind="Internal"):
        name = self._uniq(name)
        return T(self.nc.dram_tensor(name, list(shape), dt, kind=kind), name)

    def _emit(self, eng, key, inc, fn, reads, writes):
        waits = {}

        def need(dep):
            if dep is None:
                return
            k, c = dep
            if k == eng and eng == "pe":
                return
            if self.seen[eng].get(k, 0) >= c:
                return
            if waits.get(k, 0) < c:
                waits[k] = c

        if key != eng and self.cnt.get(key, 0) > 0:
            need((key, self.cnt[key]))
        for b in reads:
            need(b.w)
            if b.excl:
                for k, c in b.r.items():
                    if k != key:
                        need((k, c))
        for b in writes:
            need(b.w)
            for k, c in b.r.items():
                need((k, c))
        for k, c in waits.items():
            self.seen[eng][k] = c
        self.cnt[key] = self.cnt.get(key, 0) + 1
        my = self.cnt[key]
        self.ops[eng].append((fn, sorted(waits.items()), key, inc))
        for b in writes:
            b.w = (key, my)
            b.r = {}
        for b in reads:
            if b not in writes:
                b.r[key] = my
        self.nops += 1

    def barrier(self):
        snap = dict(self.cnt)
        for eng in self.ENG:
            waits = {}
            for k, c in snap.items():
                if self.seen[eng].get(k, 0) < c:
                    waits[k] = c
                    self.seen[eng][k] = c
            if waits:
                self.ops[eng].append((None, sorted(waits.items()), None, 0))

    def op(self, eng, fn, reads=(), writes=()):
        self._emit(eng, eng, 1, fn, list(reads), list(writes))

    NSLOT = 20

    def _dkey(self, q):
        n = self._dslot.get(q, 0)
        self._dslot[q] = n + 1
        return "dma_%s_%d" % (q, n % self.NSLOT)

    def dma(self, q, out, in_, extra_reads=(), **kw):
        key = self._dkey(q)
        o, i = _ap(out), _ap(in_)
        self._emit(q, key, 16, lambda e: e.dma_start(out=o, in_=i, **kw),
                   _ts(in_) + list(extra_reads), _ts(out))

    def gather(self, out, in_, idx, bound=None):
        o, i, ix = _ap(out), _ap(in_), _ap(idx)
        cache = self.__dict__.setdefault("_regcache", {})

        def fn(e):
            kw = {}
            if bound is not None:
                if bound not in cache:
                    cache[bound] = e.to_reg(bound)
                kw = dict(bounds_check=cache[bound], oob_is_err=False)
            return e.indirect_dma_start(out=o, out_offset=None, in_=i,
                                        in_offset=bass.IndirectOffsetOnAxis(ap=ix, axis=0), **kw)
        self._emit("pool", self._dkey("pool"), 16, fn,
                   _ts(in_, idx) + (_ts(out) if bound is not None else []), _ts(out))

    def scatter(self, out, in_, idx):
        o, i, ix = _ap(out), _ap(in_), _ap(idx)
        self._emit("pool", self._dkey("pool"), 16,
                   lambda e: e.indirect_dma_start(out=o, out_offset=bass.IndirectOffsetOnAxis(ap=ix, axis=0),
                                                  in_=i, in_offset=None),
                   _ts(in_, idx), _ts(out))

    def mm(self, out, lhsT, rhs, start=True, stop=True):
        o, l, r = _ap(out), _ap(lhsT), _ap(rhs)
        self.op("pe", lambda e: e.matmul(o, l, r, start=start, stop=stop),
                reads=_ts(lhsT, rhs), writes=_ts(out))

    def mmr(self, out, lhsT, rhs, start=True, stop=True):
        self.mm(out, lhsT.bitcast(F32R), rhs.bitcast(F32R), start=start, stop=stop)

    def tr(self, out, in_, ident):
        o, i, d = _ap(out), _ap(in_), _ap(ident)
        self.op("pe", lambda e: e.transpose(o, i, d), reads=_ts(in_, ident), writes=_ts(out))

    def tt(self, out, a, b, op, eng="dve"):
        o, x, y = _ap(out), _ap(a), _ap(b)
        self.op(eng, lambda e: e.tensor_tensor(out=o, in0=x, in1=y, op=op),
                reads=_ts(a, b), writes=_ts(out))

    def ts(self, out, a, s1, op0, s2=None, op1=None, eng="dve", accum=None):
        o, x, p1, p2, ac = _ap(out), _ap(a), _ap(s1), _ap(s2), _ap(accum)
        kw = {}
        if op1 is not None:
            kw["op1"] = op1
        if accum is not None:
            kw["accum_out"] = ac
        self.op(eng, lambda e: e.tensor_scalar(out=o, in0=x, scalar1=p1, scalar2=p2, op0=op0, **kw),
                reads=_ts(a, s1, s2), writes=_ts(out, accum))

    def stt(self, out, a, s, b, op0, op1, eng="dve"):
        o, x, p, y = _ap(out), _ap(a), _ap(s), _ap(b)
        self.op(eng, lambda e: e.scalar_tensor_tensor(out=o, in0=x, scalar=p, in1=y, op0=op0, op1=op1),
                reads=_ts(a, s, b), writes=_ts(out))

    def act(self, out, a, func, bias=None, scale=None, accum=None):
        o, x, b, s, ac = _ap(out), _ap(a), _ap(bias), _ap(scale), _ap(accum)
        kw = {}
        if bias is not None:
            kw["bias"] = b
        if scale is not None:
            kw["scale"] = s
        if accum is not None:
            kw["accum_out"] = ac
        self.op("act", lambda e: e.activation(out=o, in_=x, func=func, **kw),
                reads=_ts(a, bias, scale), writes=_ts(out, accum))

    def copy(self, out, a, eng="dve"):
        o, x = _ap(out), _ap(a)
        if eng == "act":
            self.op("act", lambda e: e.activation(out=o, in_=x, func=AF.Copy), reads=_ts(a), writes=_ts(out))
        else:
            self.op(eng, lambda e: e.tensor_copy(out=o, in_=x), reads=_ts(a), writes=_ts(out))

    def memset(self, out, val, eng="dve"):
        o = _ap(out)
        self.op(eng, lambda e: e.memset(o, val), writes=_ts(out))

    def reduce(self, out, a, op, axis=None, eng="dve"):
        o, x = _ap(out), _ap(a)
        ax = axis if axis is not None else AX.X
        self.op(eng, lambda e: e.tensor_reduce(out=o, in_=x, axis=ax, op=op), reads=_ts(a), writes=_ts(out))

    def scan(self, out, d0, d1, init, op0, op1):
        o, x, y, i = _ap(out), _ap(d0), _ap(d1), _ap(init)
        self.op("dve", lambda e: e.tensor_tensor_scan(out=o, data0=x, data1=y, initial=i, op0=op0, op1=op1),
                reads=_ts(d0, d1, init), writes=_ts(out))

    def run(self, body):
        nc = self.nc
        with contextlib.ExitStack() as st:
            self._stacks.append(st)
            body(self)
            self.barrier()
            keys = sorted(self.cnt.keys())
            sems = {k: st.enter_context(nc.semaphore("s_" + k)) for k in keys}
            blk = st.enter_context(nc.Block())
            mult = {k: (16 if k.startswith("dma_") else 1) for k in keys}

            def replay(engname):
                def f(e):
                    for fn, waits, key, inc in self.ops[engname]:
                        for k, c in waits:
                            e.wait_ge(sems[k], c * mult[k])
                        if fn is not None:
                            fn(e).then_inc(sems[key], inc)
                return f

            blk.tensor(replay("pe"))
            blk.scalar(replay("act"))
            blk.vector(replay("dve"))
            blk.gpsimd(replay("pool"))
            blk.sync(replay("sp"))
        return nc
from concourse.bass_utils import run_bass_kernel_spmd

D = 1024
INW = 3336
NEXP = 64
NEG = -1.0e30
C_Z, C_A, C_B, C_SU, C_SV = 0, 256, 260, 264, 520
NPT = 776
FM_MU, FM_W0, FM_A0, FM_KK, FM_KA, FM_RK, FM_GNG, FM_GNB, FM_CW, FM_SC = 0, 8, 10, 12, 14, 16, 18, 20, 22, 46
NFM = 52
BS_LNG, BS_LNB, BS_GN, BS_ALOG, BS_DT, BS_RB = 0, 256, 512, 768, 772, 776
NBS = 848
GELU_C = 1.5957691216057308


def make_consts(nblk):
    p = np.arange(128)
    blk = p // 64
    same = blk[:, None] == blk[None, :]
    i = p[:, None]
    j = p[None, :]
    parts = {}
    parts["ident"] = np.eye(128)
    parts["BD"] = same
    parts["UBLK"] = same & (i <= j)
    parts["ones"] = np.ones((128, 128))
    parts["NSL"] = -(same & (i > j)).astype(np.float64)
    parts["SL"] = same & (i > j)
    parts["SU"] = same & (i < j)
    parts["IU"] = same & (i <= j)
    parts["NEGL"] = np.where(same & (i >= j), 0.0, NEG)
    parts["NEGU"] = np.where(same & (i <= j), 0.0, NEG)
    parts["CHI"] = (blk[:, None] == np.arange(2)[None, :])
    parts["SU128"] = (i < j)
    parts["IU128"] = (i <= j)
    parts["iota64"] = np.tile(np.arange(64)[None, :], (128, 1))
    parts["pidx"] = p[:, None]
    parts["iotaB"] = np.tile((np.arange(nblk) * 128)[None, :], (128, 1))
    offs = {}
    cols = []
    o = 0
    for k, v in parts.items():
        v = np.asarray(v, dtype=np.float32)
        offs[k] = (o, o + v.shape[1])
        cols.append(v)
        o += v.shape[1]
    return np.ascontiguousarray(np.concatenate(cols, axis=1)), offs


TAPSHAPES = lambda SEQ, NT: {"p_tm": [SEQ, NPT], "oT": [NT, 128, 8 * 128], "x1": [SEQ, D], "h2": [SEQ, D],
                             "route": [SEQ, 6], "x2": [SEQ, D]}


def build(SEQ, DEPTH, taps=()):
    import os
    STOP = os.environ.get('KSTOP', '')
    NT = SEQ // 128
    PROWS = 2 * SEQ + NEXP * 128
    NBLK = PROWS // 128
    cvals, coffs = make_consts(NBLK)
    NCONST = cvals.shape[1]
    L = DEPTH
    nc = bass.Bass("TRN2", target_bir_lowering=False)

    def din(name, shape, dt=F32):
        return T(nc.dram_tensor(name, list(shape), dt, kind="ExternalInput"), name)

    x_d = din("x", [SEQ, D])
    cfm_d = din("cfm", [128, 8])
    adaw_d = din("ada_w", [L, D, 6 * D])
    adab_d = din("ada_b", [L, 6 * D])
    g12_d = din("g12", [L, 2 * D])
    win_d = din("w_in", [L, D, INW])
    wout_d = din("w_out", [L, D, D])
    fmp_d = din("fmp", [L, 128, NFM])
    bcs_d = din("bcs", [L, NBS])
    swT_d = din("sgu_wT", [L, 4, 128, 128])
    sbT_d = din("sgu_bT", [L, 128, 4])
    lwa_d = din("lora_wa", [L, 128, 256])
    gup_d = din("g_up", [L, 128, 256])
    wr_d = din("wr", [L, D, 72])
    ewg_d = din("ewg", [L * NEXP * 128, 2048])
    ewu_d = din("ewu", [L * NEXP * 128, 2048])
    ewd_d = din("ewd", [L * NEXP * 128, 2048])
    fing_d = din("fin_g", [D])
    con_d = din("consts", [128, NCONST])
    out_d = T(nc.dram_tensor("out", [SEQ, D], F32, kind="ExternalOutput"), "out")
    tap_d = {}
    for tname in taps:
        tap_d[tname] = T(nc.dram_tensor("tap_" + tname, TAPSHAPES(SEQ, NT)[tname], F32, kind="ExternalOutput"), tname)

    P = Prog(nc)

    def body(P):
        xres = P.dram("xres", [SEQ, D])
        hf = P.dram("hf", [SEQ, D])
        hs = P.dram("hs", [PROWS, D], BF16)
        ys = P.dram("ys", [PROWS, D])

        con = P.sb("con", [128, NCONST])
        P.dma("sp", con.v, con_d.v)

        def K(name):
            a, b = coffs[name]
            return con[:, a:b]

        ident, BDm, UBLK, ONES = K("ident"), K("BD"), K("UBLK"), K("ones")
        NSL, SLm, SUm, IUm, NEGL, NEGU = K("NSL"), K("SL"), K("SU"), K("IU"), K("NEGL"), K("NEGU")
        CHI, SU128, IU128, IOTA64, PIDX, IOTAB = K("CHI"), K("SU128"), K("IU128"), K("iota64"), K("pidx"), K("iotaB")
        identb = P.sb("identb", [128, 128], BF16)
        P.copy(identb.v, ident)

        banks = [P.ps("bank%d" % i, [128, 512]) for i in range(8)]
        bstate = [0]

        def nb():
            b = banks[bstate[0] % 8]
            bstate[0] += 1
            return b

        with P.scope():
            zt = P.sb("zt", [128, D], BF16)
            P.memset(zt.v, 0.0)
            for b in range(NBLK):
                P.dma("sp", hs[b * 128:(b + 1) * 128, :], zt.v)

        cact = P.sb("cact", [128, 8])
        P.dma("sp", cact.v, cfm_d.v)
        P.act(cact.v, cact.v, AF.Silu)

        eidx = P.sb("eidx", [128, NT, 2])
        gate = P.sb("gate", [128, NT, 2])
        rank = P.sb("rank", [128, NT, 2])
        dest_f = P.sb("dest_f", [128, NT, 2])
        dest_i = P.sb("dest_i", [128, NT, 2], I32)
        base = P.sb("base", [128, 64])
        idxb = P.sb("idxb", [128, NBLK], I32)
        modb = P.sb("modb", [128, 4, D])
        MB_GTM, MB_SHF, MB_G2, MB_GTF = 0, 1, 2, 3
        mfm = P.sb("mfm", [128, 16])
        gtf_prev = P.sb("gtf_prev", [128, D])

        def recip(out, in_):
            o, i_ = out.ap, in_.ap
            P.op("dve", lambda e: e.reciprocal(out=o, in_=i_), reads=_ts(in_), writes=_ts(out))

        def rsqrt(out, in_, eps, scale=1.0):
            P.act(out, in_, AF.Ln, bias=eps, scale=scale)
            P.act(out, out, AF.Exp, scale=-0.5)

        def tap(name, dst_fn, src):
            if name in tap_d:
                P.dma("sp", V(tap_d[name], dst_fn(tap_d[name].h)), src)

        def v3(t):
            return t.v.re("p (c t) -> p c t", c=2)

        def h4(v):
            return v.re("p (h c) -> p h c", h=4)

        def bc4(v):
            return v.unsq(2).bc([128, 4, 64])

        for l in range(L):
            with P.scope():
                pan = [P.sb("pan%d" % i, [128, 8, 512]) for i in range(2)]
                adab = P.sb("adab", [128, 6 * D])
                g12 = P.sb("g12", [128, 2 * D])
                crep = P.sb("crep", [128, 8, 128])
                mtmp = P.sb("mtmp", [128, 2, D])
                for k in range(8):
                    P.ts(crep[:, k, :], ONES, cact[:, k:k + 1], ALU.mult)
                P.dma("sp", adab.v, adab_d[l, :].pbc(128))
                P.dma("sp", g12.v, g12_d[l, :].pbc(128))
                awv = adaw_d[l].re("(k p) n -> p k n", p=128)
                dst = {0: mtmp[:, 0, :], 1: mtmp[:, 1, :], 2: modb[:, MB_GTM, :], 3: modb[:, MB_SHF, :],
                       4: modb[:, MB_G2, :], 5: modb[:, MB_GTF, :]}
                for n in range(12):
                    pn = pan[n % 2]
                    P.dma("sp", pn.v, awv[:, :, n * 512:(n + 1) * 512])
                    bk = nb()
                    for k in range(8):
                        P.mm(bk[:, :], crep[:, k, :], pn[:, k, :], start=(k == 0), stop=(k == 7))
                    seg, half = n // 2, n % 2
                    P.tt(dst[seg][:, half * 512:(half + 1) * 512], bk[:, :], adab[:, n * 512:(n + 1) * 512], ALU.add)
                P.stt(mtmp[:, 1, :], mtmp[:, 1, :], 1.0, g12[:, 0:D], ALU.add, ALU.mult)
                P.stt(modb[:, MB_G2, :], modb[:, MB_G2, :], 1.0, g12[:, D:2 * D], ALU.add, ALU.mult)
                for (which, col0) in ((1, 0), (0, 8)):
                    for half in range(2):
                        bk = nb()
                        for c4 in range(4):
                            c = half * 4 + c4
                            P.tr(bk[:, c4 * 128:(c4 + 1) * 128], mtmp[:, which, c * 128:(c + 1) * 128], ident)
                        P.copy(mfm[:, col0 + half * 4:col0 + half * 4 + 4], bk[:, :].re("p (c t) -> p c t", c=4)[:, :, 0])

            if STOP == 'mod':
                return
            with P.scope():
                w_in = P.sb("w_in", [128, 8, INW], BF16)
                w_out = P.sb("w_out", [128, 8, D], BF16)
                wiv = win_d[l].re("(k p) n -> p k n", p=128)
                wov = wout_d[l].re("(k p) n -> p k n", p=128)
                for k in range(8):
                    P.dma("pool", w_in[:, k, 0:1668], wiv[:, k, 0:1668])
                    P.dma("pool", w_in[:, k, 1668:INW], wiv[:, k, 1668:INW])
                    P.dma("pool", w_out[:, k, :], wov[:, k, :])
                fmp = P.sb("fmp", [128, NFM])
                bcs = P.sb("bcs", [128, NBS])
                P.dma("sp", fmp.v, fmp_d[l])
                P.dma("sp", bcs.v, bcs_d[l, :].pbc(128))
                nexpA = P.sb("nexpA", [128, 4])
                P.act(nexpA.v, bcs[:, BS_ALOG:BS_ALOG + 4], AF.Exp)
                P.ts(nexpA.v, nexpA.v, -1.0, ALU.mult)
                wsT = P.sb("wsT", [128, 4, 128])
                P.dma("sp", wsT.v, swT_d[l].re("h s t -> s h t"))
                for h in range(4):
                    P.tt(wsT[:, h, :], wsT[:, h, :], IU128, ALU.mult)
                bsT = P.sb("bsT", [128, 4])
                P.dma("sp", bsT.v, sbT_d[l])
                lwW = P.sb("lwW", [128, 256])
                lwA = P.sb("lwA", [128, 256])
                gup = P.sb("gup", [128, 256])
                P.memset(lwW.v, 0.0)
                P.memset(lwA.v, 0.0)
                P.dma("sp", lwW[0:64, :], lwa_d[l, 0:64, :])
                P.dma("sp", lwA[64:128, :], lwa_d[l, 64:128, :])
                P.dma("sp", gup.v, gup_d[l])
                wr = P.sb("wr", [128, 8, 72])
                P.dma("sp", wr.v, wr_d[l].re("(k p) n -> p k n", p=128))

                Sg = P.sb("Sg", [128, 2, 128])
                Sr = P.sb("Sr", [128, 2, 128])
                qkv_raw = P.sb("qkv_raw", [128, 6, 131])
                prodC = P.sb("prodC", [128, 2, 130])
                rw_raw = P.sb("rw_raw", [128, 8, 129])
                for t_ in (Sg, Sr, qkv_raw, prodC, rw_raw, base):
                    P.memset(t_.v, 0.0)

                xt = P.sb("xt", [128, D])
                hh = P.sb("hh", [128, D])
                hT = P.sb("hT", [128, 8, 128], BF16)
                h2T = P.sb("h2T", [128, 8, 128])
                h2f = h2T.v.re("p k t -> p (k t)")
                p_tm = P.sb("p_tm", [128, NPT])
                cacc = P.sb("cacc", [128, 6, 128])
                cbch = P.sb("cbch", [128, 6, 128])
                rw = P.sb("rw", [128, 8, 128])
                oT = P.sb("oT", [128, 8, 128], BF16)
                oTf = P.sb("oTf", [128, 8, 128]) if "oT" in tap_d else None
                s1 = P.sb("s1", [128, 8])
                qk = P.sb("qk", [128, 4, 128])
                sq4 = P.sb("sq4", [128, 4, 128])
                kv_tm = P.sb("kv_tm", [128, 512])
                g4 = P.sb("g4", [128, 4])
                t4 = P.sb("t4", [128, 4])
                beta4 = P.sb("beta4", [128, 4])
                gm = P.sb("gm", [128, 2, 4])
                gc = P.sb("gc", [128, 4])
                ngc = P.sb("ngc", [128, 4])
                glr = P.sb("glr", [128, 8])
                glt = P.sb("glt", [128, 4])
                egc = P.sb("egc", [128, 4])
                ktf = P.sb("ktf", [128, 4])
                cdg = P.sb("cdg", [128, 8])
                bg = P.sb("bg", [128, 4])
                gcrow = P.sb("gcrow", [128, 4, 128])
                egcrow = P.sb("egcrow", [128, 4, 128])
                qdT = P.sb("qdT", [128, 256])
                vb = P.sb("vb", [128, 256])
                kbg = P.sb("kbg", [128, 256])
                ktail = P.sb("ktail", [128, 256])
                stmp = P.sb("stmp", [128, 128])
                cdp = P.sb("cdp", [128, 4])
                u_sb = P.sb("u_sb", [128, 256])
                wT_sb = P.sb("wT_sb", [128, 256])
                vnew = P.sb("vnew", [128, 256])
                o_sb = P.sb("o_sb", [128, 256])
                o_sq = P.sb("o_sq", [128, 256])
                sz = P.sb("sz", [128, 256])
                DD = [dict(tmp=P.sb("dd_tmp%d" % i, [128, 128]), D=P.sb("dd_D%d" % i, [128, 128]),
                           DT=P.sb("dd_DT%d" % i, [128, 128])) for i in range(2)]
                HM = []
                for h in range(4):
                    HM.append(dict(M1=P.sb("hm_M1%d" % h, [128, 128]), M2=P.sb("hm_M2%d" % h, [128, 128]),
                                   M3=P.sb("hm_M3%d" % h, [128, 128]), PQ=P.sb("hm_PQ%d" % h, [128, 2, 128]),
                                   R=P.sb("hm_R%d" % h, [128, 128])))
                lg = P.sb("lg", [128, 72])
                r8 = P.sb("r8", [128, 8, 8])
                sel = P.sb("sel", [128, 8])
                ohg = P.sb("ohg", [128, 8])
                oh1 = P.sb("oh1", [128, 8])
                oh2 = P.sb("oh2", [128, 8])
                Mx = [P.sb("Mx%d" % i, [128, 64]) for i in range(3)]
                rke = P.sb("rke", [128, 64])
                rt = P.sb("rt", [128, 64])
                sc = P.sb("sc", [128, 8])
                for t_ in (vnew, o_sq, o_sb, sz):
                    P.memset(t_.v, 0.0)

                kk_t = P.sb("kk_t", [128, 4, 128])
                kkT, kmT = kk_t[:, 0:2, :], kk_t[:, 2:4, :]
                cum, eW = gcrow[:, 0:2, :], gcrow[:, 2:4, :]
                eWi, eWp = egcrow[:, 0:2, :], egcrow[:, 2:4, :]
                ab_t = P.sb("ab_t", [128, 8, 128])
                AtT, BtT, KtT, RtT = ab_t[:, 0:2, :], ab_t[:, 2:4, :], ab_t[:, 4:6, :], ab_t[:, 6:8, :]
                lz_t = P.sb("lz_t", [128, 256]); asg_t = P.sb("asg_t", [128, 256])
                gateT_t = P.sb("gateT_t", [128, 256]); lact_t = P.sb("lact_t", [128, 256])
                lz, asg, gateT, lact = v3(lz_t), v3(asg_t), v3(gateT_t), v3(lact_t)
                rkb, ynT = v3(u_sb), v3(wT_sb)
                Z_sb, U_sb, y_sb, y_sq = vnew, o_sq, o_sb, sz
                bkv_tm = cacc.v.re("p (q x) t -> p q (x t)", q=3)
                ugl, vgl, ob = o_sb, o_sq, u_sb
                wT3 = v3(wT_sb)
                qd3 = v3(qdT)

                def neumann():
                    for lvl in range(1, 6):
                        bks = []
                        for h in range(4):
                            hm = HM[h]
                            bk = nb()
                            Pk, Qk = hm["PQ"][:, 0, :], hm["PQ"][:, 1, :]
                            P.mmr(bk[:, 0:128], Qk, Pk)
                            if lvl < 5:
                                P.mmr(bk[:, 128:256], Pk, Qk)
                            bks.append(bk)
                        for h in range(4):
                            n = 256 if lvl < 5 else 128
                            P.copy(HM[h]["PQ"].v.re("p a t -> p (a t)")[:, 0:n].bitcast(F32R), bks[h][:, 0:n], eng="act")
                        for h in range(4):
                            P.mmr(bks[h][:, 256:384], HM[h]["PQ"][:, 0, :], HM[h]["R"].v)
                        for h in range(4):
                            P.tt(HM[h]["R"].v.bitcast(F32R), bks[h][:, 256:384], HM[h]["R"].v, ALU.add)

                def rstd_of(dst, src, eps):
                    P.act(h2f, src, AF.Square, accum=s1[:, 0:1])
                    rsqrt(dst, s1[:, 0:1], eps, 1.0 / D)

                for i in range(NT):
                    tsl = slice(i * 128, (i + 1) * 128)
                    if l == 0:
                        P.dma("sp", xt.v, x_d[tsl, :])
                    else:
                        P.dma("sp", xt.v, xres[tsl, :])
                        P.gather(hh.v, ys.v, dest_i[:, i, 0:1])
                        P.gather(h2f, ys.v, dest_i[:, i, 1:2])
                        P.ts(hh.v, hh.v, gate[:, i, 0:1], ALU.mult)
                        P.stt(hh.v, h2f, gate[:, i, 1:2], hh.v, ALU.mult, ALU.add)
                        P.tt(hh.v, hh.v, gtf_prev.v, ALU.mult)
                        P.tt(xt.v, xt.v, hh.v, ALU.add)
                        if l == 1:
                            tap("x2", lambda hd: hd[tsl, :], xt.v)
                    rstd_of(s1[:, 1:2], xt.v, 1e-6)
                    P.ts(hh.v, xt.v, s1[:, 1:2], ALU.mult)
                    for half in range(2):
                        bk = nb()
                        for c4 in range(4):
                            c = half * 4 + c4
                            P.tr(bk[:, c4 * 128:(c4 + 1) * 128], hh[:, c * 128:(c + 1) * 128], ident)
                        for c4 in range(4):
                            c = half * 4 + c4
                            P.act(hT[:, c, :], bk[:, c4 * 128:(c4 + 1) * 128], AF.Identity,
                                  bias=mfm[:, 8 + c:9 + c], scale=mfm[:, c:c + 1])
                    for (c0, c1) in ((768, 1280), (1280, 1544)):
                        bk = nb()
                        for k in range(8):
                            P.mm(bk[:, 0:c1 - c0], hT[:, k, :], w_in[:, k, c0:c1], start=(k == 0), stop=(k == 7))
                        P.copy(p_tm[:, c0 - 768:c1 - 768], bk[:, 0:c1 - c0], eng="act")
                    tap("p_tm", lambda hd: hd[tsl, :], p_tm.v)

                    def fm_group(col0, nch, dst_fn):
                        for g0 in range(0, nch, 4):
                            n = min(4, nch - g0)
                            bk = nb()
                            for cc in range(n):
                                cs_ = col0 + (g0 + cc) * 128
                                for k in range(8):
                                    P.mm(bk[:, cc * 128:(cc + 1) * 128], w_in[:, k, cs_:cs_ + 128], hT[:, k, :],
                                         start=(k == 0), stop=(k == 7))
                            P.copy(dst_fn(g0, n), bk[:, 0:n * 128].re("p (c t) -> p c t", c=n), eng="act")
                    fm_group(0, 6, lambda g0, n: qkv_raw[:, g0:g0 + n, 3:131])
                    fm_group(1544, 6, lambda g0, n: cbch[:, g0:g0 + n, :])
                    fm_group(2312, 8, lambda g0, n: rw_raw[:, g0:g0 + n, 1:129])

                    if STOP == 'proj':
                        continue
                    P.tt(prodC[:, :, 2:130], cbch[:, 2:4, :], cbch[:, 4:6, :], ALU.mult)
                    for c in range(2):
                        w = lambda k: fmp[:, FM_SC + c * 3 + k:FM_SC + c * 3 + k + 1]
                        P.ts(cacc[:, c, :], prodC[:, c, 2:130], w(2), ALU.mult)
                        P.stt(cacc[:, c, :], prodC[:, c, 1:129], w(1), cacc[:, c, :], ALU.mult, ALU.add)
                        P.stt(cacc[:, c, :], prodC[:, c, 0:128], w(0), cacc[:, c, :], ALU.mult, ALU.add)
                    P.tt(oT[:, 4:6, :], cacc[:, 0:2, :], cbch[:, 0:2, :], ALU.mult)
                    if oTf is not None:
                        P.tt(oTf[:, 4:6, :], cacc[:, 0:2, :], cbch[:, 0:2, :], ALU.mult)
                    P.copy(prodC[:, :, 0:2], prodC[:, :, 128:130])

                    suv = p_tm[:, C_SU:C_SU + 512]
                    gtmp = kv_tm.v
                    P.tt(gtmp, suv, suv, ALU.mult)
                    P.ts(gtmp, gtmp, 0.044715, ALU.mult, 1.0, ALU.add)
                    P.tt(gtmp, gtmp, suv, ALU.mult)
                    P.act(gtmp, gtmp, AF.Sigmoid, scale=GELU_C)
                    P.tt(ugl.v, gtmp[:, 0:256], suv[:, 0:256], ALU.mult)
                    P.tt(vgl.v, gtmp[:, 256:512], suv[:, 256:512], ALU.mult)
                    for c in range(6):
                        w = lambda k: fmp[:, FM_CW + c * 4 + k:FM_CW + c * 4 + k + 1]
                        P.ts(cacc[:, c, :], qkv_raw[:, c, 3:131], w(3), ALU.mult)
                        for k in (2, 1, 0):
                            P.stt(cacc[:, c, :], qkv_raw[:, c, k:k + 128], w(k), cacc[:, c, :], ALU.mult, ALU.add)
                    P.act(cacc.v, cacc.v, AF.Silu)
                    qkv = cacc
                    P.copy(qkv_raw[:, :, 0:3], qkv_raw[:, :, 128:131])
                    P.act(sz.v, p_tm[:, C_Z:C_Z + 256], AF.Silu)
                    P.tt(rw.v, rw_raw[:, :, 0:128], rw_raw[:, :, 1:129], ALU.subtract)
                    for c in range(8):
                        P.stt(rw[:, c, :], rw[:, c, :], fmp[:, FM_MU + c:FM_MU + c + 1], rw_raw[:, c, 1:129],
                              ALU.mult, ALU.add)
                    P.copy(rw_raw[:, :, 0:1], rw_raw[:, :, 128:129])
                    P.act(lact[0:64, 0, :], rw[0:64, 6, :], AF.Tanh)
                    P.copy(lact[64:128, 0, :], rw[64:128, 6, :])
                    P.act(lact[:, 1, :], rw[:, 7, :], AF.Sigmoid)
                    bk = nb()
                    bkg = nb()
                    for c in range(2):
                        ch = slice(c * 128, (c + 1) * 128)
                        P.mm(bk[:, c * 128:(c + 1) * 128], lwW[:, ch], lact[:, 0, :])
                        P.mm(bk[:, 256 + c * 128:256 + (c + 1) * 128], lwA[:, ch], lact[:, 0, :])
                        P.mm(bkg[:, c * 128:(c + 1) * 128], gup[:, ch], lact[:, 1, :])
                    for c in range(2):
                        P.act(lz[:, c, :], bk[:, c * 128:(c + 1) * 128], AF.Sigmoid,
                              bias=fmp[:, FM_W0 + c:FM_W0 + c + 1])
                        P.act(asg[:, c, :], bk[:, 256 + c * 128:256 + (c + 1) * 128], AF.Sigmoid,
                              bias=fmp[:, FM_A0 + c:FM_A0 + c + 1])
                    P.ts(lz, lz, -0.6065306597126334, ALU.mult)
                    P.copy(gateT, bkg[:, 0:256].re("p (c t) -> p c t", c=2), eng="act")
                    P.reduce(s1[:, 2:3], vgl.v, ALU.add)
                    P.ts(s1[:, 3:4], s1[:, 2:3], -1.0 / 256, ALU.mult)
                    P.ts(vgl.v, vgl.v, s1[:, 3:4], ALU.add)
                    P.act(gtmp[:, 0:256], vgl.v, AF.Square, accum=s1[:, 4:5])
                    rsqrt(s1[:, 5:6], s1[:, 4:5], 1e-5, 1.0 / 256)
                    P.stt(vgl.v, vgl.v, s1[:, 5:6], bcs[:, BS_LNG:BS_LNG + 256], ALU.mult, ALU.mult)
                    P.tt(vgl.v, vgl.v, bcs[:, BS_LNB:BS_LNB + 256], ALU.add)
                    bk = nb()
                    for h in range(4):
                        P.mm(bk[:, h * 64:(h + 1) * 64], wsT[:, h, :], vgl[:, h * 64:(h + 1) * 64])
                    P.tt(h4(ob.v), h4(bk[:, 0:256]), bc4(bsT.v), ALU.add)
                    P.tt(ob.v, ob.v, ugl.v, ALU.mult)
                    bk = nb()
                    for c in range(2):
                        P.tr(bk[:, c * 128:(c + 1) * 128], ob[:, c * 128:(c + 1) * 128], ident)
                    P.copy(oT[:, 2:4, :], bk[:, 0:256].re("p (c t) -> p c t", c=2))
                    if oTf is not None:
                        P.copy(oTf[:, 2:4, :], bk[:, 0:256].re("p (c t) -> p c t", c=2))

                    if STOP == 'sgu':
                        continue
                    P.tt(sq4.v, qkv[:, 0:4, :], qkv[:, 0:4, :], ALU.mult)
                    bk = nb()
                    for c in range(4):
                        P.mm(bk[:, c * 128:(c + 1) * 128], BDm, sq4[:, c, :])
                    rsqrt(sq4.v.re("p c t -> p (c t)"), bk[:, :], 1e-6)
                    P.stt(qk[:, 0:2, :].bitcast(F32R), qkv[:, 0:2, :], 0.125, sq4[:, 0:2, :], ALU.mult, ALU.mult)
                    P.tt(qk[:, 2:4, :].bitcast(F32R), qkv[:, 2:4, :], sq4[:, 2:4, :], ALU.mult)
                    bk = nb()
                    P.tr(bk[:, 0:128], qk[:, 2, :], ident)
                    P.tr(bk[:, 128:256], qk[:, 3, :], ident)
                    P.tr(bk[:, 256:384], qkv[:, 4, :], ident)
                    P.tr(bk[:, 384:512], qkv[:, 5, :], ident)
                    P.copy(kv_tm.v, bk[:, :], eng="act")
                    if STOP == 'g1':
                        continue
                    P.tt(t4.v, p_tm[:, C_A:C_A + 4], bcs[:, BS_DT:BS_DT + 4], ALU.add)
                    P.act(t4.v, t4.v, AF.Exp)
                    P.act(t4.v, t4.v, AF.Ln, bias=1.0)
                    P.tt(g4.v, t4.v, nexpA.v, ALU.mult)
                    P.act(beta4.v, p_tm[:, C_B:C_B + 4], AF.Exp, scale=-1.0)
                    P.ts(beta4.v, beta4.v, 1.0, ALU.add)
                    recip(beta4.v, beta4.v)
                    for c in range(2):
                        P.ts(gm[:, c, :], g4.v, CHI[:, c:c + 1], ALU.mult)
                    bk = nb()
                    P.mm(bk[:, 0:4], UBLK, g4.v)
                    P.mm(bk[:, 4:12], ONES, gm.v.re("p c h -> p (c h)"))
                    P.copy(gc.v, bk[:, 0:4])
                    P.copy(glr.v, bk[:, 4:12])
                    P.ts(ngc.v, gc.v, -1.0, ALU.mult)
                    P.ts(glt.v, glr[:, 0:4], CHI[:, 0:1], ALU.mult)
                    P.stt(glt.v, glr[:, 4:8], CHI[:, 1:2], glt.v, ALU.mult, ALU.add)
                    P.act(egc.v, gc.v, AF.Exp)
                    P.tt(t4.v, glt.v, gc.v, ALU.subtract)
                    P.act(ktf.v, t4.v, AF.Exp)
                    bkc_ = nb()
                    P.mm(bkc_[0:64, 0:4], ONES[:, 0:64], gm.v[:, :, 0::2])
                    P.mm(bkc_[64:128, 0:4], ONES[:, 0:64], gm.v[:, :, 1::2])
                    P.act(cdp.v, bkc_[:, 0:4], AF.Exp)
                    P.tt(bg.v, beta4.v, egc.v, ALU.mult)
                    if STOP == 'g2':
                        continue
                    G4 = sq4
                    for h in range(4):
                        P.ts(G4[:, h, :], UBLK, g4[:, h:h + 1], ALU.mult)
                    if STOP == 'x1':
                        continue
                    bk = nb()
                    P.mm(bk[:, :], ONES, G4.v.re("p h t -> p (h t)"))
                    if STOP == 'x2':
                        continue
                    P.copy(gcrow.v.re("p h t -> p (h t)"), bk[:, :])
                    if STOP == 'x3':
                        continue
                    P.act(egcrow.v.re("p h t -> p (h t)"), bk[:, :], AF.Exp)
                    if STOP == 'g2a':
                        continue
                    for h in range(4):
                        j, hp = h // 2, 64 * (h % 2)
                        P.tt(qd3[hp:hp + 64, j, :], qk[hp:hp + 64, j, :], egcrow[hp:hp + 64, h, :], ALU.mult)
                    if STOP == 'g2b':
                        continue
                    P.tt(h4(vb.v), h4(kv_tm[:, 256:512]), bc4(beta4.v), ALU.mult)
                    P.tt(h4(kbg.v), h4(kv_tm[:, 0:256]), bc4(bg.v), ALU.mult)
                    P.tt(h4(ktail.v), h4(kv_tm[:, 0:256]), bc4(ktf.v), ALU.mult)
                    if STOP == 'g3':
                        continue
                    for h in range(4):
                        j, hp = h // 2, 64 * (h % 2)
                        hm = HM[h]
                        dd = DD[h % 2]
                        kTh = qk[hp:hp + 64, 2 + j, :]
                        qTh = qk[hp:hp + 64, j, :]
                        bk = nb()
                        P.mmr(bk[:, 0:128], kTh, kTh)
                        P.mmr(bk[:, 128:256], kTh, qTh)
                        P.stt(dd["tmp"].v, gcrow[:, h, :], -1.0, NEGL, ALU.mult, ALU.add)
                        P.act(dd["D"].v, dd["tmp"].v, AF.Exp, bias=gc[:, h:h + 1])
                        P.tt(dd["tmp"].v, gcrow[:, h, :], NEGU, ALU.add)
                        P.act(dd["DT"].v, dd["tmp"].v, AF.Exp, bias=ngc[:, h:h + 1])
                        P.stt(dd["D"].v, bk[:, 0:128], beta4[:, h:h + 1], dd["D"].v, ALU.mult, ALU.mult)
                        P.tt(hm["PQ"][:, 0, :].bitcast(F32R), dd["D"].v, NSL, ALU.mult)
                        P.tt(hm["M1"].v, bk[:, 128:256], dd["DT"].v, ALU.mult)
                        bk2 = nb()
                        P.tr(bk2[:, 0:128], hm["PQ"][:, 0, :], ident)
                        P.copy(hm["PQ"][:, 1, :].bitcast(F32R), bk2[:, 0:128], eng="act")
                        P.tt(hm["R"].v.bitcast(F32R), bk2[:, 0:128], ident, ALU.add)
                    if STOP == 'g4':
                        continue
                    neumann()
                    if STOP == 'g5':
                        continue
                    bku = nb()
                    bkw = nb()
                    for h in range(4):
                        j, hp = h // 2, 64 * (h % 2)
                        TinvT = HM[h]["R"].v
                        P.mm(bku[:, h * 64:(h + 1) * 64], TinvT, vb[:, h * 64:(h + 1) * 64])
                        P.mm(bkw[hp:hp + 64, j * 128:(j + 1) * 128], kbg[:, h * 64:(h + 1) * 64], TinvT)
                    P.copy(u_sb.v, bku[:, 0:256], eng="act")
                    P.copy(wT_sb.v, bkw[:, 0:256])
                    if STOP == 'g6':
                        continue
                    bko = nb()
                    for c in range(2):
                        cs = slice(64 * c, 64 * c + 64)
                        bkv = nb()
                        for j in range(2):
                            P.mm(bkv[cs, j * 128:(j + 1) * 128], wT3[:, j, cs], Sg[:, j, :])
                        P.tt(vnew[cs, :], u_sb[cs, :], bkv[cs, 0:256], ALU.subtract)
                        bks = nb()
                        for j in range(2):
                            pc = slice(j * 128, (j + 1) * 128)
                            P.mm(bko[cs, pc], qd3[:, j, cs], Sg[:, j, :], start=True, stop=False)
                            for h_ in range(2):
                                h = 2 * j + h_
                                hc = slice(h * 64, (h + 1) * 64)
                                P.mm(bko[cs, hc], HM[h]["M1"][:, cs], vnew[:, hc], start=False, stop=(h_ == 1))
                        for j in range(2):
                            pc = slice(j * 128, (j + 1) * 128)
                            P.mm(bks[:, pc], ktail[cs, pc], vnew[cs, pc])
                        for j in range(2):
                            pc = slice(j * 128, (j + 1) * 128)
                            P.tt(stmp.v, bks[:, pc], BDm, ALU.mult)
                            P.stt(Sg[:, j, :], Sg[:, j, :], cdp[:, c * 2 + j:c * 2 + j + 1], stmp.v, ALU.mult, ALU.add)
                    P.copy(o_sb.v, bko[:, 0:256], eng="act")
                    P.tt(o_sq.v, o_sb.v, o_sb.v, ALU.mult)
                    P.reduce(s1[:, 4:8], h4(o_sq.v), ALU.add)
                    rsqrt(t4.v, s1[:, 4:8], 1e-6, 1.0 / 64)
                    P.tt(h4(o_sb.v), h4(o_sb.v), bc4(t4.v), ALU.mult)
                    P.tt(o_sb.v, o_sb.v, bcs[:, BS_GN:BS_GN + 256], ALU.mult)
                    P.tt(o_sb.v, o_sb.v, sz.v, ALU.mult)
                    bk = nb()
                    for c in range(2):
                        P.tr(bk[:, c * 128:(c + 1) * 128], o_sb[:, c * 128:(c + 1) * 128], ident)
                    P.copy(oT[:, 0:2, :], bk[:, 0:256].re("p (c t) -> p c t", c=2))
                    if oTf is not None:
                        P.copy(oTf[:, 0:2, :], bk[:, 0:256].re("p (c t) -> p c t", c=2))

                    if STOP == 'gdn':
                        continue
                    for c in range(2):
                        P.ts(kkT[:, c, :], rw[:, 2 + c, :], fmp[:, FM_KK + c:FM_KK + c + 1], ALU.mult)
                    P.tt(sq4[:, 0:2, :], kkT, kkT, ALU.mult)
                    bk = nb()
                    for c in range(2):
                        P.mm(bk[:, c * 128:(c + 1) * 128], BDm, sq4[:, c, :])
                    rsqrt(sq4[:, 0:2, :], bk[:, 0:256].re("p (c t) -> p c t", c=2), 1e-12)
                    P.tt(kkT, kkT, sq4[:, 0:2, :], ALU.mult)
                    for c in range(2):
                        P.ts(kmT[:, c, :], asg[:, c, :], -1.0, ALU.add, fmp[:, FM_KA + c:FM_KA + c + 1], ALU.mult)
                    P.stt(kmT, kmT, 1.0, rw[:, 2:4, :], ALU.add, ALU.mult)
                    for c in range(2):
                        for cc in range(2):
                            P.scan(cum[:, c, cc * 64:(cc + 1) * 64], ONES[:, 0:64], lz[:, c, cc * 64:(cc + 1) * 64],
                                   0.0, ALU.mult, ALU.add)
                    P.tt(eWp, cum, lz, ALU.subtract)
                    P.act(eWp, eWp, AF.Exp)
                    P.act(eWi, cum, AF.Exp, scale=-1.0)
                    P.act(eW, cum, AF.Exp)
                    P.stt(AtT.bitcast(F32R), kkT, -1.0, eWp, ALU.mult, ALU.mult)
                    P.tt(BtT.bitcast(F32R), kkT, asg, ALU.mult)
                    P.tt(BtT.bitcast(F32R), BtT, eWi, ALU.mult)
                    P.tt(KtT.bitcast(F32R), kmT, eWi, ALU.mult)
                    P.tt(RtT.bitcast(F32R), rw[:, 0:2, :], eW, ALU.mult)
                    for (q_, src) in ((0, BtT), (1, KtT), (2, rw[:, 4:6, :])):
                        bk = nb()
                        for c in range(2):
                            P.tr(bk[:, c * 128:(c + 1) * 128], src[:, c, :], ident)
                        P.copy(bkv_tm[:, q_, :], bk[:, 0:256], eng="act")
                    for h in range(4):
                        j, hp = h // 2, 64 * (h % 2)
                        hm = HM[h]
                        hs_ = slice(hp, hp + 64)
                        bk = nb()
                        P.mmr(bk[:, 0:128], AtT[hs_, j, :], BtT[hs_, j, :])
                        P.mmr(bk[:, 128:256], BtT[hs_, j, :], AtT[hs_, j, :])
                        P.mmr(bk[:, 256:384], KtT[hs_, j, :], AtT[hs_, j, :])
                        bk2 = nb()
                        P.mmr(bk2[:, 0:128], BtT[hs_, j, :], RtT[hs_, j, :])
                        P.mmr(bk2[:, 128:256], KtT[hs_, j, :], RtT[hs_, j, :])
                        P.tt(hm["PQ"][:, 0, :].bitcast(F32R), bk[:, 0:128], SLm, ALU.mult)
                        P.tt(hm["PQ"][:, 1, :].bitcast(F32R), bk[:, 128:256], SUm, ALU.mult)
                        P.tt(hm["R"].v.bitcast(F32R), hm["PQ"][:, 1, :], ident, ALU.add)
                        P.tt(hm["M1"].v, bk[:, 256:384], SUm, ALU.mult)
                        P.tt(hm["M2"].v, bk2[:, 0:128], IUm, ALU.mult)
                        P.tt(hm["M3"].v, bk2[:, 128:256], IUm, ALU.mult)
                    neumann()
                    bky = nb()
                    for c in range(2):
                        cs = slice(64 * c, 64 * c + 64)
                        last = 64 * c + 63
                        bkz = nb()
                        for j in range(2):
                            pc = slice(j * 128, (j + 1) * 128)
                            P.mm(bkz[cs, pc], AtT[:, j, cs], Sr[:, j, :], start=True, stop=False)
                            for h_ in range(2):
                                h = 2 * j + h_
                                hc = slice(h * 64, (h + 1) * 64)
                                P.mm(bkz[cs, hc], HM[h]["M1"][:, cs], bkv_tm[:, 2, hc], start=False, stop=(h_ == 1))
                        P.copy(Z_sb[cs, :], bkz[cs, 0:256])
                        bku_ = nb()
                        for h in range(4):
                            hc = slice(h * 64, (h + 1) * 64)
                            P.mm(bku_[cs, hc], HM[h]["R"][:, cs], Z_sb[:, hc])
                        P.copy(U_sb[cs, :], bku_[cs, 0:256], eng="act")
                        bks = nb()
                        for j in range(2):
                            pc = slice(j * 128, (j + 1) * 128)
                            P.mm(bky[cs, pc], RtT[:, j, cs], Sr[:, j, :], start=True, stop=False)
                            for h_ in range(2):
                                h = 2 * j + h_
                                hc = slice(h * 64, (h + 1) * 64)
                                P.mm(bky[cs, hc], HM[h]["M2"][:, cs], U_sb[:, hc], start=False, stop=False)
                                P.mm(bky[cs, hc], HM[h]["M3"][:, cs], bkv_tm[:, 2, hc], start=False, stop=(h_ == 1))
                        for j in range(2):
                            pc = slice(j * 128, (j + 1) * 128)
                            P.mm(bks[:, pc], bkv_tm[cs, 0, pc], U_sb[cs, pc], start=True, stop=False)
                            P.mm(bks[:, pc], bkv_tm[cs, 1, pc], bkv_tm[cs, 2, pc], start=False, stop=True)
                        for j in range(2):
                            pc = slice(j * 128, (j + 1) * 128)
                            P.tt(stmp.v, bks[:, pc], Sr[:, j, :], ALU.add)
                            P.stt(Sr[:, j, :], stmp.v, eW[:, j, last:last + 1], BDm, ALU.mult, ALU.mult)
                    P.copy(y_sb.v, bky[:, 0:256], eng="act")
                    P.reduce(s1[:, 4:8], h4(y_sb.v), ALU.add)
                    P.ts(t4.v, s1[:, 4:8], -1.0 / 64, ALU.mult)
                    P.tt(h4(y_sb.v), h4(y_sb.v), bc4(t4.v), ALU.add)
                    P.tt(y_sq.v, y_sb.v, y_sb.v, ALU.mult)
                    P.reduce(s1[:, 4:8], h4(y_sq.v), ALU.add)
                    rsqrt(t4.v, s1[:, 4:8], 64e-5, 1.0 / 64)
                    P.tt(h4(y_sb.v), h4(y_sb.v), bc4(t4.v), ALU.mult)
                    bk = nb()
                    for c in range(2):
                        P.tr(bk[:, c * 128:(c + 1) * 128], y_sb[:, c * 128:(c + 1) * 128], ident)
                    for c in range(2):
                        P.ts(ynT[:, c, :], bk[:, c * 128:(c + 1) * 128], fmp[:, FM_GNG + c:FM_GNG + c + 1], ALU.mult,
                             fmp[:, FM_GNB + c:FM_GNB + c + 1], ALU.add)
                        P.stt(rkb[:, c, :], rw[:, c, :], fmp[:, FM_RK + c:FM_RK + c + 1], kmT[:, c, :], ALU.mult, ALU.mult)
                    bk = nb()
                    for c in range(2):
                        P.mm(bk[:, c * 128:(c + 1) * 128], BDm, rkb[:, c, :])
                    P.tt(rkb, bk[:, 0:256].re("p (c t) -> p c t", c=2), rw[:, 4:6, :], ALU.mult)
                    P.tt(ynT, ynT, rkb, ALU.add)
                    P.tt(oT[:, 6:8, :], ynT, gateT, ALU.mult)
                    if oTf is not None:
                        P.tt(oTf[:, 6:8, :], ynT, gateT, ALU.mult)
                        tap("oT", lambda hd: hd[i], oTf.v.re("p c t -> p (c t)"))

                    if STOP == 'rwkv':
                        continue
                    for half in range(2):
                        bk = nb()
                        for c in range(8):
                            P.mm(bk[:, :], oT[:, c, :], w_out[:, c, half * 512:(half + 1) * 512],
                                 start=(c == 0), stop=(c == 7))
                        hsl = slice(half * 512, (half + 1) * 512)
                        P.tt(hh[:, hsl], bk[:, :], modb[:, MB_GTM, hsl], ALU.mult)
                        P.tt(xt[:, hsl], xt[:, hsl], hh[:, hsl], ALU.add)
                    P.dma("sp", xres[tsl, :], xt.v)
                    if l == 0:
                        tap("x1", lambda hd: hd[tsl, :], xt.v)

                    if STOP == 'wout':
                        continue
                    rstd_of(s1[:, 1:2], xt.v, 1e-6)
                    P.stt(hh.v, xt.v, s1[:, 1:2], modb[:, MB_G2, :], ALU.mult, ALU.mult)
                    P.tt(hh.v, hh.v, modb[:, MB_SHF, :], ALU.add)
                    P.dma("sp", hf[tsl, :], hh.v)
                    if l == 0:
                        tap("h2", lambda hd: hd[tsl, :], hh.v)
                    for half in range(2):
                        bk = nb()
                        for c4 in range(4):
                            c = half * 4 + c4
                            P.tr(bk[:, c4 * 128:(c4 + 1) * 128], hh[:, c * 128:(c + 1) * 128], ident)
                        P.copy(h2T[:, half * 4:(half + 1) * 4, :], bk[:, :].re("p (c t) -> p c t", c=4), eng="act")
                    bk = nb()
                    for k in range(8):
                        P.mm(bk[:, 0:72], h2T[:, k, :], wr[:, k, :], start=(k == 0), stop=(k == 7))
                    P.tt(lg.v, bk[:, 0:72], bcs[:, BS_RB:BS_RB + 72], ALU.add)
                    P.reduce(sc[:, 0:1], lg[:, 0:8], ALU.max)
                    P.ts(ohg.v, lg[:, 0:8], sc[:, 0:1], ALU.is_equal)
                    P.ts(sc[:, 1:2], sc[:, 0:1], -1.0, ALU.mult)
                    P.act(sel.v, lg[:, 0:8], AF.Exp, bias=sc[:, 1:2], accum=sc[:, 2:3])
                    recip(sc[:, 2:3], sc[:, 2:3])
                    P.tt(r8.v, lg[:, 8:72].re("p (g j) -> p g j", g=8), ohg.v.unsq(2).bc([128, 8, 8]), ALU.mult)
                    P.reduce(sel.v, r8.v.re("p g j -> p j g"), ALU.add)
                    P.reduce(sc[:, 3:4], sel.v, ALU.max)
                    P.ts(oh1.v, sel.v, sc[:, 3:4], ALU.is_equal)
                    P.stt(sel.v, oh1.v, -1.0e30, sel.v, ALU.mult, ALU.add)
                    P.reduce(sc[:, 4:5], sel.v, ALU.max)
                    P.ts(oh2.v, sel.v, sc[:, 4:5], ALU.is_equal)
                    P.tt(sc[:, 5:6], sc[:, 3:4], sc[:, 4:5], ALU.subtract)
                    P.act(sc[:, 5:6], sc[:, 5:6], AF.Exp, scale=-1.0)
                    P.ts(sc[:, 5:6], sc[:, 5:6], 1.0, ALU.add)
                    recip(sc[:, 5:6], sc[:, 5:6])
                    P.tt(gate[:, i, 0:1], sc[:, 5:6], sc[:, 2:3], ALU.mult)
                    P.tt(gate[:, i, 1:2], sc[:, 2:3], gate[:, i, 0:1], ALU.subtract)
                    for (mm_, oh) in ((Mx[0], oh1), (Mx[1], oh2)):
                        P.tt(mm_.v.re("p (g j) -> p g j", g=8), ohg.v.unsq(2).bc([128, 8, 8]),
                             oh.v.unsq(1).bc([128, 8, 8]), ALU.mult)
                    P.tt(Mx[2].v, Mx[0].v, Mx[1].v, ALU.add)
                    bk = nb()
                    P.mm(bk[:, 0:64], SU128, Mx[2].v)
                    P.mm(bk[:, 64:128], ONES, Mx[2].v)
                    P.tt(rke.v, bk[:, 0:64], base.v, ALU.add)
                    P.tt(base.v, base.v, bk[:, 64:128], ALU.add)
                    for cix in range(2):
                        P.tt(rt.v, Mx[cix].v, rke.v, ALU.mult)
                        P.reduce(rank[:, i, cix:cix + 1], rt.v, ALU.add)
                        P.tt(rt.v, Mx[cix].v, IOTA64, ALU.mult)
                        P.reduce(eidx[:, i, cix:cix + 1], rt.v, ALU.add)
                    if "route" in tap_d and l == 0:
                        P.copy(lg[:, 0:2], eidx[:, i, :])
                        P.copy(lg[:, 2:4], gate[:, i, :])
                        P.copy(lg[:, 4:6], rank[:, i, :])
                        tap("route", lambda hd: hd[tsl, :], lg[:, 0:6])

            if STOP in ('proj', 'sgu', 'gdn', 'rwkv', 'wout', 'A', 'x1', 'x2', 'x3', 'g1', 'g2', 'g2a', 'g2b', 'g3', 'g4', 'g5', 'g6', 'g7'):
                return
            with P.scope():
                padded = P.sb("padded", [128, 64])
                pend = P.sb("pend", [128, 64])
                pstart = P.sb("pstart", [128, 64])
                tm_ = P.sb("tm_", [128, 64])
                blke = P.sb("blke", [128, NBLK])
                dtmp = P.sb("dtmp", [128, NT * 2])
                padi = P.sb("padi", [128, 64], I32)
                P.ts(padded.v, base.v, 127.0, ALU.add)
                P.copy(padi.v, padded.v)
                P.ts(padi.v, padi.v, 7, ALU.arith_shift_right, 7, ALU.logical_shift_left)
                P.copy(padded.v, padi.v)
                P.scan(pend.v, ONES[:, 0:64], padded.v, 0.0, ALU.mult, ALU.add)
                P.tt(pstart.v, pend.v, padded.v, ALU.subtract)
                P.memset(blke.v, 0.0)
                P.copy(dest_f.v, rank.v)
                ef = eidx.v.re("p t c -> p (t c)")
                df = dest_f.v.re("p t c -> p (t c)")
                for e_ in range(NEXP):
                    P.stt(blke.v, IOTAB, pend[:, e_:e_ + 1], blke.v, ALU.is_ge, ALU.add)
                    P.stt(dtmp.v, ef, float(e_), pstart[:, e_:e_ + 1].bc([128, NT * 2]), ALU.is_equal, ALU.mult)
                    P.tt(df, df, dtmp.v, ALU.add)
                P.ts(blke.v, blke.v, 63.0, ALU.min)
                same2 = P.sb("same2", [128, NBLK])
                P.memset(same2.v, 0.0)
                P.tt(same2[:, 2:NBLK], blke[:, 2:NBLK], blke[:, 0:NBLK - 2], ALU.is_equal)
                P.ts(blke.v, blke.v, float(l * NEXP), ALU.add, 128.0, ALU.mult)
                P.ts(blke.v, blke.v, PIDX[:, 0:1], ALU.add)
                P.stt(blke.v, same2.v, 4194304.0, blke.v, ALU.mult, ALU.add)
                P.copy(idxb.v, blke.v)
                P.copy(dest_i.v, dest_f.v)
                P.copy(gtf_prev.v, modb[:, MB_GTF, :])

            if STOP == 'fin':
                return
            with P.scope():
                hbuf = [P.sb("hbuf%d" % i, [128, D]) for i in range(2)]
                for i in range(NT):
                    hb = hbuf[i % 2]
                    P.dma("sp", hb.v, hf[i * 128:(i + 1) * 128, :])
                    P.scatter(hs.v, hb.v, dest_i[:, i, 0:1])
                    P.scatter(hs.v, hb.v, dest_i[:, i, 1:2])

            if STOP == 'scatter':
                return
            with P.scope():
                wgs = [P.sb("wgs%d" % i, [128, 8, 256], BF16) for i in range(2)]
                wus = [P.sb("wus%d" % i, [128, 8, 256], BF16) for i in range(2)]
                wds = [P.sb("wds%d" % i, [128, 2, D], BF16) for i in range(2)]
                xbs = [P.sb("xbs%d" % i, [128, D], BF16) for i in range(2)]
                xbT = P.sb("xbT", [128, 8, 128], BF16)
                sg = P.sb("sg", [128, 256])
                hidT = P.sb("hidT", [128, 2, 128], BF16)
                hid = P.sb("hid", [128, 256], BF16)
                yo = [P.sb("yo%d" % i, [128, D]) for i in range(2)]
                for b in range(NBLK):
                    wg_, wu_, wd_, xb, yob = wgs[b % 2], wus[b % 2], wds[b % 2], xbs[b % 2], yo[b % 2]
                    P.dma("sp", xb.v, hs[b * 128:(b + 1) * 128, :])
                    ix = idxb[:, b:b + 1]
                    WB = L * NEXP * 128 - 1
                    P.gather(wg_.v.re("p k f -> p (k f)"), ewg_d.v, ix, bound=WB)
                    P.gather(wu_.v.re("p k f -> p (k f)"), ewu_d.v, ix, bound=WB)
                    P.gather(wd_.v.re("p j d -> p (j d)"), ewd_d.v, ix, bound=WB)
                    bk = nb()
                    bkb = bk.v.bitcast(BF16)
                    xbv = xb.v.re("p (q k) -> p k q", k=8)
                    for k in range(8):
                        P.tr(bkb[:, k * 128:(k + 1) * 128], xbv[:, k, :], identb.v)
                    P.copy(xbT.v.re("p k t -> p (k t)"), bkb[:, 0:1024], eng="act")
                    bkg_ = nb()
                    bku_ = nb()
                    for (bk_, w_) in ((bkg_, wg_), (bku_, wu_)):
                        for k in range(8):
                            P.mm(bk_[:, 0:256], xbT[:, k, :], w_[:, k, :], start=(k == 0), stop=(k == 7))
                    P.act(sg.v, bkg_[:, 0:256], AF.Silu)
                    P.tt(hid.v, sg.v, bku_[:, 0:256], ALU.mult)
                    bkh = nb()
                    bkhb = bkh.v.bitcast(BF16)
                    hv = hid.v.re("p (q j) -> p j q", j=2)
                    for j in range(2):
                        P.tr(bkhb[:, j * 128:(j + 1) * 128], hv[:, j, :], identb.v)
                    P.copy(hidT.v.re("p j t -> p (j t)"), bkhb[:, 0:256])
                    for half in range(2):
                        bk = nb()
                        for j in range(2):
                            P.mm(bk[:, :], hidT[:, j, :], wd_[:, j, half * 512:(half + 1) * 512],
                                 start=(j == 0), stop=(j == 1))
                        P.copy(yob[:, half * 512:(half + 1) * 512], bk[:, :], eng=("act" if half else "dve"))
                    P.dma("sp", ys[b * 128:(b + 1) * 128, :], yob.v)

        if STOP == 'B':
            return
        with P.scope():
            fing = P.sb("fing", [128, D])
            P.dma("sp", fing.v, fing_d.v.pbc(128))
            xf = [P.sb("xf%d" % i, [128, D]) for i in range(2)]
            yf = [P.sb("yf%d" % i, [128, D]) for i in range(2)]
            zf = [P.sb("zf%d" % i, [128, D]) for i in range(2)]
            sf = P.sb("sf", [128, 4])
            for i in range(NT):
                tsl = slice(i * 128, (i + 1) * 128)
                xt, ya, yb = xf[i % 2], yf[i % 2], zf[i % 2]
                P.dma("sp", xt.v, xres[tsl, :])
                P.gather(ya.v, ys.v, dest_i[:, i, 0:1])
                P.gather(yb.v, ys.v, dest_i[:, i, 1:2])
                P.ts(ya.v, ya.v, gate[:, i, 0:1], ALU.mult)
                P.stt(ya.v, yb.v, gate[:, i, 1:2], ya.v, ALU.mult, ALU.add)
                P.tt(ya.v, ya.v, gtf_prev.v, ALU.mult)
                P.tt(xt.v, xt.v, ya.v, ALU.add)
                if L == 1:
                    tap("x2", lambda hd: hd[tsl, :], xt.v)
                P.act(yb.v, xt.v, AF.Square, accum=sf[:, 0:1])
                rsqrt(sf[:, 1:2], sf[:, 0:1], 1e-6, 1.0 / D)
                P.stt(xt.v, xt.v, sf[:, 1:2], fing.v, ALU.mult, ALU.mult)
                P.dma("sp", out_d[tsl, :], xt.v)

    P.run(body)
    return nc, cvals, P


def prep_inputs(inp, SEQ, DEPTH, cvals):
    f = lambda a: np.ascontiguousarray(np.asarray(a, dtype=np.float32))
    L = DEPTH
    H, N, G = 4, 64, 256

    def fm(v, nch):
        return np.asarray(v).reshape(L, nch, 128).transpose(0, 2, 1)

    fmp = np.concatenate([
        fm(inp["rw_mu"][:L], 8), fm(inp["rw_w0"][:L], 2), fm(inp["rw_a0"][:L], 2), fm(inp["rw_k_k"][:L], 2),
        fm(inp["rw_k_a"][:L], 2), fm(np.asarray(inp["rw_r_k"][:L]).reshape(L, G), 2), fm(inp["rw_gn_g"][:L], 2),
        fm(inp["rw_gn_b"][:L], 2),
        np.asarray(inp["gdn_conv_w"][:L]).reshape(L, 4, 6, 128).transpose(0, 3, 2, 1).reshape(L, 128, 24),
        np.asarray(inp["sc_conv_w"][:L]).reshape(L, 3, 2, 128).transpose(0, 3, 2, 1).reshape(L, 128, 6),
    ], axis=2)
    assert fmp.shape[2] == NFM
    bcs = np.concatenate([
        inp["sgu_ln_g"][:L], inp["sgu_ln_b"][:L], np.tile(np.asarray(inp["gdn_norm_g"][:L]), (1, 4)),
        inp["gdn_a_log"][:L], inp["gdn_dt_bias"][:L], inp["moe_b_group"][:L], inp["moe_b_router"][:L]], axis=1)
    assert bcs.shape[1] == NBS
    shared = {
        "ada_w": f(inp["ada_w"][:L]), "ada_b": f(inp["ada_b"][:L]),
        "g12": f(np.concatenate([inp["mix_norm_g"][:L], inp["ffn_norm_g"][:L]], axis=1)),
        "w_in": f(inp["w_in"][:L]), "w_out": f(inp["w_out"][:L]),
        "fmp": f(fmp), "bcs": f(bcs),
        "sgu_wT": f(np.asarray(inp["sgu_w"][:L]).transpose(0, 1, 3, 2)),
        "sgu_bT": f(np.asarray(inp["sgu_b"][:L]).transpose(0, 2, 1)),
        "lora_wa": f(np.concatenate([inp["rw_w_up"][:L], inp["rw_a_up"][:L]], axis=1)),
        "g_up": f(inp["rw_g_up"][:L]),
        "wr": f(np.concatenate([inp["moe_w_group"][:L], inp["moe_w_router"][:L]], axis=2)),
        "ewg": f(np.asarray(inp["moe_w_gate"][:L]).reshape(L * NEXP * 128, 2048)),
        "ewu": f(np.asarray(inp["moe_w_up"][:L]).reshape(L * NEXP * 128, 2048)),
        "ewd": f(np.asarray(inp["moe_w_down"][:L]).reshape(L * NEXP * 128, 2048)),
        "fin_g": f(inp["final_norm_g"]),
        "consts": cvals,
    }
    x = np.asarray(inp["x"])
    c = np.asarray(inp["c"])
    per_core = []
    for b in range(x.shape[0]):
        d = dict(shared)
        d["x"] = f(x[b, :SEQ])
        d["cfm"] = f(c[b].reshape(8, 128).T)
        per_core.append(d)
    return per_core


_CACHE = {}


def kernel(**inputs):
    SEQ, DEPTH, B = 8192, 4, 8
    if "nc" not in _CACHE:
        _CACHE["nc"] = build(SEQ, DEPTH)
    nc, cvals, _ = _CACHE["nc"]
    in_maps = prep_inputs(inputs, SEQ, DEPTH, cvals)
    res = run_bass_kernel_spmd(nc, in_maps, core_ids=list(range(B)))
    return np.stack([np.asarray(r["out"], dtype=np.float32) for r in res.results], axis=0)
```

```python
import contextlib
import numpy as np
import concourse.bass as bass
import concourse.mybir as mybir

F32 = mybir.dt.float32
BF16 = mybir.dt.bfloat16
I32 = mybir.dt.int32
F32R = mybir.dt.float32r
AF = mybir.ActivationFunctionType
ALU = mybir.AluOpType
AX = mybir.AxisListType


class V:
    __slots__ = ("t", "ap")

    def __init__(self, t, ap):
        self.t = t
        self.ap = ap

    def __getitem__(self, k):
        return V(self.t, self.ap[k])

    def bc(self, shape):
        return V(self.t, self.ap.to_broadcast(list(shape)))

    def re(self, pat, **kw):
        return V(self.t, self.ap.rearrange(pat, **kw))

    def bitcast(self, dt):
        return V(self.t, self.ap.bitcast(dt))

    def pbc(self, n):
        return V(self.t, self.ap.partition_broadcast(n))

    def unsq(self, ax):
        return V(self.t, self.ap.unsqueeze(ax))


class T:
    def __init__(self, h, name):
        self.h = h
        self.name = name
        self.w = None
        self.r = {}
        self.excl = False

    def __getitem__(self, k):
        return V(self, self.h[k])

    @property
    def v(self):
        return V(self, self.h[:])


def _ap(x):
    return x.ap if isinstance(x, V) else x


def _ts(*xs):
    out = []
    for x in xs:
        if isinstance(x, V) and x.t not in out:
            out.append(x.t)
    return out


class Prog:
    ENG = ("pe", "act", "dve", "pool", "sp")

    def __init__(self, nc):
        self.nc = nc
        self.ops = {e: [] for e in self.ENG}
        self.cnt = {}
        self.seen = {e: {} for e in self.ENG}
        self.nops = 0
        self._stacks = []
        self._dslot = {}

    @contextlib.contextmanager
    def scope(self):
        st = contextlib.ExitStack()
        self._stacks.append(st)
        try:
            with st:
                yield
                self.barrier()
        finally:
            self._stacks.pop()

    def _uniq(self, name):
        self._uid = getattr(self, "_uid", 0) + 1
        return "%s_u%d" % (name, self._uid)

    def sb(self, name, shape, dt=F32):
        name = self._uniq(name)
        h = self._stacks[-1].enter_context(self.nc.sbuf_tensor(name, list(shape), dt))
        return T(h, name)

    def ps(self, name, shape, dt=F32):
        name = self._uniq(name)
        h = self._stacks[-1].enter_context(self.nc.psum_tensor(name, list(shape), dt))
        t = T(h, name)
        t.excl = True
        return t

    def dram(self, name, shape, dt=F32, kind="Internal"):
        name = self._uniq(name)
        return T(self.nc.dram_tensor(name, list(shape), dt, kind=kind), name)

    def _emit(self, eng, key, inc, fn, reads, writes):
        waits = {}

        def need(dep):
            if dep is None:
                return
            k, c = dep
            if k == eng and eng == "pe":
                return
            if self.seen[eng].get(k, 0) >= c:
                return
            if waits.get(k, 0) < c:
                waits[k] = c

        if key != eng and self.cnt.get(key, 0) > 0:
            need((key, self.cnt[key]))
        for b in reads:
            need(b.w)
            if b.excl:
                for k, c in b.r.items():
                    if k != key:
                        need((k, c))
        for b in writes:
            need(b.w)
            for k, c in b.r.items():
                need((k, c))
        for k, c in waits.items():
            self.seen[eng][k] = c
        self.cnt[key] = self.cnt.get(key, 0) + 1
        my = self.cnt[key]
        self.ops[eng].append((fn, sorted(waits.items()), key, inc))
        for b in writes:
            b.w = (key, my)
            b.r = {}
        for b in reads:
            if b not in writes:
                b.r[key] = my
        self.nops += 1

    def barrier(self):
        snap = dict(self.cnt)
        for eng in self.ENG:
            waits = {}
            for k, c in snap.items():
                if self.seen[eng].get(k, 0) < c:
                    waits[k] = c
                    self.seen[eng][k] = c
            if waits:
                self.ops[eng].append((None, sorted(waits.items()), None, 0))

    def op(self, eng, fn, reads=(), writes=()):
        self._emit(eng, eng, 1, fn, list(reads), list(writes))

    NSLOT = 20

    def _dkey(self, q):
        n = self._dslot.get(q, 0)
        self._dslot[q] = n + 1
        return "dma_%s_%d" % (q, n % self.NSLOT)

    def dma(self, q, out, in_, extra_reads=(), **kw):
        key = self._dkey(q)
        o, i = _ap(out), _ap(in_)
        self._emit(q, key, 16, lambda e: e.dma_start(out=o, in_=i, **kw),
                   _ts(in_) + list(extra_reads), _ts(out))

    def gather(self, out, in_, idx, bound=None):
        o, i, ix = _ap(out), _ap(in_), _ap(idx)
        cache = self.__dict__.setdefault("_regcache", {})

        def fn(e):
            kw = {}
            if bound is not None:
                if bound not in cache:
                    cache[bound] = e.to_reg(bound)
                kw = dict(bounds_check=cache[bound], oob_is_err=False)
            return e.indirect_dma_start(out=o, out_offset=None, in_=i,
                                        in_offset=bass.IndirectOffsetOnAxis(ap=ix, axis=0), **kw)
        self._emit("pool", self._dkey("pool"), 16, fn,
                   _ts(in_, idx) + (_ts(out) if bound is not None else []), _ts(out))

    def scatter(self, out, in_, idx):
        o, i, ix = _ap(out), _ap(in_), _ap(idx)
        self._emit("pool", self._dkey("pool"), 16,
                   lambda e: e.indirect_dma_start(out=o, out_offset=bass.IndirectOffsetOnAxis(ap=ix, axis=0),
                                                  in_=i, in_offset=None),
                   _ts(in_, idx), _ts(out))

    def mm(self, out, lhsT, rhs, start=True, stop=True):
        o, l, r = _ap(out), _ap(lhsT), _ap(rhs)
        self.op("pe", lambda e: e.matmul(o, l, r, start=start, stop=stop),
                reads=_ts(lhsT, rhs), writes=_ts(out))

    def mmr(self, out, lhsT, rhs, start=True, stop=True):
        self.mm(out, lhsT.bitcast(F32R), rhs.bitcast(F32R), start=start, stop=stop)

    def tr(self, out, in_, ident):
        o, i, d = _ap(out), _ap(in_), _ap(ident)
        self.op("pe", lambda e: e.transpose(o, i, d), reads=_ts(in_, ident), writes=_ts(out))

    def tt(self, out, a, b, op, eng="dve"):
        o, x, y = _ap(out), _ap(a), _ap(b)
        self.op(eng, lambda e: e.tensor_tensor(out=o, in0=x, in1=y, op=op),
                reads=_ts(a, b), writes=_ts(out))

    def ts(self, out, a, s1, op0, s2=None, op1=None, eng="dve", accum=None):
        o, x, p1, p2, ac = _ap(out), _ap(a), _ap(s1), _ap(s2), _ap(accum)
        kw = {}
        if op1 is not None:
            kw["op1"] = op1
        if accum is not None:
            kw["accum_out"] = ac
        self.op(eng, lambda e: e.tensor_scalar(out=o, in0=x, scalar1=p1, scalar2=p2, op0=op0, **kw),
                reads=_ts(a, s1, s2), writes=_ts(out, accum))

    def stt(self, out, a, s, b, op0, op1, eng="dve"):
        o, x, p, y = _ap(out), _ap(a), _ap(s), _ap(b)
        self.op(eng, lambda e: e.scalar_tensor_tensor(out=o, in0=x, scalar=p, in1=y, op0=op0, op1=op1),
                reads=_ts(a, s, b), writes=_ts(out))

    def act(self, out, a, func, bias=None, scale=None, accum=None):
        o, x, b, s, ac = _ap(out), _ap(a), _ap(bias), _ap(scale), _ap(accum)
        kw = {}
        if bias is not None:
            kw["bias"] = b
        if scale is not None:
            kw["scale"] = s
        if accum is not None:
            kw["accum_out"] = ac
        self.op("act", lambda e: e.activation(out=o, in_=x, func=func, **kw),
                reads=_ts(a, bias, scale), writes=_ts(out, accum))

    def copy(self, out, a, eng="dve"):
        o, x = _ap(out), _ap(a)
        if eng == "act":
            self.op("act", lambda e: e.activation(out=o, in_=x, func=AF.Copy), reads=_ts(a), writes=_ts(out))
        else:
            self.op(eng, lambda e: e.tensor_copy(out=o, in_=x), reads=_ts(a), writes=_ts(out))

    def memset(self, out, val, eng="dve"):
        o = _ap(out)
        self.op(eng, lambda e: e.memset(o, val), writes=_ts(out))

    def reduce(self, out, a, op, axis=None, eng="dve"):
        o, x = _ap(out), _ap(a)
        ax = axis if axis is not None else AX.X
        self.op(eng, lambda e: e.tensor_reduce(out=o, in_=x, axis=ax, op=op), reads=_ts(a), writes=_ts(out))

    def scan(self, out, d0, d1, init, op0, op1):
        o, x, y, i = _ap(out), _ap(d0), _ap(d1), _ap(init)
        self.op("dve", lambda e: e.tensor_tensor_scan(out=o, data0=x, data1=y, initial=i, op0=op0, op1=op1),
                reads=_ts(d0, d1, init), writes=_ts(out))

    def run(self, body):
        nc = self.nc
        with contextlib.ExitStack() as st:
            self._stacks.append(st)
            body(self)
            self.barrier()
            keys = sorted(self.cnt.keys())
            sems = {k: st.enter_context(nc.semaphore("s_" + k)) for k in keys}
            blk = st.enter_context(nc.Block())
            mult = {k: (16 if k.startswith("dma_") else 1) for k in keys}

            def replay(engname):
                def f(e):
                    for fn, waits, key, inc in self.ops[engname]:
                        for k, c in waits:
                            e.wait_ge(sems[k], c * mult[k])
                        if fn is not None:
                            fn(e).then_inc(sems[key], inc)
                return f

            blk.tensor(replay("pe"))
            blk.scalar(replay("act"))
            blk.vector(replay("dve"))
            blk.gpsimd(replay("pool"))
            blk.sync(replay("sp"))
        return nc
from concourse.bass_utils import run_bass_kernel_spmd

D = 1024
INW = 3336
NEXP = 64
NEG = -1.0e30
C_Z, C_A, C_B, C_SU, C_SV = 0, 256, 260, 264, 520
NPT = 776
FM_MU, FM_W0, FM_A0, FM_KK, FM_KA, FM_RK, FM_GNG, FM_GNB, FM_CW, FM_SC = 0, 8, 10, 12, 14, 16, 18, 20, 22, 46
NFM = 52
BS_LNG, BS_LNB, BS_GN, BS_ALOG, BS_DT, BS_RB = 0, 256, 512, 768, 772, 776
NBS = 848
GELU_C = 1.5957691216057308


def make_consts(nblk):
    p = np.arange(128)
    blk = p // 64
    same = blk[:, None] == blk[None, :]
    i = p[:, None]
    j = p[None, :]
    parts = {}
    parts["ident"] = np.eye(128)
    parts["BD"] = same
    parts["UBLK"] = same & (i <= j)
    parts["ones"] = np.ones((128, 128))
    parts["NSL"] = -(same & (i > j)).astype(np.float64)
    parts["SL"] = same & (i > j)
    parts["SU"] = same & (i < j)
    parts["IU"] = same & (i <= j)
    parts["NEGL"] = np.where(same & (i >= j), 0.0, NEG)
    parts["NEGU"] = np.where(same & (i <= j), 0.0, NEG)
    parts["CHI"] = (blk[:, None] == np.arange(2)[None, :])
    parts["SU128"] = (i < j)
    parts["IU128"] = (i <= j)
    parts["iota64"] = np.tile(np.arange(64)[None, :], (128, 1))
    parts["pidx"] = p[:, None]
    parts["iotaB"] = np.tile((np.arange(nblk) * 128)[None, :], (128, 1))
    offs = {}
    cols = []
    o = 0
    for k, v in parts.items():
        v = np.asarray(v, dtype=np.float32)
        offs[k] = (o, o + v.shape[1])
        cols.append(v)
        o += v.shape[1]
    return np.ascontiguousarray(np.concatenate(cols, axis=1)), offs


TAPSHAPES = lambda SEQ, NT: {"p_tm": [SEQ, NPT], "oT": [NT, 128, 8 * 128], "x1": [SEQ, D], "h2": [SEQ, D],
                             "route": [SEQ, 6], "x2": [SEQ, D]}


def build(SEQ, DEPTH, taps=()):
    import os
    STOP = os.environ.get('KSTOP', '')
    NT = SEQ // 128
    PROWS = 2 * SEQ + NEXP * 128
    NBLK = PROWS // 128
    cvals, coffs = make_consts(NBLK)
    NCONST = cvals.shape[1]
    L = DEPTH
    nc = bass.Bass("TRN2", target_bir_lowering=False)

    def din(name, shape, dt=F32):
        return T(nc.dram_tensor(name, list(shape), dt, kind="ExternalInput"), name)

    x_d = din("x", [SEQ, D])
    cfm_d = din("cfm", [128, 8])
    adaw_d = din("ada_w", [L, D, 6 * D])
    adab_d = din("ada_b", [L, 6 * D])
    g12_d = din("g12", [L, 2 * D])
    win_d = din("w_in", [L, D, INW])
    wout_d = din("w_out", [L, D, D])
    fmp_d = din("fmp", [L, 128, NFM])
    bcs_d = din("bcs", [L, NBS])
    swT_d = din("sgu_wT", [L, 4, 128, 128])
    sbT_d = din("sgu_bT", [L, 128, 4])
    lwa_d = din("lora_wa", [L, 128, 256])
    gup_d = din("g_up", [L, 128, 256])
    wr_d = din("wr", [L, D, 72])
    ewg_d = din("ewg", [L * NEXP * 128, 2048])
    ewu_d = din("ewu", [L * NEXP * 128, 2048])
    ewd_d = din("ewd", [L * NEXP * 128, 2048])
    fing_d = din("fin_g", [D])
    con_d = din("consts", [128, NCONST])
    out_d = T(nc.dram_tensor("out", [SEQ, D], F32, kind="ExternalOutput"), "out")
    tap_d = {}
    for tname in taps:
        tap_d[tname] = T(nc.dram_tensor("tap_" + tname, TAPSHAPES(SEQ, NT)[tname], F32, kind="ExternalOutput"), tname)

    P = Prog(nc)

    def body(P):
        xres = P.dram("xres", [SEQ, D])
        hf = P.dram("hf", [SEQ, D])
        hs = P.dram("hs", [PROWS, D], BF16)
        ys = P.dram("ys", [PROWS, D])

        con = P.sb("con", [128, NCONST])
        P.dma("sp", con.v, con_d.v)

        def K(name):
            a, b = coffs[name]
            return con[:, a:b]

        ident, BDm, UBLK, ONES = K("ident"), K("BD"), K("UBLK"), K("ones")
        NSL, SLm, SUm, IUm, NEGL, NEGU = K("NSL"), K("SL"), K("SU"), K("IU"), K("NEGL"), K("NEGU")
        CHI, SU128, IU128, IOTA64, PIDX, IOTAB = K("CHI"), K("SU128"), K("IU128"), K("iota64"), K("pidx"), K("iotaB")
        identb = P.sb("identb", [128, 128], BF16)
        P.copy(identb.v, ident)

        banks = [P.ps("bank%d" % i, [128, 512]) for i in range(8)]
        bstate = [0]

        def nb():
            b = banks[bstate[0] % 8]
            bstate[0] += 1
            return b

        with P.scope():
            zt = P.sb("zt", [128, D], BF16)
            P.memset(zt.v, 0.0)
            for b in range(NBLK):
                P.dma("sp", hs[b * 128:(b + 1) * 128, :], zt.v)

        cact = P.sb("cact", [128, 8])
        P.dma("sp", cact.v, cfm_d.v)
        P.act(cact.v, cact.v, AF.Silu)

        eidx = P.sb("eidx", [128, NT, 2])
        gate = P.sb("gate", [128, NT, 2])
        rank = P.sb("rank", [128, NT, 2])
        dest_f = P.sb("dest_f", [128, NT, 2])
        dest_i = P.sb("dest_i", [128, NT, 2], I32)
        base = P.sb("base", [128, 64])
        idxb = P.sb("idxb", [128, NBLK], I32)
        modb = P.sb("modb", [128, 4, D])
        MB_GTM, MB_SHF, MB_G2, MB_GTF = 0, 1, 2, 3
        mfm = P.sb("mfm", [128, 16])
        gtf_prev = P.sb("gtf_prev", [128, D])

        def recip(out, in_):
            o, i_ = out.ap, in_.ap
            P.op("dve", lambda e: e.reciprocal(out=o, in_=i_), reads=_ts(in_), writes=_ts(out))

        def rsqrt(out, in_, eps, scale=1.0):
            P.act(out, in_, AF.Ln, bias=eps, scale=scale)
            P.act(out, out, AF.Exp, scale=-0.5)

        def tap(name, dst_fn, src):
            if name in tap_d:
                P.dma("sp", V(tap_d[name], dst_fn(tap_d[name].h)), src)

        def v3(t):
            return t.v.re("p (c t) -> p c t", c=2)

        def h4(v):
            return v.re("p (h c) -> p h c", h=4)

        def bc4(v):
            return v.unsq(2).bc([128, 4, 64])

        for l in range(L):
            with P.scope():
                pan = [P.sb("pan%d" % i, [128, 8, 512]) for i in range(2)]
                adab = P.sb("adab", [128, 6 * D])
                g12 = P.sb("g12", [128, 2 * D])
                crep = P.sb("crep", [128, 8, 128])
                mtmp = P.sb("mtmp", [128, 2, D])
                for k in range(8):
                    P.ts(crep[:, k, :], ONES, cact[:, k:k + 1], ALU.mult)
                P.dma("sp", adab.v, adab_d[l, :].pbc(128))
                P.dma("sp", g12.v, g12_d[l, :].pbc(128))
                awv = adaw_d[l].re("(k p) n -> p k n", p=128)
                dst = {0: mtmp[:, 0, :], 1: mtmp[:, 1, :], 2: modb[:, MB_GTM, :], 3: modb[:, MB_SHF, :],
                       4: modb[:, MB_G2, :], 5: modb[:, MB_GTF, :]}
                for n in range(12):
                    pn = pan[n % 2]
                    P.dma("sp", pn.v, awv[:, :, n * 512:(n + 1) * 512])
                    bk = nb()
                    for k in range(8):
                        P.mm(bk[:, :], crep[:, k, :], pn[:, k, :], start=(k == 0), stop=(k == 7))
                    seg, half = n // 2, n % 2
                    P.tt(dst[seg][:, half * 512:(half + 1) * 512], bk[:, :], adab[:, n * 512:(n + 1) * 512], ALU.add)
                P.stt(mtmp[:, 1, :], mtmp[:, 1, :], 1.0, g12[:, 0:D], ALU.add, ALU.mult)
                P.stt(modb[:, MB_G2, :], modb[:, MB_G2, :], 1.0, g12[:, D:2 * D], ALU.add, ALU.mult)
                for (which, col0) in ((1, 0), (0, 8)):
                    for half in range(2):
                        bk = nb()
                        for c4 in range(4):
                            c = half * 4 + c4
                            P.tr(bk[:, c4 * 128:(c4 + 1) * 128], mtmp[:, which, c * 128:(c + 1) * 128], ident)
                        P.copy(mfm[:, col0 + half * 4:col0 + half * 4 + 4], bk[:, :].re("p (c t) -> p c t", c=4)[:, :, 0])

            if STOP == 'mod':
                return
            with P.scope():
                w_in = P.sb("w_in", [128, 8, INW], BF16)
                w_out = P.sb("w_out", [128, 8, D], BF16)
                wiv = win_d[l].re("(k p) n -> p k n", p=128)
                wov = wout_d[l].re("(k p) n -> p k n", p=128)
                for k in range(8):
                    P.dma("pool", w_in[:, k, 0:1668], wiv[:, k, 0:1668])
                    P.dma("pool", w_in[:, k, 1668:INW], wiv[:, k, 1668:INW])
                    P.dma("pool", w_out[:, k, :], wov[:, k, :])
                fmp = P.sb("fmp", [128, NFM])
                bcs = P.sb("bcs", [128, NBS])
                P.dma("sp", fmp.v, fmp_d[l])
                P.dma("sp", bcs.v, bcs_d[l, :].pbc(128))
                nexpA = P.sb("nexpA", [128, 4])
                P.act(nexpA.v, bcs[:, BS_ALOG:BS_ALOG + 4], AF.Exp)
                P.ts(nexpA.v, nexpA.v, -1.0, ALU.mult)
                wsT = P.sb("wsT", [128, 4, 128])
                P.dma("sp", wsT.v, swT_d[l].re("h s t -> s h t"))
                for h in range(4):
                    P.tt(wsT[:, h, :], wsT[:, h, :], IU128, ALU.mult)
                bsT = P.sb("bsT", [128, 4])
                P.dma("sp", bsT.v, sbT_d[l])
                lwW = P.sb("lwW", [128, 256])
                lwA = P.sb("lwA", [128, 256])
                gup = P.sb("gup", [128, 256])
                P.memset(lwW.v, 0.0)
                P.memset(lwA.v, 0.0)
                P.dma("sp", lwW[0:64, :], lwa_d[l, 0:64, :])
                P.dma("sp", lwA[64:128, :], lwa_d[l, 64:128, :])
                P.dma("sp", gup.v, gup_d[l])
                wr = P.sb("wr", [128, 8, 72])
                P.dma("sp", wr.v, wr_d[l].re("(k p) n -> p k n", p=128))

                Sg = P.sb("Sg", [128, 2, 128])
                Sr = P.sb("Sr", [128, 2, 128])
                qkv_raw = P.sb("qkv_raw", [128, 6, 131])
                prodC = P.sb("prodC", [128, 2, 130])
                rw_raw = P.sb("rw_raw", [128, 8, 129])
                for t_ in (Sg, Sr, qkv_raw, prodC, rw_raw, base):
                    P.memset(t_.v, 0.0)

                xt = P.sb("xt", [128, D])
                hh = P.sb("hh", [128, D])
                hT = P.sb("hT", [128, 8, 128], BF16)
                h2T = P.sb("h2T", [128, 8, 128])
                h2f = h2T.v.re("p k t -> p (k t)")
                p_tm = P.sb("p_tm", [128, NPT])
                cacc = P.sb("cacc", [128, 6, 128])
                cbch = P.sb("cbch", [128, 6, 128])
                rw = P.sb("rw", [128, 8, 128])
                oT = P.sb("oT", [128, 8, 128], BF16)
                oTf = P.sb("oTf", [128, 8, 128]) if "oT" in tap_d else None
                s1 = P.sb("s1", [128, 8])
                qk = P.sb("qk", [128, 4, 128])
                sq4 = P.sb("sq4", [128, 4, 128])
                kv_tm = P.sb("kv_tm", [128, 512])
                g4 = P.sb("g4", [128, 4])
                t4 = P.sb("t4", [128, 4])
                beta4 = P.sb("beta4", [128, 4])
                gm = P.sb("gm", [128, 2, 4])
                gc = P.sb("gc", [128, 4])
                ngc = P.sb("ngc", [128, 4])
                glr = P.sb("glr", [128, 8])
                glt = P.sb("glt", [128, 4])
                egc = P.sb("egc", [128, 4])
                ktf = P.sb("ktf", [128, 4])
                cdg = P.sb("cdg", [128, 8])
                bg = P.sb("bg", [128, 4])
                gcrow = P.sb("gcrow", [128, 4, 128])
                egcrow = P.sb("egcrow", [128, 4, 128])
                qdT = P.sb("qdT", [128, 256])
                vb = P.sb("vb", [128, 256])
                kbg = P.sb("kbg", [128, 256])
                ktail = P.sb("ktail", [128, 256])
                stmp = P.sb("stmp", [128, 128])
                cdp = P.sb("cdp", [128, 4])
                u_sb = P.sb("u_sb", [128, 256])
                wT_sb = P.sb("wT_sb", [128, 256])
                vnew = P.sb("vnew", [128, 256])
                o_sb = P.sb("o_sb", [128, 256])
                o_sq = P.sb("o_sq", [128, 256])
                sz = P.sb("sz", [128, 256])
                DD = [dict(tmp=P.sb("dd_tmp%d" % i, [128, 128]), D=P.sb("dd_D%d" % i, [128, 128]),
                           DT=P.sb("dd_DT%d" % i, [128, 128])) for i in range(2)]
                HM = []
                for h in range(4):
                    HM.append(dict(M1=P.sb("hm_M1%d" % h, [128, 128]), M2=P.sb("hm_M2%d" % h, [128, 128]),
                                   M3=P.sb("hm_M3%d" % h, [128, 128]), PQ=P.sb("hm_PQ%d" % h, [128, 2, 128]),
                                   R=P.sb("hm_R%d" % h, [128, 128])))
                lg = P.sb("lg", [128, 72])
                r8 = P.sb("r8", [128, 8, 8])
                sel = P.sb("sel", [128, 8])
                ohg = P.sb("ohg", [128, 8])
                oh1 = P.sb("oh1", [128, 8])
                oh2 = P.sb("oh2", [128, 8])
                Mx = [P.sb("Mx%d" % i, [128, 64]) for i in range(3)]
                rke = P.sb("rke", [128, 64])
                rt = P.sb("rt", [128, 64])
                sc = P.sb("sc", [128, 8])
                for t_ in (vnew, o_sq, o_sb, sz):
                    P.memset(t_.v, 0.0)

                kk_t = P.sb("kk_t", [128, 4, 128])
                kkT, kmT = kk_t[:, 0:2, :], kk_t[:, 2:4, :]
                cum, eW = gcrow[:, 0:2, :], gcrow[:, 2:4, :]
                eWi, eWp = egcrow[:, 0:2, :], egcrow[:, 2:4, :]
                ab_t = P.sb("ab_t", [128, 8, 128])
                AtT, BtT, KtT, RtT = ab_t[:, 0:2, :], ab_t[:, 2:4, :], ab_t[:, 4:6, :], ab_t[:, 6:8, :]
                lz_t = P.sb("lz_t", [128, 256]); asg_t = P.sb("asg_t", [128, 256])
                gateT_t = P.sb("gateT_t", [128, 256]); lact_t = P.sb("lact_t", [128, 256])
                lz, asg, gateT, lact = v3(lz_t), v3(asg_t), v3(gateT_t), v3(lact_t)
                rkb, ynT = v3(u_sb), v3(wT_sb)
                Z_sb, U_sb, y_sb, y_sq = vnew, o_sq, o_sb, sz
                bkv_tm = cacc.v.re("p (q x) t -> p q (x t)", q=3)
                ugl, vgl, ob = o_sb, o_sq, u_sb
                wT3 = v3(wT_sb)
                qd3 = v3(qdT)

                def neumann():
                    for lvl in range(1, 6):
                        bks = []
                        for h in range(4):
                            hm = HM[h]
                            bk = nb()
                            Pk, Qk = hm["PQ"][:, 0, :], hm["PQ"][:, 1, :]
                            P.mmr(bk[:, 0:128], Qk, Pk)
                            if lvl < 5:
                                P.mmr(bk[:, 128:256], Pk, Qk)
                            bks.append(bk)
                        for h in range(4):
                            n = 256 if lvl < 5 else 128
                            P.copy(HM[h]["PQ"].v.re("p a t -> p (a t)")[:, 0:n].bitcast(F32R), bks[h][:, 0:n], eng="act")
                        for h in range(4):
                            P.mmr(bks[h][:, 256:384], HM[h]["PQ"][:, 0, :], HM[h]["R"].v)
                        for h in range(4):
                            P.tt(HM[h]["R"].v.bitcast(F32R), bks[h][:, 256:384], HM[h]["R"].v, ALU.add)

                def rstd_of(dst, src, eps):
                    P.act(h2f, src, AF.Square, accum=s1[:, 0:1])
                    rsqrt(dst, s1[:, 0:1], eps, 1.0 / D)

                for i in range(NT):
                    tsl = slice(i * 128, (i + 1) * 128)
                    if l == 0:
                        P.dma("sp", xt.v, x_d[tsl, :])
                    else:
                        P.dma("sp", xt.v, xres[tsl, :])
                        P.gather(hh.v, ys.v, dest_i[:, i, 0:1])
                        P.gather(h2f, ys.v, dest_i[:, i, 1:2])
                        P.ts(hh.v, hh.v, gate[:, i, 0:1], ALU.mult)
                        P.stt(hh.v, h2f, gate[:, i, 1:2], hh.v, ALU.mult, ALU.add)
                        P.tt(hh.v, hh.v, gtf_prev.v, ALU.mult)
                        P.tt(xt.v, xt.v, hh.v, ALU.add)
                        if l == 1:
                            tap("x2", lambda hd: hd[tsl, :], xt.v)
                    rstd_of(s1[:, 1:2], xt.v, 1e-6)
                    P.ts(hh.v, xt.v, s1[:, 1:2], ALU.mult)
                    for half in range(2):
                        bk = nb()
                        for c4 in range(4):
                            c = half * 4 + c4
                            P.tr(bk[:, c4 * 128:(c4 + 1) * 128], hh[:, c * 128:(c + 1) * 128], ident)
                        for c4 in range(4):
                            c = half * 4 + c4
                            P.act(hT[:, c, :], bk[:, c4 * 128:(c4 + 1) * 128], AF.Identity,
                                  bias=mfm[:, 8 + c:9 + c], scale=mfm[:, c:c + 1])
                    for (c0, c1) in ((768, 1280), (1280, 1544)):
                        bk = nb()
                        for k in range(8):
                            P.mm(bk[:, 0:c1 - c0], hT[:, k, :], w_in[:, k, c0:c1], start=(k == 0), stop=(k == 7))
                        P.copy(p_tm[:, c0 - 768:c1 - 768], bk[:, 0:c1 - c0], eng="act")
                    tap("p_tm", lambda hd: hd[tsl, :], p_tm.v)

                    def fm_group(col0, nch, dst_fn):
                        for g0 in range(0, nch, 4):
                            n = min(4, nch - g0)
                            bk = nb()
                            for cc in range(n):
                                cs_ = col0 + (g0 + cc) * 128
                                for k in range(8):
                                    P.mm(bk[:, cc * 128:(cc + 1) * 128], w_in[:, k, cs_:cs_ + 128], hT[:, k, :],
                                         start=(k == 0), stop=(k == 7))
                            P.copy(dst_fn(g0, n), bk[:, 0:n * 128].re("p (c t) -> p c t", c=n), eng="act")
                    fm_group(0, 6, lambda g0, n: qkv_raw[:, g0:g0 + n, 3:131])
                    fm_group(1544, 6, lambda g0, n: cbch[:, g0:g0 + n, :])
                    fm_group(2312, 8, lambda g0, n: rw_raw[:, g0:g0 + n, 1:129])

                    if STOP == 'proj':
                        continue
                    P.tt(prodC[:, :, 2:130], cbch[:, 2:4, :], cbch[:, 4:6, :], ALU.mult)
                    for c in range(2):
                        w = lambda k: fmp[:, FM_SC + c * 3 + k:FM_SC + c * 3 + k + 1]
                        P.ts(cacc[:, c, :], prodC[:, c, 2:130], w(2), ALU.mult)
                        P.stt(cacc[:, c, :], prodC[:, c, 1:129], w(1), cacc[:, c, :], ALU.mult, ALU.add)
                        P.stt(cacc[:, c, :], prodC[:, c, 0:128], w(0), cacc[:, c, :], ALU.mult, ALU.add)
                    P.tt(oT[:, 4:6, :], cacc[:, 0:2, :], cbch[:, 0:2, :], ALU.mult)
                    if oTf is not None:
                        P.tt(oTf[:, 4:6, :], cacc[:, 0:2, :], cbch[:, 0:2, :], ALU.mult)
                    P.copy(prodC[:, :, 0:2], prodC[:, :, 128:130])

                    suv = p_tm[:, C_SU:C_SU + 512]
                    gtmp = kv_tm.v
                    P.tt(gtmp, suv, suv, ALU.mult)
                    P.ts(gtmp, gtmp, 0.044715, ALU.mult, 1.0, ALU.add)
                    P.tt(gtmp, gtmp, suv, ALU.mult)
                    P.act(gtmp, gtmp, AF.Sigmoid, scale=GELU_C)
                    P.tt(ugl.v, gtmp[:, 0:256], suv[:, 0:256], ALU.mult)
                    P.tt(vgl.v, gtmp[:, 256:512], suv[:, 256:512], ALU.mult)
                    for c in range(6):
                        w = lambda k: fmp[:, FM_CW + c * 4 + k:FM_CW + c * 4 + k + 1]
                        P.ts(cacc[:, c, :], qkv_raw[:, c, 3:131], w(3), ALU.mult)
                        for k in (2, 1, 0):
                            P.stt(cacc[:, c, :], qkv_raw[:, c, k:k + 128], w(k), cacc[:, c, :], ALU.mult, ALU.add)
                    P.act(cacc.v, cacc.v, AF.Silu)
                    qkv = cacc
                    P.copy(qkv_raw[:, :, 0:3], qkv_raw[:, :, 128:131])
                    P.act(sz.v, p_tm[:, C_Z:C_Z + 256], AF.Silu)
                    P.tt(rw.v, rw_raw[:, :, 0:128], rw_raw[:, :, 1:129], ALU.subtract)
                    for c in range(8):
                        P.stt(rw[:, c, :], rw[:, c, :], fmp[:, FM_MU + c:FM_MU + c + 1], rw_raw[:, c, 1:129],
                              ALU.mult, ALU.add)
                    P.copy(rw_raw[:, :, 0:1], rw_raw[:, :, 128:129])
                    P.act(lact[0:64, 0, :], rw[0:64, 6, :], AF.Tanh)
                    P.copy(lact[64:128, 0, :], rw[64:128, 6, :])
                    P.act(lact[:, 1, :], rw[:, 7, :], AF.Sigmoid)
                    bk = nb()
                    bkg = nb()
                    for c in range(2):
                        ch = slice(c * 128, (c + 1) * 128)
                        P.mm(bk[:, c * 128:(c + 1) * 128], lwW[:, ch], lact[:, 0, :])
                        P.mm(bk[:, 256 + c * 128:256 + (c + 1) * 128], lwA[:, ch], lact[:, 0, :])
                        P.mm(bkg[:, c * 128:(c + 1) * 128], gup[:, ch], lact[:, 1, :])
                    for c in range(2):
                        P.act(lz[:, c, :], bk[:, c * 128:(c + 1) * 128], AF.Sigmoid,
                              bias=fmp[:, FM_W0 + c:FM_W0 + c + 1])
                        P.act(asg[:, c, :], bk[:, 256 + c * 128:256 + (c + 1) * 128], AF.Sigmoid,
                              bias=fmp[:, FM_A0 + c:FM_A0 + c + 1])
                    P.ts(lz, lz, -0.6065306597126334, ALU.mult)
                    P.copy(gateT, bkg[:, 0:256].re("p (c t) -> p c t", c=2), eng="act")
                    P.reduce(s1[:, 2:3], vgl.v, ALU.add)
                    P.ts(s1[:, 3:4], s1[:, 2:3], -1.0 / 256, ALU.mult)
                    P.ts(vgl.v, vgl.v, s1[:, 3:4], ALU.add)
                    P.act(gtmp[:, 0:256], vgl.v, AF.Square, accum=s1[:, 4:5])
                    rsqrt(s1[:, 5:6], s1[:, 4:5], 1e-5, 1.0 / 256)
                    P.stt(vgl.v, vgl.v, s1[:, 5:6], bcs[:, BS_LNG:BS_LNG + 256], ALU.mult, ALU.mult)
                    P.tt(vgl.v, vgl.v, bcs[:, BS_LNB:BS_LNB + 256], ALU.add)
                    bk = nb()
                    for h in range(4):
                        P.mm(bk[:, h * 64:(h + 1) * 64], wsT[:, h, :], vgl[:, h * 64:(h + 1) * 64])
                    P.tt(h4(ob.v), h4(bk[:, 0:256]), bc4(bsT.v), ALU.add)
                    P.tt(ob.v, ob.v, ugl.v, ALU.mult)
                    bk = nb()
                    for c in range(2):
                        P.tr(bk[:, c * 128:(c + 1) * 128], ob[:, c * 128:(c + 1) * 128], ident)
                    P.copy(oT[:, 2:4, :], bk[:, 0:256].re("p (c t) -> p c t", c=2))
                    if oTf is not None:
                        P.copy(oTf[:, 2:4, :], bk[:, 0:256].re("p (c t) -> p c t", c=2))

                    if STOP == 'sgu':
                        continue
                    P.tt(sq4.v, qkv[:, 0:4, :], qkv[:, 0:4, :], ALU.mult)
                    bk = nb()
                    for c in range(4):
                        P.mm(bk[:, c * 128:(c + 1) * 128], BDm, sq4[:, c, :])
                    rsqrt(sq4.v.re("p c t -> p (c t)"), bk[:, :], 1e-6)
                    P.stt(qk[:, 0:2, :].bitcast(F32R), qkv[:, 0:2, :], 0.125, sq4[:, 0:2, :], ALU.mult, ALU.mult)
                    P.tt(qk[:, 2:4, :].bitcast(F32R), qkv[:, 2:4, :], sq4[:, 2:4, :], ALU.mult)
                    bk = nb()
                    P.tr(bk[:, 0:128], qk[:, 2, :], ident)
                    P.tr(bk[:, 128:256], qk[:, 3, :], ident)
                    P.tr(bk[:, 256:384], qkv[:, 4, :], ident)
                    P.tr(bk[:, 384:512], qkv[:, 5, :], ident)
                    P.copy(kv_tm.v, bk[:, :], eng="act")
                    if STOP == 'g1':
                        continue
                    P.tt(t4.v, p_tm[:, C_A:C_A + 4], bcs[:, BS_DT:BS_DT + 4], ALU.add)
                    P.act(t4.v, t4.v, AF.Exp)
                    P.act(t4.v, t4.v, AF.Ln, bias=1.0)
                    P.tt(g4.v, t4.v, nexpA.v, ALU.mult)
                    P.act(beta4.v, p_tm[:, C_B:C_B + 4], AF.Exp, scale=-1.0)
                    P.ts(beta4.v, beta4.v, 1.0, ALU.add)
                    recip(beta4.v, beta4.v)
                    for c in range(2):
                        P.ts(gm[:, c, :], g4.v, CHI[:, c:c + 1], ALU.mult)
                    bk = nb()
                    P.mm(bk[:, 0:4], UBLK, g4.v)
                    P.mm(bk[:, 4:12], ONES, gm.v.re("p c h -> p (c h)"))
                    P.copy(gc.v, bk[:, 0:4])
                    P.copy(glr.v, bk[:, 4:12])
                    P.ts(ngc.v, gc.v, -1.0, ALU.mult)
                    P.ts(glt.v, glr[:, 0:4], CHI[:, 0:1], ALU.mult)
                    P.stt(glt.v, glr[:, 4:8], CHI[:, 1:2], glt.v, ALU.mult, ALU.add)
                    P.act(egc.v, gc.v, AF.Exp)
                    P.tt(t4.v, glt.v, gc.v, ALU.subtract)
                    P.act(ktf.v, t4.v, AF.Exp)
                    bkc_ = nb()
                    P.mm(bkc_[0:64, 0:4], ONES[:, 0:64], gm.v[:, :, 0::2])
                    P.mm(bkc_[64:128, 0:4], ONES[:, 0:64], gm.v[:, :, 1::2])
                    P.act(cdp.v, bkc_[:, 0:4], AF.Exp)
                    P.tt(bg.v, beta4.v, egc.v, ALU.mult)
                    if STOP == 'g2':
                        continue
                    G4 = sq4
                    for h in range(4):
                        P.ts(G4[:, h, :], UBLK, g4[:, h:h + 1], ALU.mult)
                    if STOP == 'x1':
                        continue
                    bk = nb()
                    P.mm(bk[:, :], ONES, G4.v.re("p h t -> p (h t)"))
                    if STOP == 'x2':
                        continue
                    P.copy(gcrow.v.re("p h t -> p (h t)"), bk[:, :])
                    if STOP == 'x3':
                        continue
                    P.act(egcrow.v.re("p h t -> p (h t)"), bk[:, :], AF.Exp)
                    if STOP == 'g2a':
                        continue
                    for h in range(4):
                        j, hp = h // 2, 64 * (h % 2)
                        P.tt(qd3[hp:hp + 64, j, :], qk[hp:hp + 64, j, :], egcrow[hp:hp + 64, h, :], ALU.mult)
                    if STOP == 'g2b':
                        continue
                    P.tt(h4(vb.v), h4(kv_tm[:, 256:512]), bc4(beta4.v), ALU.mult)
                    P.tt(h4(kbg.v), h4(kv_tm[:, 0:256]), bc4(bg.v), ALU.mult)
                    P.tt(h4(ktail.v), h4(kv_tm[:, 0:256]), bc4(ktf.v), ALU.mult)
                    if STOP == 'g3':
                        continue
                    for h in range(4):
                        j, hp = h // 2, 64 * (h % 2)
                        hm = HM[h]
                        dd = DD[h % 2]
                        kTh = qk[hp:hp + 64, 2 + j, :]
                        qTh = qk[hp:hp + 64, j, :]
                        bk = nb()
                        P.mmr(bk[:, 0:128], kTh, kTh)
                        P.mmr(bk[:, 128:256], kTh, qTh)
                        P.stt(dd["tmp"].v, gcrow[:, h, :], -1.0, NEGL, ALU.mult, ALU.add)
                        P.act(dd["D"].v, dd["tmp"].v, AF.Exp, bias=gc[:, h:h + 1])
                        P.tt(dd["tmp"].v, gcrow[:, h, :], NEGU, ALU.add)
                        P.act(dd["DT"].v, dd["tmp"].v, AF.Exp, bias=ngc[:, h:h + 1])
                        P.stt(dd["D"].v, bk[:, 0:128], beta4[:, h:h + 1], dd["D"].v, ALU.mult, ALU.mult)
                        P.tt(hm["PQ"][:, 0, :].bitcast(F32R), dd["D"].v, NSL, ALU.mult)
                        P.tt(hm["M1"].v, bk[:, 128:256], dd["DT"].v, ALU.mult)
                        bk2 = nb()
                        P.tr(bk2[:, 0:128], hm["PQ"][:, 0, :], ident)
                        P.copy(hm["PQ"][:, 1, :].bitcast(F32R), bk2[:, 0:128], eng="act")
                        P.tt(hm["R"].v.bitcast(F32R), bk2[:, 0:128], ident, ALU.add)
                    if STOP == 'g4':
                        continue
                    neumann()
                    if STOP == 'g5':
                        continue
                    bku = nb()
                    bkw = nb()
                    for h in range(4):
                        j, hp = h // 2, 64 * (h % 2)
                        TinvT = HM[h]["R"].v
                        P.mm(bku[:, h * 64:(h + 1) * 64], TinvT, vb[:, h * 64:(h + 1) * 64])
                        P.mm(bkw[hp:hp + 64, j * 128:(j + 1) * 128], kbg[:, h * 64:(h + 1) * 64], TinvT)
                    P.copy(u_sb.v, bku[:, 0:256], eng="act")
                    P.copy(wT_sb.v, bkw[:, 0:256])
                    if STOP == 'g6':
                        continue
                    bko = nb()
                    for c in range(2):
                        cs = slice(64 * c, 64 * c + 64)
                        bkv = nb()
                        for j in range(2):
                            P.mm(bkv[cs, j * 128:(j + 1) * 128], wT3[:, j, cs], Sg[:, j, :])
                        P.tt(vnew[cs, :], u_sb[cs, :], bkv[cs, 0:256], ALU.subtract)
                        bks = nb()
                        for j in range(2):
                            pc = slice(j * 128, (j + 1) * 128)
                            P.mm(bko[cs, pc], qd3[:, j, cs], Sg[:, j, :], start=True, stop=False)
                            for h_ in range(2):
                                h = 2 * j + h_
                                hc = slice(h * 64, (h + 1) * 64)
                                P.mm(bko[cs, hc], HM[h]["M1"][:, cs], vnew[:, hc], start=False, stop=(h_ == 1))
                        for j in range(2):
                            pc = slice(j * 128, (j + 1) * 128)
                            P.mm(bks[:, pc], ktail[cs, pc], vnew[cs, pc])
                        for j in range(2):
                            pc = slice(j * 128, (j + 1) * 128)
                            P.tt(stmp.v, bks[:, pc], BDm, ALU.mult)
                            P.stt(Sg[:, j, :], Sg[:, j, :], cdp[:, c * 2 + j:c * 2 + j + 1], stmp.v, ALU.mult, ALU.add)
                    P.copy(o_sb.v, bko[:, 0:256], eng="act")
                    P.tt(o_sq.v, o_sb.v, o_sb.v, ALU.mult)
                    P.reduce(s1[:, 4:8], h4(o_sq.v), ALU.add)
                    rsqrt(t4.v, s1[:, 4:8], 1e-6, 1.0 / 64)
                    P.tt(h4(o_sb.v), h4(o_sb.v), bc4(t4.v), ALU.mult)
                    P.tt(o_sb.v, o_sb.v, bcs[:, BS_GN:BS_GN + 256], ALU.mult)
                    P.tt(o_sb.v, o_sb.v, sz.v, ALU.mult)
                    bk = nb()
                    for c in range(2):
                        P.tr(bk[:, c * 128:(c + 1) * 128], o_sb[:, c * 128:(c + 1) * 128], ident)
                    P.copy(oT[:, 0:2, :], bk[:, 0:256].re("p (c t) -> p c t", c=2))
                    if oTf is not None:
                        P.copy(oTf[:, 0:2, :], bk[:, 0:256].re("p (c t) -> p c t", c=2))

                    if STOP == 'gdn':
                        continue
                    for c in range(2):
                        P.ts(kkT[:, c, :], rw[:, 2 + c, :], fmp[:, FM_KK + c:FM_KK + c + 1], ALU.mult)
                    P.tt(sq4[:, 0:2, :], kkT, kkT, ALU.mult)
                    bk = nb()
                    for c in range(2):
                        P.mm(bk[:, c * 128:(c + 1) * 128], BDm, sq4[:, c, :])
                    rsqrt(sq4[:, 0:2, :], bk[:, 0:256].re("p (c t) -> p c t", c=2), 1e-12)
                    P.tt(kkT, kkT, sq4[:, 0:2, :], ALU.mult)
                    for c in range(2):
                        P.ts(kmT[:, c, :], asg[:, c, :], -1.0, ALU.add, fmp[:, FM_KA + c:FM_KA + c + 1], ALU.mult)
                    P.stt(kmT, kmT, 1.0, rw[:, 2:4, :], ALU.add, ALU.mult)
                    for c in range(2):
                        for cc in range(2):
                            P.scan(cum[:, c, cc * 64:(cc + 1) * 64], ONES[:, 0:64], lz[:, c, cc * 64:(cc + 1) * 64],
                                   0.0, ALU.mult, ALU.add)
                    P.tt(eWp, cum, lz, ALU.subtract)
                    P.act(eWp, eWp, AF.Exp)
                    P.act(eWi, cum, AF.Exp, scale=-1.0)
                    P.act(eW, cum, AF.Exp)
                    P.stt(AtT.bitcast(F32R), kkT, -1.0, eWp, ALU.mult, ALU.mult)
                    P.tt(BtT.bitcast(F32R), kkT, asg, ALU.mult)
                    P.tt(BtT.bitcast(F32R), BtT, eWi, ALU.mult)
                    P.tt(KtT.bitcast(F32R), kmT, eWi, ALU.mult)
                    P.tt(RtT.bitcast(F32R), rw[:, 0:2, :], eW, ALU.mult)
                    for (q_, src) in ((0, BtT), (1, KtT), (2, rw[:, 4:6, :])):
                        bk = nb()
                        for c in range(2):
                            P.tr(bk[:, c * 128:(c + 1) * 128], src[:, c, :], ident)
                        P.copy(bkv_tm[:, q_, :], bk[:, 0:256], eng="act")
                    for h in range(4):
                        j, hp = h // 2, 64 * (h % 2)
                        hm = HM[h]
                        hs_ = slice(hp, hp + 64)
                        bk = nb()
                        P.mmr(bk[:, 0:128], AtT[hs_, j, :], BtT[hs_, j, :])
                        P.mmr(bk[:, 128:256], BtT[hs_, j, :], AtT[hs_, j, :])
                        P.mmr(bk[:, 256:384], KtT[hs_, j, :], AtT[hs_, j, :])
                        bk2 = nb()
                        P.mmr(bk2[:, 0:128], BtT[hs_, j, :], RtT[hs_, j, :])
                        P.mmr(bk2[:, 128:256], KtT[hs_, j, :], RtT[hs_, j, :])
                        P.tt(hm["PQ"][:, 0, :].bitcast(F32R), bk[:, 0:128], SLm, ALU.mult)
                        P.tt(hm["PQ"][:, 1, :].bitcast(F32R), bk[:, 128:256], SUm, ALU.mult)
                        P.tt(hm["R"].v.bitcast(F32R), hm["PQ"][:, 1, :], ident, ALU.add)
                        P.tt(hm["M1"].v, bk[:, 256:384], SUm, ALU.mult)
                        P.tt(hm["M2"].v, bk2[:, 0:128], IUm, ALU.mult)
                        P.tt(hm["M3"].v, bk2[:, 128:256], IUm, ALU.mult)
                    neumann()
                    bky = nb()
                    for c in range(2):
                        cs = slice(64 * c, 64 * c + 64)
                        last = 64 * c + 63
                        bkz = nb()
                        for j in range(2):
                            pc = slice(j * 128, (j + 1) * 128)
                            P.mm(bkz[cs, pc], AtT[:, j, cs], Sr[:, j, :], start=True, stop=False)
                            for h_ in range(2):
                                h = 2 * j + h_
                                hc = slice(h * 64, (h + 1) * 64)
                                P.mm(bkz[cs, hc], HM[h]["M1"][:, cs], bkv_tm[:, 2, hc], start=False, stop=(h_ == 1))
                        P.copy(Z_sb[cs, :], bkz[cs, 0:256])
                        bku_ = nb()
                        for h in range(4):
                            hc = slice(h * 64, (h + 1) * 64)
                            P.mm(bku_[cs, hc], HM[h]["R"][:, cs], Z_sb[:, hc])
                        P.copy(U_sb[cs, :], bku_[cs, 0:256], eng="act")
                        bks = nb()
                        for j in range(2):
                            pc = slice(j * 128, (j + 1) * 128)
                            P.mm(bky[cs, pc], RtT[:, j, cs], Sr[:, j, :], start=True, stop=False)
                            for h_ in range(2):
                                h = 2 * j + h_
                                hc = slice(h * 64, (h + 1) * 64)
                                P.mm(bky[cs, hc], HM[h]["M2"][:, cs], U_sb[:, hc], start=False, stop=False)
                                P.mm(bky[cs, hc], HM[h]["M3"][:, cs], bkv_tm[:, 2, hc], start=False, stop=(h_ == 1))
                        for j in range(2):
                            pc = slice(j * 128, (j + 1) * 128)
                            P.mm(bks[:, pc], bkv_tm[cs, 0, pc], U_sb[cs, pc], start=True, stop=False)
                            P.mm(bks[:, pc], bkv_tm[cs, 1, pc], bkv_tm[cs, 2, pc], start=False, stop=True)
                        for j in range(2):
                            pc = slice(j * 128, (j + 1) * 128)
                            P.tt(stmp.v, bks[:, pc], Sr[:, j, :], ALU.add)
                            P.stt(Sr[:, j, :], stmp.v, eW[:, j, last:last + 1], BDm, ALU.mult, ALU.mult)
                    P.copy(y_sb.v, bky[:, 0:256], eng="act")
                    P.reduce(s1[:, 4:8], h4(y_sb.v), ALU.add)
                    P.ts(t4.v, s1[:, 4:8], -1.0 / 64, ALU.mult)
                    P.tt(h4(y_sb.v), h4(y_sb.v), bc4(t4.v), ALU.add)
                    P.tt(y_sq.v, y_sb.v, y_sb.v, ALU.mult)
                    P.reduce(s1[:, 4:8], h4(y_sq.v), ALU.add)
                    rsqrt(t4.v, s1[:, 4:8], 64e-5, 1.0 / 64)
                    P.tt(h4(y_sb.v), h4(y_sb.v), bc4(t4.v), ALU.mult)
                    bk = nb()
                    for c in range(2):
                        P.tr(bk[:, c * 128:(c + 1) * 128], y_sb[:, c * 128:(c + 1) * 128], ident)
                    for c in range(2):
                        P.ts(ynT[:, c, :], bk[:, c * 128:(c + 1) * 128], fmp[:, FM_GNG + c:FM_GNG + c + 1], ALU.mult,
                             fmp[:, FM_GNB + c:FM_GNB + c + 1], ALU.add)
                        P.stt(rkb[:, c, :], rw[:, c, :], fmp[:, FM_RK + c:FM_RK + c + 1], kmT[:, c, :], ALU.mult, ALU.mult)
                    bk = nb()
                    for c in range(2):
                        P.mm(bk[:, c * 128:(c + 1) * 128], BDm, rkb[:, c, :])
                    P.tt(rkb, bk[:, 0:256].re("p (c t) -> p c t", c=2), rw[:, 4:6, :], ALU.mult)
                    P.tt(ynT, ynT, rkb, ALU.add)
                    P.tt(oT[:, 6:8, :], ynT, gateT, ALU.mult)
                    if oTf is not None:
                        P.tt(oTf[:, 6:8, :], ynT, gateT, ALU.mult)
                        tap("oT", lambda hd: hd[i], oTf.v.re("p c t -> p (c t)"))

                    if STOP == 'rwkv':
                        continue
                    for half in range(2):
                        bk = nb()
                        for c in range(8):
                            P.mm(bk[:, :], oT[:, c, :], w_out[:, c, half * 512:(half + 1) * 512],
                                 start=(c == 0), stop=(c == 7))
                        hsl = slice(half * 512, (half + 1) * 512)
                        P.tt(hh[:, hsl], bk[:, :], modb[:, MB_GTM, hsl], ALU.mult)
                        P.tt(xt[:, hsl], xt[:, hsl], hh[:, hsl], ALU.add)
                    P.dma("sp", xres[tsl, :], xt.v)
                    if l == 0:
                        tap("x1", lambda hd: hd[tsl, :], xt.v)

                    if STOP == 'wout':
                        continue
                    rstd_of(s1[:, 1:2], xt.v, 1e-6)
                    P.stt(hh.v, xt.v, s1[:, 1:2], modb[:, MB_G2, :], ALU.mult, ALU.mult)
                    P.tt(hh.v, hh.v, modb[:, MB_SHF, :], ALU.add)
                    P.dma("sp", hf[tsl, :], hh.v)
                    if l == 0:
                        tap("h2", lambda hd: hd[tsl, :], hh.v)
                    for half in range(2):
                        bk = nb()
                        for c4 in range(4):
                            c = half * 4 + c4
                            P.tr(bk[:, c4 * 128:(c4 + 1) * 128], hh[:, c * 128:(c + 1) * 128], ident)
                        P.copy(h2T[:, half * 4:(half + 1) * 4, :], bk[:, :].re("p (c t) -> p c t", c=4), eng="act")
                    bk = nb()
                    for k in range(8):
                        P.mm(bk[:, 0:72], h2T[:, k, :], wr[:, k, :], start=(k == 0), stop=(k == 7))
                    P.tt(lg.v, bk[:, 0:72], bcs[:, BS_RB:BS_RB + 72], ALU.add)
                    P.reduce(sc[:, 0:1], lg[:, 0:8], ALU.max)
                    P.ts(ohg.v, lg[:, 0:8], sc[:, 0:1], ALU.is_equal)
                    P.ts(sc[:, 1:2], sc[:, 0:1], -1.0, ALU.mult)
                    P.act(sel.v, lg[:, 0:8], AF.Exp, bias=sc[:, 1:2], accum=sc[:, 2:3])
                    recip(sc[:, 2:3], sc[:, 2:3])
                    P.tt(r8.v, lg[:, 8:72].re("p (g j) -> p g j", g=8), ohg.v.unsq(2).bc([128, 8, 8]), ALU.mult)
                    P.reduce(sel.v, r8.v.re("p g j -> p j g"), ALU.add)
                    P.reduce(sc[:, 3:4], sel.v, ALU.max)
                    P.ts(oh1.v, sel.v, sc[:, 3:4], ALU.is_equal)
                    P.stt(sel.v, oh1.v, -1.0e30, sel.v, ALU.mult, ALU.add)
                    P.reduce(sc[:, 4:5], sel.v, ALU.max)
                    P.ts(oh2.v, sel.v, sc[:, 4:5], ALU.is_equal)
                    P.tt(sc[:, 5:6], sc[:, 3:4], sc[:, 4:5], ALU.subtract)
                    P.act(sc[:, 5:6], sc[:, 5:6], AF.Exp, scale=-1.0)
                    P.ts(sc[:, 5:6], sc[:, 5:6], 1.0, ALU.add)
                    recip(sc[:, 5:6], sc[:, 5:6])
                    P.tt(gate[:, i, 0:1], sc[:, 5:6], sc[:, 2:3], ALU.mult)
                    P.tt(gate[:, i, 1:2], sc[:, 2:3], gate[:, i, 0:1], ALU.subtract)
                    for (mm_, oh) in ((Mx[0], oh1), (Mx[1], oh2)):
                        P.tt(mm_.v.re("p (g j) -> p g j", g=8), ohg.v.unsq(2).bc([128, 8, 8]),
                             oh.v.unsq(1).bc([128, 8, 8]), ALU.mult)
                    P.tt(Mx[2].v, Mx[0].v, Mx[1].v, ALU.add)
                    bk = nb()
                    P.mm(bk[:, 0:64], SU128, Mx[2].v)
                    P.mm(bk[:, 64:128], ONES, Mx[2].v)
                    P.tt(rke.v, bk[:, 0:64], base.v, ALU.add)
                    P.tt(base.v, base.v, bk[:, 64:128], ALU.add)
                    for cix in range(2):
                        P.tt(rt.v, Mx[cix].v, rke.v, ALU.mult)
                        P.reduce(rank[:, i, cix:cix + 1], rt.v, ALU.add)
                        P.tt(rt.v, Mx[cix].v, IOTA64, ALU.mult)
                        P.reduce(eidx[:, i, cix:cix + 1], rt.v, ALU.add)
                    if "route" in tap_d and l == 0:
                        P.copy(lg[:, 0:2], eidx[:, i, :])
                        P.copy(lg[:, 2:4], gate[:, i, :])
                        P.copy(lg[:, 4:6], rank[:, i, :])
                        tap("route", lambda hd: hd[tsl, :], lg[:, 0:6])

            if STOP in ('proj', 'sgu', 'gdn', 'rwkv', 'wout', 'A', 'x1', 'x2', 'x3', 'g1', 'g2', 'g2a', 'g2b', 'g3', 'g4', 'g5', 'g6', 'g7'):
                return
            with P.scope():
                padded = P.sb("padded", [128, 64])
                pend = P.sb("pend", [128, 64])
                pstart = P.sb("pstart", [128, 64])
                tm_ = P.sb("tm_", [128, 64])
                blke = P.sb("blke", [128, NBLK])
                dtmp = P.sb("dtmp", [128, NT * 2])
                padi = P.sb("padi", [128, 64], I32)
                P.ts(padded.v, base.v, 127.0, ALU.add)
                P.copy(padi.v, padded.v)
                P.ts(padi.v, padi.v, 7, ALU.arith_shift_right, 7, ALU.logical_shift_left)
                P.copy(padded.v, padi.v)
                P.scan(pend.v, ONES[:, 0:64], padded.v, 0.0, ALU.mult, ALU.add)
                P.tt(pstart.v, pend.v, padded.v, ALU.subtract)
                P.memset(blke.v, 0.0)
                P.copy(dest_f.v, rank.v)
                ef = eidx.v.re("p t c -> p (t c)")
                df = dest_f.v.re("p t c -> p (t c)")
                for e_ in range(NEXP):
                    P.stt(blke.v, IOTAB, pend[:, e_:e_ + 1], blke.v, ALU.is_ge, ALU.add)
                    P.stt(dtmp.v, ef, float(e_), pstart[:, e_:e_ + 1].bc([128, NT * 2]), ALU.is_equal, ALU.mult)
                    P.tt(df, df, dtmp.v, ALU.add)
                P.ts(blke.v, blke.v, 63.0, ALU.min)
                same2 = P.sb("same2", [128, NBLK])
                P.memset(same2.v, 0.0)
                P.tt(same2[:, 2:NBLK], blke[:, 2:NBLK], blke[:, 0:NBLK - 2], ALU.is_equal)
                P.ts(blke.v, blke.v, float(l * NEXP), ALU.add, 128.0, ALU.mult)
                P.ts(blke.v, blke.v, PIDX[:, 0:1], ALU.add)
                P.stt(blke.v, same2.v, 4194304.0, blke.v, ALU.mult, ALU.add)
                P.copy(idxb.v, blke.v)
                P.copy(dest_i.v, dest_f.v)
                P.copy(gtf_prev.v, modb[:, MB_GTF, :])

            if STOP == 'fin':
                return
            with P.scope():
                hbuf = [P.sb("hbuf%d" % i, [128, D]) for i in range(2)]
                for i in range(NT):
                    hb = hbuf[i % 2]
                    P.dma("sp", hb.v, hf[i * 128:(i + 1) * 128, :])
                    P.scatter(hs.v, hb.v, dest_i[:, i, 0:1])
                    P.scatter(hs.v, hb.v, dest_i[:, i, 1:2])

            if STOP == 'scatter':
                return
            with P.scope():
                wgs = [P.sb("wgs%d" % i, [128, 8, 256], BF16) for i in range(2)]
                wus = [P.sb("wus%d" % i, [128, 8, 256], BF16) for i in range(2)]
                wds = [P.sb("wds%d" % i, [128, 2, D], BF16) for i in range(2)]
                xbs = [P.sb("xbs%d" % i, [128, D], BF16) for i in range(2)]
                xbT = P.sb("xbT", [128, 8, 128], BF16)
                sg = P.sb("sg", [128, 256])
                hidT = P.sb("hidT", [128, 2, 128], BF16)
                hid = P.sb("hid", [128, 256], BF16)
                yo = [P.sb("yo%d" % i, [128, D]) for i in range(2)]
                xbTs = [xbT, P.sb("xbT2", [128, 8, 128], BF16)]
                WB = L * NEXP * 128 - 1

                def s1(b):
                    wg_, wu_, wd_, xb = wgs[b % 2], wus[b % 2], wds[b % 2], xbs[b % 2]
                    P.dma("sp", xb.v, hs[b * 128:(b + 1) * 128, :])
                    ix = idxb[:, b:b + 1]
                    P.gather(wg_.v.re("p k f -> p (k f)"), ewg_d.v, ix, bound=WB)
                    P.gather(wu_.v.re("p k f -> p (k f)"), ewu_d.v, ix, bound=WB)
                    P.gather(wd_.v.re("p j d -> p (j d)"), ewd_d.v, ix, bound=WB)
                    bk = nb()
                    bkb = bk.v.bitcast(BF16)
                    xbv = xb.v.re("p (q k) -> p k q", k=8)
                    for k in range(8):
                        P.tr(bkb[:, k * 128:(k + 1) * 128], xbv[:, k, :], identb.v)
                    P.copy(xbTs[b % 2].v.re("p k t -> p (k t)"), bkb[:, 0:1024], eng="act")

                def s2(b):
                    wg_, wu_ = wgs[b % 2], wus[b % 2]
                    bkg_ = nb()
                    bku_ = nb()
                    for (bk_, w_) in ((bkg_, wg_), (bku_, wu_)):
                        for k in range(8):
                            P.mm(bk_[:, 0:256], xbTs[b % 2][:, k, :], w_[:, k, :], start=(k == 0), stop=(k == 7))
                    P.act(sg.v, bkg_[:, 0:256], AF.Silu)
                    P.tt(hid.v, sg.v, bku_[:, 0:256], ALU.mult)

                def s3(b):
                    wd_, yob = wds[b % 2], yo[b % 2]
                    bkh = nb()
                    bkhb = bkh.v.bitcast(BF16)
                    hv = hid.v.re("p (q j) -> p j q", j=2)
                    for j in range(2):
                        P.tr(bkhb[:, j * 128:(j + 1) * 128], hv[:, j, :], identb.v)
                    P.copy(hidT.v.re("p j t -> p (j t)"), bkhb[:, 0:256])
                    for half in range(2):
                        bk = nb()
                        for j in range(2):
                            P.mm(bk[:, :], hidT[:, j, :], wd_[:, j, half * 512:(half + 1) * 512],
                                 start=(j == 0), stop=(j == 1))
                        P.copy(yob[:, half * 512:(half + 1) * 512], bk[:, :], eng=("act" if half else "dve"))
                    P.dma("sp", ys[b * 128:(b + 1) * 128, :], yob.v)

                s1(0)
                for b in range(NBLK):
                    s2(b)
                    if b + 1 < NBLK:
                        s1(b + 1)
                    s3(b)

        if STOP == 'B':
            return
        with P.scope():
            fing = P.sb("fing", [128, D])
            P.dma("sp", fing.v, fing_d.v.pbc(128))
            xf = [P.sb("xf%d" % i, [128, D]) for i in range(2)]
            yf = [P.sb("yf%d" % i, [128, D]) for i in range(2)]
            zf = [P.sb("zf%d" % i, [128, D]) for i in range(2)]
            sf = P.sb("sf", [128, 4])
            for i in range(NT):
                tsl = slice(i * 128, (i + 1) * 128)
                xt, ya, yb = xf[i % 2], yf[i % 2], zf[i % 2]
                P.dma("sp", xt.v, xres[tsl, :])
                P.gather(ya.v, ys.v, dest_i[:, i, 0:1])
                P.gather(yb.v, ys.v, dest_i[:, i, 1:2])
                P.ts(ya.v, ya.v, gate[:, i, 0:1], ALU.mult)
                P.stt(ya.v, yb.v, gate[:, i, 1:2], ya.v, ALU.mult, ALU.add)
                P.tt(ya.v, ya.v, gtf_prev.v, ALU.mult)
                P.tt(xt.v, xt.v, ya.v, ALU.add)
                if L == 1:
                    tap("x2", lambda hd: hd[tsl, :], xt.v)
                P.act(yb.v, xt.v, AF.Square, accum=sf[:, 0:1])
                rsqrt(sf[:, 1:2], sf[:, 0:1], 1e-6, 1.0 / D)
                P.stt(xt.v, xt.v, sf[:, 1:2], fing.v, ALU.mult, ALU.mult)
                P.dma("sp", out_d[tsl, :], xt.v)

    P.run(body)
    return nc, cvals, P


def prep_inputs(inp, SEQ, DEPTH, cvals):
    f = lambda a: np.ascontiguousarray(np.asarray(a, dtype=np.float32))
    L = DEPTH
    H, N, G = 4, 64, 256

    def fm(v, nch):
        return np.asarray(v).reshape(L, nch, 128).transpose(0, 2, 1)

    fmp = np.concatenate([
        fm(inp["rw_mu"][:L], 8), fm(inp["rw_w0"][:L], 2), fm(inp["rw_a0"][:L], 2), fm(inp["rw_k_k"][:L], 2),
        fm(inp["rw_k_a"][:L], 2), fm(np.asarray(inp["rw_r_k"][:L]).reshape(L, G), 2), fm(inp["rw_gn_g"][:L], 2),
        fm(inp["rw_gn_b"][:L], 2),
        np.asarray(inp["gdn_conv_w"][:L]).reshape(L, 4, 6, 128).transpose(0, 3, 2, 1).reshape(L, 128, 24),
        np.asarray(inp["sc_conv_w"][:L]).reshape(L, 3, 2, 128).transpose(0, 3, 2, 1).reshape(L, 128, 6),
    ], axis=2)
    assert fmp.shape[2] == NFM
    bcs = np.concatenate([
        inp["sgu_ln_g"][:L], inp["sgu_ln_b"][:L], np.tile(np.asarray(inp["gdn_norm_g"][:L]), (1, 4)),
        inp["gdn_a_log"][:L], inp["gdn_dt_bias"][:L], inp["moe_b_group"][:L], inp["moe_b_router"][:L]], axis=1)
    assert bcs.shape[1] == NBS
    shared = {
        "ada_w": f(inp["ada_w"][:L]), "ada_b": f(inp["ada_b"][:L]),
        "g12": f(np.concatenate([inp["mix_norm_g"][:L], inp["ffn_norm_g"][:L]], axis=1)),
        "w_in": f(inp["w_in"][:L]), "w_out": f(inp["w_out"][:L]),
        "fmp": f(fmp), "bcs": f(bcs),
        "sgu_wT": f(np.asarray(inp["sgu_w"][:L]).transpose(0, 1, 3, 2)),
        "sgu_bT": f(np.asarray(inp["sgu_b"][:L]).transpose(0, 2, 1)),
        "lora_wa": f(np.concatenate([inp["rw_w_up"][:L], inp["rw_a_up"][:L]], axis=1)),
        "g_up": f(inp["rw_g_up"][:L]),
        "wr": f(np.concatenate([inp["moe_w_group"][:L], inp["moe_w_router"][:L]], axis=2)),
        "ewg": f(np.asarray(inp["moe_w_gate"][:L]).reshape(L * NEXP * 128, 2048)),
        "ewu": f(np.asarray(inp["moe_w_up"][:L]).reshape(L * NEXP * 128, 2048)),
        "ewd": f(np.asarray(inp["moe_w_down"][:L]).reshape(L * NEXP * 128, 2048)),
        "fin_g": f(inp["final_norm_g"]),
        "consts": cvals,
    }
    x = np.asarray(inp["x"])
    c = np.asarray(inp["c"])
    per_core = []
    for b in range(x.shape[0]):
        d = dict(shared)
        d["x"] = f(x[b, :SEQ])
        d["cfm"] = f(c[b].reshape(8, 128).T)
        per_core.append(d)
    return per_core


_CACHE = {}


def kernel(**inputs):
    SEQ, DEPTH, B = 8192, 4, 8
    if "nc" not in _CACHE:
        _CACHE["nc"] = build(SEQ, DEPTH)
    nc, cvals, _ = _CACHE["nc"]
    in_maps = prep_inputs(inputs, SEQ, DEPTH, cvals)
    res = run_bass_kernel_spmd(nc, in_maps, core_ids=list(range(B)))
    return np.stack([np.asarray(r["out"], dtype=np.float32) for r in res.results], axis=0)
```
